# Optimizing a Trainium2 kernel written in Bass

```python
import jax, jax.numpy as jnp
from jax import lax
import numpy as np

D_MODEL = 2048
BATCH = 4
SEQ = 4096
DEPTH = 2

GRID_W = 64
CTX_LEN = 256

SWA_HEADS = 6
SWA_KV_HEADS = 2
SWA_HEAD_DIM = 128
SWA_WINDOW = 128
SWA_BLOCK = 128
RET_HEADS = 6
RET_QK_DIM = 64
RET_V_DIM = 128
RET_CHUNK = 128
RET_DECAY_BASE = 5.0
MLA_HEADS = 4
MLA_NOPE_DIM = 128
MLA_ROPE_DIM = 64
MLA_V_DIM = 128
MLA_KV_RANK = 256
MLA_Q_BLOCK = 128

SWA_WIDTH = SWA_HEADS * SWA_HEAD_DIM
RET_WIDTH = RET_HEADS * RET_V_DIM
MLA_WIDTH = MLA_HEADS * MLA_V_DIM
MIX_WIDTH = SWA_WIDTH + RET_WIDTH + MLA_WIDTH

IN_SPLITS = (SWA_HEADS * SWA_HEAD_DIM, SWA_KV_HEADS * SWA_HEAD_DIM, SWA_KV_HEADS * SWA_HEAD_DIM,
             RET_HEADS * RET_QK_DIM, RET_HEADS * RET_QK_DIM, RET_WIDTH, RET_WIDTH, RET_WIDTH,
             MLA_HEADS * (MLA_NOPE_DIM + MLA_ROPE_DIM), MLA_KV_RANK, MLA_ROPE_DIM)
IN_WIDTH = sum(IN_SPLITS)

N_GROUPS = 4
EXPERTS_PER_GROUP = 8
N_EXPERTS = N_GROUPS * EXPERTS_PER_GROUP
TOP_K = 2
EXPERT_HIDDEN = 1024
MOE_BLOCK = 128

ALPHA = (2 * DEPTH) ** 0.25
BETA = (8 * DEPTH) ** -0.25
ROPE_BASE = 10000.0
NORM_EPS = 1e-6
NEG_INF = -1e30

kernel_name = 'hybrid_dit_swa_retnet_mla_hmoe'


def layer_norm(x, eps=NORM_EPS):
    xf = x.astype(jnp.float32)
    mu = jnp.mean(xf, axis=-1, keepdims=True)
    xc = xf - mu
    var = jnp.mean(xc * xc, axis=-1, keepdims=True)
    return (xc * lax.rsqrt(var + eps)).astype(x.dtype)


def affine_layer_norm(x, g, b):
    return layer_norm(x) * g + b


def rms_norm(x, g, eps=NORM_EPS):
    xf = x.astype(jnp.float32)
    y = xf * lax.rsqrt(jnp.mean(xf * xf, axis=-1, keepdims=True) + eps)
    return y.astype(x.dtype) * g


def modulate(x, shift, scale):
    return layer_norm(x) * (1.0 + scale) + shift


def rope_1d(x, pos):
    half = x.shape[-1] // 2
    inv_freq = ROPE_BASE ** (-jnp.arange(half, dtype=jnp.float32) / half)
    ang = pos.astype(jnp.float32)[:, None] * inv_freq[None, :]
    cos = jnp.cos(ang)[:, None, :].astype(x.dtype)
    sin = jnp.sin(ang)[:, None, :].astype(x.dtype)
    x1, x2 = x[..., :half], x[..., half:]
    return jnp.concatenate([x1 * cos - x2 * sin, x2 * cos + x1 * sin], axis=-1)


def rope_2d(x, rows, cols):
    d = x.shape[-1] // 2
    return jnp.concatenate([rope_1d(x[..., :d], rows), rope_1d(x[..., d:], cols)], axis=-1)


def split_projection(t):
    pts, acc = [], 0
    for s in IN_SPLITS[:-1]:
        acc += s
        pts.append(acc)
    return jnp.split(t, pts, axis=-1)


def swa_mixer(q, k, v, qc, kc, vc, sink, rows, cols, compute_ctx):
    B, L = q.shape[0], q.shape[1]
    n_ctx = kc.shape[1]
    nb = L // SWA_BLOCK
    G = SWA_HEADS // SWA_KV_HEADS
    scale = SWA_HEAD_DIM ** -0.5
    qb = rope_2d(q, rows, cols).reshape(B, nb, SWA_BLOCK, SWA_KV_HEADS, G, SWA_HEAD_DIM)
    k = rope_2d(k, rows, cols)

    def band(t):
        pad = jnp.zeros((B, SWA_BLOCK) + t.shape[2:], t.dtype)
        tb = jnp.concatenate([pad, t, pad], axis=1).reshape((B, nb + 2, SWA_BLOCK) + t.shape[2:])
        return jnp.concatenate([tb[:, :-2], tb[:, 1:-1], tb[:, 2:]], axis=2)

    kw, vw = band(k), band(v)
    q_pos = jnp.arange(nb)[:, None] * SWA_BLOCK + jnp.arange(SWA_BLOCK)[None, :]
    k_pos = jnp.arange(nb)[:, None] * SWA_BLOCK - SWA_BLOCK + jnp.arange(3 * SWA_BLOCK)[None, :]
    valid = ((jnp.abs(q_pos[:, :, None] - k_pos[:, None, :]) <= SWA_WINDOW)
             & (k_pos >= 0)[:, None, :] & (k_pos < L)[:, None, :])
    s_loc = jnp.einsum('bnqhgd,bnkhd->bnhgqk', qb, kw).astype(jnp.float32) * scale
    s_loc = jnp.where(valid[None, :, None, None], s_loc, NEG_INF)
    s_ctx = jnp.einsum('bnqhgd,bkhd->bnhgqk', qb, kc).astype(jnp.float32) * scale
    sink_f = sink.astype(jnp.float32)
    sink_l = jnp.broadcast_to(sink_f.reshape(1, 1, SWA_KV_HEADS, G, 1, 1), s_loc.shape[:-1] + (1,))
    p = jax.nn.softmax(jnp.concatenate([sink_l, s_ctx, s_loc], axis=-1), axis=-1).astype(v.dtype)
    out = (jnp.einsum('bnhgqk,bkhd->bnqhgd', p[..., 1:1 + n_ctx], vc)
           + jnp.einsum('bnhgqk,bnkhd->bnqhgd', p[..., 1 + n_ctx:], vw)).reshape(B, L, SWA_WIDTH)
    out_c = None
    if compute_ctx:
        qcg = qc.reshape(B, n_ctx, SWA_KV_HEADS, G, SWA_HEAD_DIM)
        s = jnp.einsum('bqhgd,bkhd->bhgqk', qcg, kc).astype(jnp.float32) * scale
        sink_c = jnp.broadcast_to(sink_f.reshape(1, SWA_KV_HEADS, G, 1, 1), s.shape[:-1] + (1,))
        pc = jax.nn.softmax(jnp.concatenate([sink_c, s], axis=-1), axis=-1).astype(vc.dtype)
        out_c = jnp.einsum('bhgqk,bkhd->bqhgd', pc[..., 1:], vc).reshape(B, n_ctx, SWA_WIDTH)
    return out, out_c


def retention_chunks(q, k, v, log_gamma, s0):
    B, H, T, dk = q.shape
    dv = v.shape[-1]
    C = RET_CHUNK
    n = T // C
    qc = q.reshape(B, H, n, C, dk)
    kc = k.reshape(B, H, n, C, dk)
    vc = v.reshape(B, H, n, C, dv)
    idx = jnp.arange(C, dtype=jnp.float32)
    diff = idx[:, None] - idx[None, :]
    dmat = jnp.where(diff >= 0, jnp.exp(log_gamma[:, None, None] * jnp.maximum(diff, 0.0)), 0.0)
    q_decay = jnp.exp(log_gamma[:, None] * (idx + 1.0))
    k_decay = jnp.exp(log_gamma[:, None] * (C - 1.0 - idx))
    c_decay = jnp.exp(log_gamma * C)
    scores = jnp.einsum('bhncd,bhnsd->bhncs', qc, kc) * dmat[None, :, None]
    y = jnp.einsum('bhncs,bhnse->bhnce', scores, vc)
    kv = jnp.einsum('bhnsd,bhnse->nbhde', kc * k_decay[None, :, None, :, None], vc)

    def step(s, kv_i):
        return c_decay[None, :, None, None] * s + kv_i, s

    s_last, s_prev = lax.scan(step, s0, kv)
    y = y + jnp.einsum('bhncd,nbhde->bhnce', qc * q_decay[None, :, None, :, None], s_prev)
    return y.reshape(B, H, T, dv), s_last


def retention_mixer(q, k, v, gf, gb, qc, kc, vc, gfc, gbc, decay_exp, compute_ctx):
    B, L = q.shape[0], q.shape[1]
    n_ctx = qc.shape[1]
    pos_c = jnp.arange(n_ctx, dtype=jnp.float32)
    pos_l = n_ctx + jnp.arange(L, dtype=jnp.float32)
    qk_scale = RET_QK_DIM ** -0.5

    def heads(t):
        return jnp.transpose(t, (0, 2, 1, 3)).astype(jnp.float32)

    q_l, k_l, v_l = heads(rope_1d(q, pos_l) * qk_scale), heads(rope_1d(k, pos_l)), heads(v)
    q_c, k_c, v_c = heads(rope_1d(qc, pos_c) * qk_scale), heads(rope_1d(kc, pos_c)), heads(vc)
    log_gamma = jnp.log1p(-jnp.exp2(-decay_exp.astype(jnp.float32)))
    s0 = jnp.zeros((B, RET_HEADS, RET_QK_DIM, RET_V_DIM), jnp.float32)

    def rev(t):
        return jnp.flip(t, axis=2)

    yc_f, sc_f = retention_chunks(q_c, k_c, v_c, log_gamma[0], s0)
    yc_b, sc_b = retention_chunks(rev(q_c), rev(k_c), rev(v_c), log_gamma[1], s0)
    y_f, _ = retention_chunks(q_l, k_l, v_l, log_gamma[0], sc_f)
    y_b, _ = retention_chunks(rev(q_l), rev(k_l), rev(v_l), log_gamma[1], sc_b)

    def merge(yf, yb, g_f, g_b):
        def norm(y):
            yn = layer_norm(y)
            return jnp.transpose(yn, (0, 2, 1, 3)).reshape(B, -1, RET_WIDTH).astype(g_f.dtype)
        return jax.nn.silu(g_f) * norm(yf) + jax.nn.silu(g_b) * norm(yb)

    out = merge(y_f, rev(y_b), gf, gb)
    out_c = merge(yc_f, rev(yc_b), gfc, gbc) if compute_ctx else None
    return out, out_c


def mla_mixer(q, ckv, k_rope, qc, ckv_c, k_rope_c, kv_norm_g, w_uk, w_uv, rows, cols, compute_ctx):
    B, L = q.shape[0], q.shape[1]
    n_ctx = qc.shape[1]
    scale = (MLA_NOPE_DIM + MLA_ROPE_DIM) ** -0.5
    w_uk = w_uk.reshape(MLA_KV_RANK, MLA_HEADS, MLA_NOPE_DIM)
    w_uv = w_uv.reshape(MLA_KV_RANK, MLA_HEADS, MLA_V_DIM)

    def expand(c):
        c = rms_norm(c, kv_norm_g)
        return jnp.einsum('btr,rhd->bthd', c, w_uk), jnp.einsum('btr,rhd->bthd', c, w_uv)

    k_nope, v = expand(ckv)
    kc_nope, vc = expand(ckv_c)
    k_rope = rope_2d(k_rope[:, :, None, :], rows, cols)[:, :, 0]
    q_nope = q[..., :MLA_NOPE_DIM]
    q_rope = rope_2d(q[..., MLA_NOPE_DIM:], rows, cols)
    nb = L // MLA_Q_BLOCK

    def to_blocks(t):
        return jnp.moveaxis(t.reshape((B, nb, MLA_Q_BLOCK) + t.shape[2:]), 1, 0)

    def attend(args):
        qn, qr = args
        s_c = jnp.einsum('bqhd,bkhd->bhqk', qn, kc_nope) + jnp.einsum('bqhd,bkd->bhqk', qr, k_rope_c)
        s_l = jnp.einsum('bqhd,bkhd->bhqk', qn, k_nope) + jnp.einsum('bqhd,bkd->bhqk', qr, k_rope)
        p = jax.nn.softmax(jnp.concatenate([s_c, s_l], axis=-1).astype(jnp.float32) * scale, axis=-1).astype(v.dtype)
        return jnp.einsum('bhqk,bkhd->bqhd', p[..., :n_ctx], vc) + jnp.einsum('bhqk,bkhd->bqhd', p[..., n_ctx:], v)

    out = lax.map(attend, (to_blocks(q_nope), to_blocks(q_rope)))
    out = jnp.moveaxis(out, 0, 1).reshape(B, L, MLA_WIDTH)
    out_c = None
    if compute_ctx:
        s = (jnp.einsum('bqhd,bkhd->bhqk', qc[..., :MLA_NOPE_DIM], kc_nope)
             + jnp.einsum('bqhd,bkd->bhqk', qc[..., MLA_NOPE_DIM:], k_rope_c))
        pc = jax.nn.softmax(s.astype(jnp.float32) * scale, axis=-1).astype(vc.dtype)
        out_c = jnp.einsum('bhqk,bkhd->bqhd', pc, vc).reshape(B, n_ctx, MLA_WIDTH)
    return out, out_c


def token_mixer(h, hc, w_in, w_out, swa_sink, ret_decay, mla_kv_norm, mla_w_uk, mla_w_uv, rows, cols, compute_ctx):
    B, L, _ = h.shape
    n_ctx = hc.shape[1]

    def hd(t, n):
        return t.reshape(t.shape[0], t.shape[1], n, -1)

    sq, sk, sv, rq, rk, rv, rgf, rgb, mq, mckv, mkr = split_projection(h @ w_in)
    csq, csk, csv, crq, crk, crv, crgf, crgb, cmq, cmckv, cmkr = split_projection(hc @ w_in)
    a, a_c = swa_mixer(hd(sq, SWA_HEADS), hd(sk, SWA_KV_HEADS), hd(sv, SWA_KV_HEADS),
                       hd(csq, SWA_HEADS), hd(csk, SWA_KV_HEADS), hd(csv, SWA_KV_HEADS),
                       swa_sink, rows, cols, compute_ctx)
    r, r_c = retention_mixer(hd(rq, RET_HEADS), hd(rk, RET_HEADS), hd(rv, RET_HEADS), rgf, rgb,
                             hd(crq, RET_HEADS), hd(crk, RET_HEADS), hd(crv, RET_HEADS), crgf, crgb,
                             ret_decay, compute_ctx)
    m, m_c = mla_mixer(hd(mq, MLA_HEADS), mckv, mkr, hd(cmq, MLA_HEADS), cmckv, cmkr,
                       mla_kv_norm, mla_w_uk, mla_w_uv, rows, cols, compute_ctx)
    y = jnp.concatenate([a, r, m], axis=-1) @ w_out
    y_c = jnp.concatenate([a_c, r_c, m_c], axis=-1) @ w_out if compute_ctx else None
    return y, y_c


def routed_experts(h, expert_id, weight, w_gate, w_up, w_down):
    N, D = h.shape
    A = expert_id.shape[0]
    n_blocks = (A + MOE_BLOCK - 1) // MOE_BLOCK + N_EXPERTS
    rows_total = n_blocks * MOE_BLOCK
    tok = jnp.arange(A, dtype=jnp.int32) // TOP_K
    order = jnp.argsort(expert_id)
    e_sorted = expert_id[order]
    counts = jnp.bincount(expert_id, length=N_EXPERTS)
    starts = jnp.cumsum(counts) - counts
    padded = (counts + MOE_BLOCK - 1) // MOE_BLOCK * MOE_BLOCK
    ends = jnp.cumsum(padded)
    pstarts = ends - padded
    dest = pstarts[e_sorted] + jnp.arange(A, dtype=jnp.int32) - starts[e_sorted]
    buf_tok = jnp.full((rows_total,), N, jnp.int32).at[dest].set(tok[order])
    buf_w = jnp.zeros((rows_total,), jnp.float32).at[dest].set(weight[order].astype(jnp.float32))
    block_expert = jnp.minimum(jnp.searchsorted(ends, jnp.arange(n_blocks, dtype=jnp.int32) * MOE_BLOCK, side='right'),
                               N_EXPERTS - 1)
    h_pad = jnp.concatenate([h, jnp.zeros((1, D), h.dtype)], axis=0)

    def expert_block(args):
        tok_blk, e = args
        xb = h_pad[tok_blk]
        return (jax.nn.silu(xb @ w_gate[e]) * (xb @ w_up[e])) @ w_down[e]

    yb = lax.map(expert_block, (buf_tok.reshape(n_blocks, MOE_BLOCK), block_expert)).reshape(rows_total, D)
    yb = yb * buf_w[:, None].astype(yb.dtype)
    return jax.ops.segment_sum(yb, buf_tok, num_segments=N + 1)[:N]


def hier_moe(h, w_group, b_group, w_expert, b_expert, w_gate, w_up, w_down):
    N = h.shape[0]
    g_logits = (h @ w_group).astype(jnp.float32) + b_group.astype(jnp.float32)
    g_prob = jax.nn.softmax(g_logits, axis=-1)
    g_idx = jnp.argmax(g_logits, axis=-1)
    rows = jnp.arange(N)
    p_group = g_prob[rows, g_idx][:, None]
    e_logits = ((h @ w_expert).astype(jnp.float32) + b_expert.astype(jnp.float32)).reshape(N, N_GROUPS, EXPERTS_PER_GROUP)
    e_in = e_logits[rows, g_idx]
    e_top, e_idx = lax.top_k(e_in, TOP_K)
    gate = jax.nn.softmax(e_top, axis=-1) * p_group
    expert_id = (g_idx[:, None] * EXPERTS_PER_GROUP + e_idx).astype(jnp.int32).reshape(-1)
    return routed_experts(h, expert_id, gate.reshape(-1), w_gate, w_up, w_down)


def setup_inputs(seed: int = 0) -> dict:
    key = jax.random.key(seed)
    ks = jax.random.split(key, 26)
    f32 = jnp.float32
    Dm = D_MODEL

    def nrm(k, shape, scale):
        return jax.random.normal(k, shape, f32) * scale

    return {
        'x': nrm(ks[0], (BATCH, SEQ, Dm), 1.0),
        'c': nrm(ks[1], (BATCH, Dm), 1.0),
        'ctx': nrm(ks[2], (BATCH, CTX_LEN, Dm), 1.0),
        'c_ctx': nrm(ks[3], (Dm,), 1.0),
        'w_ada': nrm(ks[4], (DEPTH, Dm, 6 * Dm), Dm ** -0.5),
        'b_ada': nrm(ks[5], (DEPTH, 6 * Dm), 0.02),
        'w_in': nrm(ks[6], (DEPTH, Dm, IN_WIDTH), Dm ** -0.5),
        'swa_sink': nrm(ks[7], (DEPTH, SWA_HEADS), 0.5),
        'ret_decay': RET_DECAY_BASE + jnp.arange(RET_HEADS, dtype=f32) + nrm(ks[8], (DEPTH, 2, RET_HEADS), 0.1),
        'mla_kv_norm': 1.0 + nrm(ks[9], (DEPTH, MLA_KV_RANK), 0.02),
        'mla_w_uk': nrm(ks[10], (DEPTH, MLA_KV_RANK, MLA_HEADS * MLA_NOPE_DIM), MLA_KV_RANK ** -0.5),
        'mla_w_uv': nrm(ks[11], (DEPTH, MLA_KV_RANK, MLA_HEADS * MLA_V_DIM), MLA_KV_RANK ** -0.5),
        'w_out': nrm(ks[12], (DEPTH, MIX_WIDTH, Dm), BETA * MIX_WIDTH ** -0.5),
        'ln1_g': 1.0 + nrm(ks[13], (DEPTH, Dm), 0.02),
        'ln1_b': nrm(ks[14], (DEPTH, Dm), 0.02),
        'ln2_g': 1.0 + nrm(ks[15], (DEPTH, Dm), 0.02),
        'ln2_b': nrm(ks[16], (DEPTH, Dm), 0.02),
        'moe_w_group': nrm(ks[17], (DEPTH, Dm, N_GROUPS), Dm ** -0.5),
        'moe_b_group': nrm(ks[18], (DEPTH, N_GROUPS), 0.01),
        'moe_w_expert': nrm(ks[19], (DEPTH, Dm, N_EXPERTS), Dm ** -0.5),
        'moe_b_expert': nrm(ks[20], (DEPTH, N_EXPERTS), 0.01),
        'moe_w_gate': nrm(ks[21], (DEPTH, N_EXPERTS, Dm, EXPERT_HIDDEN), Dm ** -0.5),
        'moe_w_up': nrm(ks[22], (DEPTH, N_EXPERTS, Dm, EXPERT_HIDDEN), Dm ** -0.5),
        'moe_w_down': nrm(ks[23], (DEPTH, N_EXPERTS, EXPERT_HIDDEN, Dm), BETA * EXPERT_HIDDEN ** -0.5),
    }


def reference(x, c, ctx, c_ctx, w_ada, b_ada, w_in, swa_sink, ret_decay, mla_kv_norm, mla_w_uk, mla_w_uv,
              w_out, ln1_g, ln1_b, ln2_g, ln2_b, moe_w_group, moe_b_group, moe_w_expert, moe_b_expert,
              moe_w_gate, moe_w_up, moe_w_down):
    B, L, D = x.shape
    n_rows = L // GRID_W
    rows = jnp.repeat(jnp.arange(n_rows, dtype=jnp.float32), GRID_W)
    cols = jnp.tile(jnp.arange(GRID_W, dtype=jnp.float32), n_rows)
    for l in range(DEPTH):
        ctx_out = l < DEPTH - 1
        mod = (jax.nn.silu(c) @ w_ada[l] + b_ada[l]).reshape(B, 6, D)
        mod_c = (jax.nn.silu(c_ctx) @ w_ada[l] + b_ada[l]).reshape(6, D)
        h = modulate(x, mod[:, 0, None], mod[:, 1, None])
        hc = modulate(ctx, mod_c[0], mod_c[1])
        y, y_c = token_mixer(h, hc, w_in[l], w_out[l], swa_sink[l], ret_decay[l], mla_kv_norm[l],
                             mla_w_uk[l], mla_w_uv[l], rows, cols, ctx_out)
        x = affine_layer_norm(ALPHA * x + mod[:, 2, None] * y, ln1_g[l], ln1_b[l])
        h = modulate(x, mod[:, 3, None], mod[:, 4, None]).reshape(B * L, D)
        if ctx_out:
            ctx = affine_layer_norm(ALPHA * ctx + mod_c[2] * y_c, ln1_g[l], ln1_b[l])
            hc = modulate(ctx, mod_c[3], mod_c[4]).reshape(-1, D)
            tokens = jnp.concatenate([h, hc], axis=0)
        else:
            tokens = h
        f = hier_moe(tokens, moe_w_group[l], moe_b_group[l], moe_w_expert[l], moe_b_expert[l],
                     moe_w_gate[l], moe_w_up[l], moe_w_down[l])
        x = affine_layer_norm(ALPHA * x + mod[:, 5, None] * f[:B * L].reshape(B, L, D), ln2_g[l], ln2_b[l])
        if ctx_out:
            ctx = affine_layer_norm(ALPHA * ctx + mod_c[5] * f[B * L:].reshape(B, -1, D), ln2_g[l], ln2_b[l])
    return x
```

```python
import math
from contextlib import ExitStack
import numpy as np
import ml_dtypes
import concourse.bass as bass
import concourse.mybir as mybir
from concourse.bass_utils import run_bass_kernel_spmd

F32 = mybir.dt.float32
BF16 = mybir.dt.bfloat16
I32 = mybir.dt.int32
AF = mybir.ActivationFunctionType
ALU = mybir.AluOpType
AX = mybir.AxisListType

D = 2048
NCTX = 256
DEPTH = 2
IN_W = 5440
ALPHA = (2 * DEPTH) ** 0.25
EPS = 1e-6
NE = 32
EH = 1024
COMPUTE = ("pe", "act", "dve", "pool")


class Prog:
    def __init__(self, nc, stack, n_dma_sems=12):
        self.nc = nc
        self.eng = {"pe": nc.tensor, "act": nc.scalar, "dve": nc.vector,
                    "pool": nc.gpsimd, "sp": nc.sync}
        self.ops = []
        self.nd = n_dma_sems
        self.csem = {e: stack.enter_context(nc.semaphore("cs_" + e)) for e in COMPUTE}
        self.ccount = {e: 0 for e in COMPUTE}
        self.sem_obj = {("c", e): self.csem[e] for e in COMPUTE}
        self.dstate = {}
        for e in ("sp", "act", "pool"):
            sems = [stack.enter_context(nc.semaphore("ds_%s_%d" % (e, k))) for k in range(n_dma_sems)]
            for k, s in enumerate(sems):
                self.sem_obj[("d", e, k)] = s
            self.dstate[e] = dict(next=0, cnt=[0] * n_dma_sems)
        self.waited = {}
        self.carry = {}
        self.pend = {e: {} for e in self.eng}
        self.n_inst = 0

    def op(self, eng, fn, reads=(), writes=(), dma=False):
        self.ops.append((eng, fn, tuple(reads), tuple(writes), dma))

    def dma(self, eng, out, in_, reads=(), writes=(), **kw):
        self.op(eng, lambda e: e.dma_start(out=out, in_=in_, **kw), reads, writes, dma=True)

    def _wait(self, eng, sk, val):
        key = (eng, sk)
        if self.waited.get(key, 0) >= val:
            return
        self.waited[key] = val
        self.eng[eng].wait_ge(self.sem_obj[sk], val)
        self.n_inst += 1

    def flush(self):
        ops = self.ops
        self.ops = []
        n = len(ops)
        last_w, readers = {}, {}
        deps = [None] * n
        last_on_eng = {}
        dma_ops = []
        for i, (eng, fn, reads, writes, dma) in enumerate(ops):
            d = set()
            for k in reads:
                if k in last_w:
                    d.add(last_w[k])
            for k in writes:
                if k in last_w:
                    d.add(last_w[k])
                r = readers.get(k)
                if r:
                    d.update(r[0].values())
                    d.update(r[1])
            d.discard(i)
            deps[i] = d
            for k in reads:
                r = readers.setdefault(k, ({}, []))
                if dma:
                    r[1].append(i)
                else:
                    r[0][eng] = i
            for k in writes:
                last_w[k] = i
                readers[k] = ({}, [])
            if dma:
                dma_ops.append(i)
            else:
                last_on_eng[eng] = i
        signal = [False] * n
        for i, (eng, fn, reads, writes, dma) in enumerate(ops):
            keep = set()
            for j in deps[i]:
                ej, _, rj, wj, dj = ops[j]
                if (not dj) and ej == eng and not dma:
                    if eng == "pe":
                        continue
                    if not (set(wj) & set(reads)):
                        continue
                keep.add(j)
            deps[i] = keep
            for j in keep:
                signal[j] = True
        for e, j in last_on_eng.items():
            signal[j] = True
        event = [None] * n
        for i, (eng, fn, reads, writes, dma) in enumerate(ops):
            need = {}
            if self.pend[eng]:
                need.update(self.pend[eng])
                self.pend[eng] = {}
            for j in deps[i]:
                sk, val = event[j]
                if need.get(sk, 0) < val:
                    need[sk] = val
            for sk, val in need.items():
                self._wait(eng, sk, val)
            if dma:
                st = self.dstate[eng]
                k = st["next"]
                st["next"] = (k + 1) % self.nd
                sk = ("d", eng, k)
                if st["cnt"][k] > 0:
                    self._wait(eng, sk, st["cnt"][k])
                st["cnt"][k] += 16
                ins = fn(self.eng[eng])
                ins.then_inc(self.sem_obj[sk], 16)
                event[i] = (sk, st["cnt"][k])
            else:
                ins = fn(self.eng[eng])
                if signal[i]:
                    self.ccount[eng] += 1
                    ins.then_inc(self.csem[eng], 1)
                    event[i] = (("c", eng), self.ccount[eng])
            self.n_inst += 1
        carry = {}
        for e in COMPUTE:
            if self.ccount[e] > 0:
                carry[("c", e)] = self.ccount[e]
        for e, st in self.dstate.items():
            for k in range(self.nd):
                if st["cnt"][k] > 0:
                    carry[("d", e, k)] = st["cnt"][k]
        for e in self.eng:
            self.pend[e] = dict(carry)

    def finish(self):
        self.flush()
        for sk, val in self.pend["sp"].items():
            self._wait("sp", sk, val)


def V(t, off, *dims):
    F = 1
    for s in t.shape[1:]:
        F *= s
    npart = dims[0]
    return bass.AP(t, off, [[F, npart]] + [list(d) for d in dims[1:]])


CHUNKS = [("sq0", 0, 512), ("sq1", 512, 256), ("sk", 768, 256), ("sv", 1024, 256),
          ("rq", 1280, 384), ("rk", 1664, 384), ("rv0", 2048, 512), ("rv1", 2560, 256),
          ("gf0", 2816, 512), ("gf1", 3328, 256), ("gb0", 3584, 512), ("gb1", 4096, 256),
          ("mq0", 4352, 384), ("mq1", 4736, 384), ("ckv", 5120, 320)]


def build(L, dbg=()):
    nc = bass.Bass("TRN2", target_bir_lowering=False)
    NT = (NCTX + L) // 128
    T = NT * 128
    NLT = L // 128

    in_names = []
    nc._in_names = in_names

    def din(name, shape, dt=F32):
        in_names.append(name)
        return nc.dram_tensor(name, list(shape), dt, kind="ExternalInput").ap()

    def dscr(name, shape, dt=F32):
        kind = "ExternalOutput" if name in dbg else "Internal"
        return nc.dram_tensor(name, list(shape), dt, kind=kind).ap()

    x_in = din("x", [L, D])
    ctx_in = din("ctx", [NCTX, D])
    c2_in = din("c2", [2, D])
    w_ada = din("w_ada", [DEPTH, D, 6 * D])
    b_ada = din("b_ada", [DEPTH, 6 * D])
    w_in = din("w_in", [DEPTH, D, IN_W])
    swa_sink = din("swa_sink", [DEPTH, 6])
    ret_decay = din("ret_decay", [DEPTH, 2, 6])
    kv_norm = din("mla_kv_norm", [DEPTH, 256])
    w_uk = din("mla_w_uk", [DEPTH, 256, 512])
    w_uv = din("mla_w_uv", [DEPTH, 256, 512])
    w_out = din("w_out", [DEPTH, D, D])
    ln_g = [din("ln1_g", [DEPTH, D]), din("ln2_g", [DEPTH, D])]
    ln_b = [din("ln1_b", [DEPTH, D]), din("ln2_b", [DEPTH, D])]
    w_grp = din("moe_w_group", [DEPTH, D, 4])
    b_grp = din("moe_b_group", [DEPTH, 4])
    w_exp = din("moe_w_expert", [DEPTH, D, NE])
    b_exp = din("moe_b_expert", [DEPTH, NE])
    if not (set(dbg) & {"P2", "P5", "P6"}):
        w_gate = din("moe_w_gate", [DEPTH, NE, D, EH])
        w_up = din("moe_w_up", [DEPTH, NE, D, EH])
        w_down = din("moe_w_down", [DEPTH, NE, EH, D])
    ident_in = din("ident", [128, 128])
    rtab_in = din("rtab", [T, 512])
    out_d = nc.dram_tensor("out", [L, D], F32, kind="ExternalOutput").ap()

    xres = dscr("xres", [T, D])
    gsc = dscr("gsc", [DEPTH, 4, 2, D])
    qTs = dscr("qTs", [6, 128, T], BF16)
    kTs = dscr("kTs", [2, 128, T], BF16)
    vs = dscr("vs", [T, 256], BF16)
    qTr = dscr("qTr", [3, 128, T], BF16)
    kTr = dscr("kTr", [3, 128, T], BF16)
    kr = dscr("kr", [T, 384], BF16)
    vr = dscr("vr", [T, 768], BF16)
    gfb = dscr("gfb", [2, T, 768])
    qnT = dscr("qnT", [4, 128, T], BF16)
    qrT = dscr("qrT", [4, 64, T], BF16)
    knT = dscr("knT", [4, 128, T], BF16)
    krT = dscr("krT", [64, T], BF16)
    vm = dscr("vm", [T, 512], BF16)
    catT = dscr("catT", [16, 128, T], BF16)

    with ExitStack() as st:
        p = Prog(nc, st)
        _uid = [0]

        def SB(name, shape, dt=F32, s=st):
            _uid[0] += 1
            return s.enter_context(nc.sbuf_tensor("s%d_%s" % (_uid[0], name), list(shape), dt))
        ps = [st.enter_context(nc.psum_tensor("ps%d" % i, [128, 512], F32)) for i in range(8)]
        psk = ["ps%d" % i for i in range(8)]
        psb = [b[:].bitcast(BF16) for b in ps]

        ident = SB("ident", [128, 128])
        identb = SB("identb", [128, 128], BF16)
        modcol = SB("modcol", [128, 96, 2])
        scT = SB("scT", [128, 16, 2], BF16)
        p.dma("sp", ident[:], ident_in, writes=["ident"])
        p.op("dve", lambda e: e.tensor_copy(identb[:], ident[:]), ["ident"], ["identb"])

        with ExitStack() as ph:
            c2 = SB("c2", [2, D], F32, ph)
            p.dma("sp", c2[:], c2_in, writes=["c2"])
            p.op("act", lambda e: e.activation(c2[:], c2[:], AF.Silu), ["c2"], ["c2"])
            for kc in range(16):
                p.op("pe", lambda e, kc=kc: e.transpose(ps[0][:, kc * 2:kc * 2 + 2], c2[:, kc * 128:(kc + 1) * 128],
                                                        ident[0:2, 0:2]), ["c2", "ident"], [psk[0]])
            p.op("dve", lambda e: e.tensor_copy(scT[:].rearrange("p a b -> p (a b)"), ps[0][:, 0:32]), [psk[0]], ["scT"])
            p.flush()

        cst_in = din("cst", [128, 6, 128])
        scol_in = din("scol", [128, 2])
        cst = SB("cst", [128, 6, 128])
        scol = SB("scol", [128, 2])
        mle = SB("mle", [128, 128], BF16)
        mge = SB("mge", [128, 128], BF16)
        onesb = SB("onesb", [128, 128], BF16)
        onesf = SB("onesf", [128, 128])
        p.dma("sp", cst[:], cst_in, writes=["cst"])
        p.dma("sp", scol[:], scol_in, writes=["scol"])
        p.op("dve", lambda e: e.tensor_copy(mle[:], cst[:, 0, :]), ["cst"], ["mle"])
        p.op("dve", lambda e: e.tensor_copy(mge[:], cst[:, 1, :]), ["cst"], ["mge"])
        p.op("dve", lambda e: e.memset(onesb[:], 1.0), [], ["onesb"])
        p.op("dve", lambda e: e.memset(onesf[:], 1.0), [], ["onesf"])
        p.flush()

        def blk_src(l, t):
            if l == 0:
                return ctx_in[t * 128:(t + 1) * 128, :] if t < 2 else x_in[(t - 2) * 128:(t - 1) * 128, :]
            return xres[t * 128:(t + 1) * 128, :]

        cnt = [0]

        def ln_stats(xt, xk, stt, mv, rstd, nmr, pre):
            for c4 in range(4):
                p.op("dve", lambda e, c4=c4: e.bn_stats(stt[:, c4, :], xt[:, c4 * 512:(c4 + 1) * 512]), [xk], [pre + "st"])
            p.op("dve", lambda e: e.bn_aggr(mv[:], stt[:].rearrange("p a b -> p (a b)")), [pre + "st"], [pre + "mv"])
            p.op("dve", lambda e: e.tensor_scalar(rstd[:], mv[:, 1:2], EPS, None, ALU.add), [pre + "mv"], [pre + "rstd"])
            p.op("act", lambda e: e.activation(rstd[:], rstd[:], AF.Sqrt), [pre + "rstd"], [pre + "rstd"])
            p.op("dve", lambda e: e.reciprocal(rstd[:], rstd[:]), [pre + "rstd"], [pre + "rstd"])
            p.op("dve", lambda e: e.scalar_tensor_tensor(nmr[:], mv[:, 0:1], -1.0, rstd[:], ALU.mult, ALU.mult),
                 [pre + "mv", pre + "rstd"], [pre + "nmr"])

        def ln_mod_T(src_ap, m, jsh, jsc, hT, hk, slot, B):
            i = cnt[0]
            cnt[0] += 1
            xt, xk = B["xt"][i % 2], "xt%d" % (i % 2)
            p.dma("sp", xt[:], src_ap, writes=[xk])
            ln_stats(xt, xk, B["stt"], B["mv"], B["rstd"], B["nmr"], "l")
            xn = B["xn"]
            p.op("act", lambda e: e.activation(xn[:], xt[:], AF.Identity, bias=B["nmr"][:], scale=B["rstd"][:]),
                 [xk, "lnmr", "lrstd"], ["xn"])
            for q in range(4):
                bk = 4 + q
                for k4 in range(4):
                    kc = q * 4 + k4
                    p.op("pe", lambda e, kc=kc, k4=k4, bk=bk: e.transpose(
                        ps[bk][:, k4 * 128:(k4 + 1) * 128], xn[:, kc * 128:(kc + 1) * 128], ident[:]),
                        ["xn", "ident"], [psk[bk]])
                for k4 in range(4):
                    kc = q * 4 + k4
                    p.op("act", lambda e, kc=kc, k4=k4, bk=bk: e.activation(
                        hT[:, kc, slot * 128:(slot + 1) * 128], ps[bk][:, k4 * 128:(k4 + 1) * 128], AF.Identity,
                        bias=modcol[:, jsh * 16 + kc, m:m + 1], scale=modcol[:, jsc * 16 + kc, m:m + 1]),
                        [psk[bk], "modcol"], [hk])

        def rope(rs, W, H, d, hb, hstride, hoff, tab, tk, cb, sb, r1, r2, rb):
            nb2 = d // (2 * hb)
            full = lambda t, o=0: V(t, hoff + o, 128, [hstride, H], [2 * hb, nb2], [1, hb])
            p.op("dve", lambda e: e.tensor_tensor(
                V(r1, hoff, 128, [hstride, H], [1, d]), V(rs, hoff, 128, [hstride, H], [1, d]),
                V(tab, cb, 128, [0, H], [1, d]), ALU.mult), ["rs", tk], ["r1"])
            for b in range(2):
                p.op("pool", lambda e, b=b: e.tensor_tensor(
                    full(r2, b * hb), full(rs, (1 - b) * hb),
                    V(tab, sb + b * hb, 128, [0, H], [2 * hb, nb2], [1, hb]), ALU.mult), ["rs", tk], ["r2"])
            p.op("dve", lambda e: e.tensor_tensor(
                V(rb, hoff, 128, [hstride, H], [1, d]), V(r1, hoff, 128, [hstride, H], [1, d]),
                V(r2, hoff, 128, [hstride, H], [1, d]), ALU.add), ["r1", "r2"], ["rb"])

        for l in range(DEPTH):
            with ExitStack() as ph:
                wa = [SB("wa%d" % i, [128, 16, 1024], BF16, ph) for i in range(2)]
                bar = SB("bar", [96, 128], F32, ph)
                bcol = SB("bcol", [128, 96], F32, ph)
                rows2 = SB("rows2", [2, 4, D], F32, ph)
                p.dma("sp", bar[:], b_ada[l].rearrange("(a b) -> a b", b=128), writes=["bar"])
                p.op("pe", lambda e: e.transpose(ps[1][:, 0:96], bar[:], ident[0:96, 0:96]), ["bar", "ident"], [psk[1]])
                p.op("dve", lambda e: e.tensor_copy(bcol[:], ps[1][:, 0:96]), [psk[1]], ["bcol"])
                wav = w_ada[l].rearrange("(kc p) n -> p kc n", p=128)
                for g in range(12):
                    w = wa[g % 2]
                    wk = "wa%d" % (g % 2)
                    p.dma("pool", w[:], wav[:, :, g * 1024:(g + 1) * 1024], writes=[wk])
                    for nn in range(8):
                        n = g * 8 + nn
                        for kc in range(16):
                            p.op("pe", lambda e, w=w, nn=nn, n=n, kc=kc: e.matmul(
                                ps[0][:, 2 * n:2 * n + 2], w[:, kc, nn * 128:(nn + 1) * 128], scT[:, kc, :],
                                start=(kc == 0), stop=(kc == 15)), [wk, "scT"], [psk[0]])
                p.op("dve", lambda e: e.tensor_tensor(
                    modcol[:], ps[0][:, 0:192].rearrange("p (a b) -> p a b", b=2),
                    V(bcol, 0, 128, [1, 96], [0, 2]), ALU.add), [psk[0], "bcol"], ["modcol"])
                for j in (1, 4):
                    p.op("dve", lambda e, j=j: e.tensor_scalar_add(modcol[:, j * 16:(j + 1) * 16, :],
                                                                   modcol[:, j * 16:(j + 1) * 16, :], 1.0),
                         ["modcol"], ["modcol"])
                for si, j in enumerate((2, 5, 3, 4)):
                    for kc in range(16):
                        bk = 2 + kc // 4
                        p.op("pe", lambda e, j=j, kc=kc, bk=bk: e.transpose(
                            ps[bk][0:2, (kc % 4) * 128:(kc % 4 + 1) * 128], modcol[:, j * 16 + kc, :], ident[:]),
                            ["modcol", "ident"], [psk[bk]])
                    for q in range(4):
                        p.op("dve", lambda e, si=si, q=q: e.tensor_copy(rows2[:, si, q * 512:(q + 1) * 512], ps[2 + q][0:2, :]),
                             [psk[2 + q]], ["rows2"])
                p.dma("sp", gsc[l].rearrange("s m d -> m s d"), rows2[:], reads=["rows2"], writes=[])
                p.flush()

            with ExitStack() as ph:
                B = dict(xt=[SB("xt%d" % i, [128, D], F32, ph) for i in range(2)], xn=SB("xn", [128, D], F32, ph),
                         stt=SB("stt", [128, 4, 6], F32, ph), mv=SB("mv", [128, 2], F32, ph),
                         rstd=SB("rstd", [128, 1], F32, ph), nmr=SB("nmr", [128, 1], F32, ph))
                hT = SB("hT", [128, 16, 512], BF16, ph)
                wch = [SB("wch%d" % i, [128, 16, 512], BF16, ph) for i in range(2)]
                rt = [SB("rt%d" % i, [128, 512], F32, ph) for i in range(4)]
                rs = SB("rs", [128, 768], F32, ph)
                r1 = SB("r1", [128, 768], F32, ph)
                r2 = SB("r2", [128, 768], F32, ph)
                rb = SB("rb", [128, 768], BF16, ph)
                tT = SB("tT", [128, 6, 128], BF16, ph)
                gst = SB("gst", [128, 512], F32, ph)
                gkv = SB("gkv", [128, 256], F32, ph)
                wuk = SB("wuk", [128, 2, 512], BF16, ph)
                wuv = SB("wuv", [128, 2, 512], BF16, ph)
                cnT = SB("cnT", [128, 2, 128], BF16, ph)
                ssq = SB("ssq", [128, 1], F32, ph)
                p.dma("sp", gkv[:], kv_norm[l:l + 1, :].to_broadcast([128, 256]), writes=["gkv"])
                p.dma("pool", wuk[:], w_uk[l].rearrange("(rc p) n -> p rc n", p=128), writes=["wuk"])
                p.dma("pool", wuv[:], w_uv[l].rearrange("(rc p) n -> p rc n", p=128), writes=["wuv"])
                wiv = w_in[l].rearrange("(kc p) n -> p kc n", p=128)
                wi = 0
                mmi = 0
                for g0 in range(0, NT, 4):
                    tblks = list(range(g0, min(g0 + 4, NT)))
                    for s_, t in enumerate(tblks):
                        ln_mod_T(blk_src(l, t), 1 if t < 2 else 0, 0, 1, hT, "hT", s_, B)
                        p.dma("sp", rt[s_][:], rtab_in[t * 128:(t + 1) * 128, :], writes=["rt%d" % s_])
                    for (cname, c0, cw) in CHUNKS:
                        w = wch[wi % 2]
                        wk = "wch%d" % (wi % 2)
                        wi += 1
                        p.dma("pool", w[:, :, 0:cw], wiv[:, :, c0:c0 + cw], writes=[wk])
                        for s_, t in enumerate(tblks):
                            tok = slice(t * 128, (t + 1) * 128)
                            bk = mmi % 4
                            mmi += 1
                            pk = psk[bk]
                            pt = ps[bk]
                            tab, tk = rt[s_], "rt%d" % s_
                            for kc in range(16):
                                p.op("pe", lambda e, kc=kc, s_=s_, w=w, pt=pt, cw=cw: e.matmul(
                                    pt[:, 0:cw], hT[:, kc, s_ * 128:(s_ + 1) * 128], w[:, kc, 0:cw],
                                    start=(kc == 0), stop=(kc == 15)), ["hT", wk], [pk])

                            def transp(nblk, width, dst_ap, srcoff=lambda b: b * 128):
                                for b in range(nblk):
                                    p.op("pe", lambda e, b=b: e.transpose(
                                        psb[7][0:width, b * 128:(b + 1) * 128], rb[:, srcoff(b):srcoff(b) + width], identb[:]),
                                        ["rb", "identb"], [psk[7]])
                                p.op("act", lambda e: e.copy(tT[0:width, 0:nblk, :],
                                                             psb[7][0:width, 0:nblk * 128].rearrange("p (a b) -> p a b", b=128)),
                                     [psk[7]], ["tT"])
                                p.dma("sp", dst_ap, tT[0:width, 0:nblk, :], reads=["tT"], writes=[])

                            if cname in ("sq0", "sq1", "sk"):
                                H = cw // 128
                                sc = 128 ** -0.5 if cname != "sk" else 1.0
                                p.op("act", lambda e, pt=pt, cw=cw, sc=sc: e.mul(rs[:, 0:cw], pt[:, 0:cw], sc), [pk], ["rs"])
                                rope(rs, cw, H, 128, 32, 128, 0, tab, tk, 0, 128, r1, r2, rb)
                                if cname == "sk":
                                    dst = kTs[:, :, tok]
                                else:
                                    h0 = 0 if cname == "sq0" else 4
                                    dst = qTs[h0:h0 + H, :, tok]
                                transp(H, 128, dst.rearrange("h p t -> p h t"))
                            elif cname in ("sv", "rv0", "rv1"):
                                p.op("act", lambda e, pt=pt, cw=cw: e.copy(rb[:, 0:cw], pt[:, 0:cw]), [pk], ["rb"])
                                if cname == "sv":
                                    dst = vs[tok, :]
                                else:
                                    o = 0 if cname == "rv0" else 512
                                    dst = vr[tok, o:o + cw]
                                p.dma("sp", dst, rb[:, 0:cw], reads=["rb"], writes=[])
                            elif cname in ("gf0", "gf1", "gb0", "gb1"):
                                p.op("act", lambda e, pt=pt, cw=cw: e.activation(gst[:, 0:cw], pt[:, 0:cw], AF.Silu), [pk], ["gst"])
                                o = 0 if cname[2] == "0" else 512
                                p.dma("sp", gfb[0 if cname[1] == "f" else 1, tok, o:o + cw], gst[:, 0:cw], reads=["gst"], writes=[])
                            elif cname in ("rq", "rk"):
                                sc = 64 ** -0.5 if cname == "rq" else 1.0
                                p.op("act", lambda e, pt=pt, sc=sc: e.mul(rs[:, 0:384], pt[:, 0:384], sc), [pk], ["rs"])
                                rope(rs, 384, 6, 64, 32, 64, 0, tab, tk, 256, 320, r1, r2, rb)
                                if cname == "rk":
                                    p.dma("sp", kr[tok, :], rb[:, 0:384], reads=["rb"], writes=[])
                                dst = (qTr if cname == "rq" else kTr)[:, :, tok]
                                transp(3, 128, dst.rearrange("h p t -> p h t"))
                            elif cname in ("mq0", "mq1"):
                                sc = 192 ** -0.5
                                p.op("act", lambda e, pt=pt, sc=sc: e.mul(rs[:, 0:384], pt[:, 0:384], sc), [pk], ["rs"])
                                p.op("dve", lambda e: e.tensor_copy(rb[:, 0:384], rs[:, 0:384]), ["rs"], ["rb"])
                                rope(rs, 384, 2, 64, 16, 192, 128, tab, tk, 384, 448, r1, r2, rb)
                                h0 = 0 if cname == "mq0" else 2
                                transp(2, 128, qnT[h0:h0 + 2, :, tok].rearrange("h p t -> p h t"), srcoff=lambda b: b * 192)
                                transp(2, 64, qrT[h0:h0 + 2, :, tok].rearrange("h p t -> p h t"), srcoff=lambda b: b * 192 + 128)
                            else:
                                p.op("act", lambda e, pt=pt: e.copy(rs[:, 0:320], pt[:, 0:320]), [pk], ["rs"])
                                rope(rs, 64, 1, 64, 16, 64, 256, tab, tk, 384, 448, r1, r2, rb)
                                transp(1, 64, krT[:, tok].rearrange("p (a t) -> p a t", a=1), srcoff=lambda b: 256)
                                p.op("dve", lambda e: e.tensor_tensor(r1[:, 0:256], rs[:, 0:256], rs[:, 0:256], ALU.mult), ["rs"], ["r1"])
                                p.op("dve", lambda e: e.reduce_sum(ssq[:], r1[:, 0:256], axis=AX.X), ["r1"], ["ssq"])
                                p.op("dve", lambda e: e.tensor_scalar(ssq[:], ssq[:], 1.0 / 256, EPS, ALU.mult, ALU.add), ["ssq"], ["ssq"])
                                p.op("act", lambda e: e.activation(ssq[:], ssq[:], AF.Sqrt), ["ssq"], ["ssq"])
                                p.op("dve", lambda e: e.reciprocal(ssq[:], ssq[:]), ["ssq"], ["ssq"])
                                p.op("dve", lambda e: e.scalar_tensor_tensor(rb[:, 0:256], rs[:, 0:256], ssq[:, 0:1], gkv[:],
                                                                             ALU.mult, ALU.mult), ["rs", "ssq", "gkv"], ["rb"])
                                for b in range(2):
                                    p.op("pe", lambda e, b=b: e.transpose(psb[7][:, b * 128:(b + 1) * 128], rb[:, b * 128:(b + 1) * 128], identb[:]),
                                         ["rb", "identb"], [psk[7]])
                                p.op("act", lambda e: e.copy(cnT[:].rearrange("p a b -> p (a b)"), psb[7][:, 0:256]), [psk[7]], ["cnT"])
                                bk2 = mmi % 4
                                mmi += 1
                                for h in range(4):
                                    for rc in range(2):
                                        p.op("pe", lambda e, h=h, rc=rc, bk2=bk2: e.matmul(
                                            ps[bk2][:, h * 128:(h + 1) * 128], wuk[:, rc, h * 128:(h + 1) * 128], cnT[:, rc, :],
                                            start=(rc == 0), stop=(rc == 1)), ["wuk", "cnT"], [psk[bk2]])
                                p.op("act", lambda e, bk2=bk2: e.copy(tT[:, 0:4, :].rearrange("p a b -> p (a b)"), ps[bk2][:, 0:512]), [psk[bk2]], ["tT"])
                                p.dma("sp", knT[:, :, tok].rearrange("h p t -> p h t"), tT[:, 0:4, :], reads=["tT"], writes=[])
                                bk3 = mmi % 4
                                mmi += 1
                                for rc in range(2):
                                    p.op("pe", lambda e, rc=rc, bk3=bk3: e.matmul(
                                        ps[bk3][:, 0:512], cnT[:, rc, :], wuv[:, rc, :], start=(rc == 0), stop=(rc == 1)),
                                        ["wuv", "cnT"], [psk[bk3]])
                                p.op("act", lambda e, bk3=bk3: e.copy(rb[:, 0:512], ps[bk3][:, 0:512]), [psk[bk3]], ["rb"])
                                p.dma("sp", vm[tok, :], rb[:, 0:512], reads=["rb"], writes=[])
                p.flush()
            if "P2" in dbg:
                break
            LN2 = math.log(2.0)
            import os
            SKIP = os.environ.get("KSKIP", "").split(",")
            with ExitStack() as ph:
              if "P3" not in SKIP:
                  kT = SB("kT_s", [128, T], BF16, ph)
                  vv = SB("v_s", [128, NT, 128], BF16, ph)
                  q3 = [SB("q3_%d" % i, [128, 3, 128], BF16, ph) for i in range(2)]
                  PT = [SB("PT%d" % i, [128, 384], BF16, ph) for i in range(2)]
                  sink6 = SB("sink6", [1, 6], F32, ph)
                  esrow = SB("esrow", [1, 768], BF16, ph)
                  rec = SB("rec", [128, 384], F32, ph)
                  oT = SB("oT", [128, 384], BF16, ph)
                  p.dma("sp", sink6[:], swa_sink[l:l + 1, :], writes=["sink6"])
                  p.op("act", lambda e: e.activation(sink6[:], sink6[:], AF.Exp), ["sink6"], ["sink6"])
                  for h in range(6):
                      p.op("dve", lambda e, h=h: e.tensor_scalar(esrow[0:1, h * 128:(h + 1) * 128], onesf[0:1, 0:128],
                                                                 sink6[0:1, h:h + 1], None, ALU.mult), ["sink6", "onesf"], ["esrow"])
                  qi = 0
                  pi = 0
                  for hk in range(2):
                      p.dma("sp", kT[:], kTs[hk], writes=["kT"])
                      p.dma("sp", vv[:], vs[:, hk * 128:(hk + 1) * 128].rearrange("(n p) d -> p n d", p=128), writes=["vv"])
                      for t in range(NT):
                          keys = [(0, None), (1, None)]
                          if t >= 2:
                              n = t - 2
                              if n > 0:
                                  keys.append((t - 1, mge))
                              keys.append((t, None))
                              if n < NLT - 1:
                                  keys.append((t + 1, mle))
                          q = q3[qi % 2]
                          qk = "q3_%d" % (qi % 2)
                          dn, nm = (2, 3) if qi % 2 == 0 else (4, 5)
                          qi += 1
                          tok = slice(t * 128, (t + 1) * 128)
                          p.dma("sp", q[:], qTs[hk * 3:(hk + 1) * 3, :, tok].rearrange("h p t -> p h t"), writes=[qk])
                          for ki, (kt, mk) in enumerate(keys):
                              sb_ = pi % 2
                              P_ = PT[pi % 2]
                              Pk = "PT%d" % (pi % 2)
                              pi += 1
                              p.op("pe", lambda e, sb_=sb_, kt=kt, q=q: e.matmul(
                                  ps[sb_][:, 0:384], kT[:, kt * 128:(kt + 1) * 128], q[:].rearrange("p a b -> p (a b)"),
                                  start=True, stop=True), ["kT", qk], [psk[sb_]])
                              p.op("act", lambda e, sb_=sb_, P_=P_: e.activation(P_[:], ps[sb_][:, 0:384], AF.Exp), [psk[sb_]], [Pk])
                              if mk is not None:
                                  p.op("dve", lambda e, P_=P_, mk=mk: e.tensor_tensor(
                                      V(P_, 0, 128, [128, 3], [1, 128]), V(P_, 0, 128, [128, 3], [1, 128]),
                                      V(mk, 0, 128, [0, 3], [1, 128]), ALU.mult), [Pk, "mle", "mge"], [Pk])
                              p.op("pe", lambda e, P_=P_, ki=ki, dn=dn: e.matmul(
                                  ps[dn][:, 0:384], onesb[:], P_[:], start=(ki == 0), stop=False), [Pk, "onesb"], [psk[dn]])
                              p.op("pe", lambda e, P_=P_, ki=ki, nm=nm, kt=kt, last=(ki == len(keys) - 1): e.matmul(
                                  ps[nm][:, 0:384], vv[:, kt, :], P_[:], start=(ki == 0), stop=last), [Pk, "vv"], [psk[nm]])
                          p.op("pe", lambda e, dn=dn, hk=hk: e.matmul(
                              ps[dn][:, 0:384], onesb[0:1, :], esrow[0:1, hk * 384:(hk + 1) * 384], start=False, stop=True),
                              ["esrow", "onesb"], [psk[dn]])
                          p.op("dve", lambda e, dn=dn: e.reciprocal(rec[:], ps[dn][:, 0:384]), [psk[dn]], ["rec"])
                          p.op("dve", lambda e, nm=nm: e.tensor_tensor(oT[:], ps[nm][:, 0:384], rec[:], ALU.mult), [psk[nm], "rec"], ["oT"])
                          p.dma("sp", catT[hk * 3:(hk + 1) * 3, :, tok].rearrange("h p t -> p h t"),
                                oT[:].rearrange("p (a b) -> p a b", b=128), reads=["oT"])
                  p.flush()

            with ExitStack() as ph:
              if "P5" not in SKIP:
                  knS = SB("knS", [128, T], BF16, ph)
                  krS = SB("krS", [64, T], BF16, ph)
                  vS = SB("vS", [128, NT, 128], BF16, ph)
                  qn = [SB("qn%d" % i, [128, 512], BF16, ph) for i in range(2)]
                  qr = [SB("qr%d" % i, [64, 512], BF16, ph) for i in range(2)]
                  PT = [SB("PTm%d" % i, [128, 512], BF16, ph) for i in range(2)]
                  rec = SB("recm", [128, 512], F32, ph)
                  oT = SB("oTm", [128, 512], BF16, ph)
                  p.dma("sp", krS[:], krT, writes=["krS"])
                  groups = [(0, 2, [0, 1])] + [(g0, min(4, NT - g0), list(range(NT))) for g0 in range(2, NT, 4)]
                  qi = 0
                  pi = 0
                  for h in range(4):
                      p.dma("sp", knS[:], knT[h], writes=["knS"])
                      p.dma("sp", vS[:], vm[:, h * 128:(h + 1) * 128].rearrange("(n p) d -> p n d", p=128), writes=["vS"])
                      for (g0, ng, kblks) in groups:
                          N = ng * 128
                          cols = slice(g0 * 128, g0 * 128 + N)
                          qn_, qr_ = qn[qi % 2], qr[qi % 2]
                          qk = "qm%d" % (qi % 2)
                          dn, nm = (2, 3) if qi % 2 == 0 else (4, 5)
                          qi += 1
                          p.dma("sp", qn_[:, 0:N], qnT[h][:, cols], writes=[qk])
                          p.dma("sp", qr_[:, 0:N], qrT[h][:, cols], writes=[qk])
                          for ki, kt in enumerate(kblks):
                              sb_ = pi % 2
                              P_ = PT[pi % 2]
                              Pk = "PTm%d" % (pi % 2)
                              pi += 1
                              p.op("pe", lambda e, sb_=sb_, kt=kt, qn_=qn_, N=N: e.matmul(
                                  ps[sb_][:, 0:N], knS[:, kt * 128:(kt + 1) * 128], qn_[:, 0:N], start=True, stop=False),
                                  ["knS", qk], [psk[sb_]])
                              p.op("pe", lambda e, sb_=sb_, kt=kt, qr_=qr_, N=N: e.matmul(
                                  ps[sb_][:, 0:N], krS[:, kt * 128:(kt + 1) * 128], qr_[:, 0:N], start=False, stop=True),
                                  ["krS", qk], [psk[sb_]])
                              p.op("act", lambda e, sb_=sb_, P_=P_, N=N: e.activation(P_[:, 0:N], ps[sb_][:, 0:N], AF.Exp), [psk[sb_]], [Pk])
                              last = (ki == len(kblks) - 1)
                              p.op("pe", lambda e, P_=P_, ki=ki, dn=dn, N=N, last=last: e.matmul(
                                  ps[dn][:, 0:N], onesb[:], P_[:, 0:N], start=(ki == 0), stop=last), [Pk, "onesb"], [psk[dn]])
                              p.op("pe", lambda e, P_=P_, ki=ki, nm=nm, kt=kt, N=N, last=last: e.matmul(
                                  ps[nm][:, 0:N], vS[:, kt, :], P_[:, 0:N], start=(ki == 0), stop=last), [Pk, "vS"], [psk[nm]])
                          p.op("dve", lambda e, dn=dn, N=N: e.reciprocal(rec[:, 0:N], ps[dn][:, 0:N]), [psk[dn]], ["recm"])
                          p.op("dve", lambda e, nm=nm, N=N: e.tensor_tensor(oT[:, 0:N], ps[nm][:, 0:N], rec[:, 0:N], ALU.mult),
                               [psk[nm], "recm"], ["oTm"])
                          p.dma("sp", catT[12 + h][:, cols], oT[:, 0:N], reads=["oTm"])
                  p.flush()

            with ExitStack() as ph:
              if "P4" not in SKIP:
                  lgb = SB("lgb", [128, 2, 6], F32, ph)
                  nlgb = SB("nlgb", [128, 2, 6], F32, ph)
                  lgcol = SB("lgcol", [128, 2, 3], F32, ph)
                  maskT = SB("maskT", [128, 2, 6, 128], F32, ph)
                  mtmp = SB("mtmp", [128, 128], F32, ph)
                  qdec = SB("qdec", [64, 2, 6, 128], F32, ph)
                  kdf = SB("kdf", [128, 2, 6], F32, ph)
                  gc = SB("gc", [64, 2, 6], F32, ph)
                  gCt = SB("gCt", [64, 2, 6, 128], F32, ph)
                  S = SB("S", [64, 6, 128], F32, ph)
                  Stmp = SB("Stmp", [64, 6, 128], F32, ph)
                  Sb = SB("Sb", [64, 6, 128], BF16, ph)
                  qt = [SB("qt%d" % i, [64, 6, 128], BF16, ph) for i in range(2)]
                  ktT = [SB("ktT%d" % i, [64, 6, 128], BF16, ph) for i in range(2)]
                  ktm = [SB("ktm%d" % i, [128, 384], BF16, ph) for i in range(2)]
                  vt = [SB("vt%d" % i, [128, 768], BF16, ph) for i in range(2)]
                  gt = [SB("gt%d" % i, [128, 768], F32, ph) for i in range(2)]
                  ra = [SB("ra%d" % i, [128, 768], F32, ph) for i in range(2)]
                  PTr = SB("PTr", [128, 6, 128], BF16, ph)
                  qd = SB("qd", [64, 6, 128], BF16, ph)
                  kd = SB("kd", [128, 6, 64], BF16, ph)
                  st6 = SB("st6", [128, 6, 6], F32, ph)
                  mv6 = SB("mv6", [128, 6, 2], F32, ph)
                  rs6 = SB("rs6", [128, 6], F32, ph)
                  y1 = SB("y1", [128, 6, 128], F32, ph)
                  y2 = SB("y2", [128, 6, 128], F32, ph)
                  yb = SB("yb", [128, 768], BF16, ph)
                  tT2 = SB("tT2", [128, 6, 128], BF16, ph)
                  racc = dscr("racc%d" % l, [T, 768])
                  p.dma("sp", lgb[:].rearrange("p a b -> p (a b)"),
                        ret_decay[l:l + 1].rearrange("o a b -> o (a b)").to_broadcast([128, 12]), writes=["lgb"])
                  p.op("act", lambda e: e.activation(lgb[:], lgb[:], AF.Exp, scale=-LN2), ["lgb"], ["lgb"])
                  p.op("act", lambda e: e.activation(lgb[:], lgb[:], AF.Ln, scale=-1.0, bias=1.0), ["lgb"], ["lgb"])
                  p.op("dve", lambda e: e.tensor_scalar(nlgb[:], lgb[:], -1.0, None, ALU.mult), ["lgb"], ["nlgb"])
                  for dr in range(2):
                      for j in range(3):
                          for hh in range(2):
                              p.op("dve", lambda e, dr=dr, j=j, hh=hh: e.tensor_copy(
                                  lgcol[hh * 64:(hh + 1) * 64, dr, j:j + 1], lgb[hh * 64:(hh + 1) * 64, dr, 2 * j + hh:2 * j + hh + 1]),
                                  ["lgb"], ["lgcol"])
                  for dr in range(2):
                      for h in range(6):
                          src = lgb if dr == 0 else nlgb
                          p.op("act", lambda e, dr=dr, h=h, src=src: e.activation(mtmp[:], cst[:, 2, :], AF.Exp, scale=src[:, dr, h:h + 1]),
                               ["cst", "lgb", "nlgb"], ["mtmp"])
                          p.op("dve", lambda e, dr=dr, h=h: e.tensor_tensor(maskT[:, dr, h, :], mtmp[:], cst[:, dr, :], ALU.mult),
                               ["mtmp", "cst"], ["maskT"])
                      for h in range(6):
                          p.op("act", lambda e, dr=dr, h=h: e.activation(qdec[:, dr, h, :], cst[0:64, 3 + dr, :], AF.Exp,
                                                                         scale=lgb[0:64, dr, h:h + 1]), ["cst", "lgb"], ["qdec"])
                      p.op("dve", lambda e, dr=dr: e.tensor_scalar(kdf[:, dr, :], lgb[:, dr, :], scol[:, dr:dr + 1], None, ALU.mult),
                           ["lgb", "scol"], ["kdf"])
                      p.op("act", lambda e, dr=dr: e.activation(kdf[:, dr, :], kdf[:, dr, :], AF.Exp), ["kdf"], ["kdf"])
                      p.op("act", lambda e, dr=dr: e.activation(gc[:, dr, :], lgb[0:64, dr, :], AF.Exp, scale=128.0), ["lgb"], ["gc"])
                      p.op("dve", lambda e, dr=dr: e.tensor_copy(gCt[:, dr], V(gc, dr * 6, 64, [1, 6], [0, 128])), ["gc"], ["gCt"])
                  bi = 0
                  RS = int(os.environ.get("RS", "9"))
                  for dr in range(2 if RS > 0 else 0):
                      order = list(range(NT)) if dr == 0 else [1, 0] + list(range(NT - 1, 1, -1))
                      p.op("dve", lambda e: e.memset(S[:], 0.0), [], ["S"])
                      p.op("dve", lambda e: e.memset(Sb[:], 0.0), [], ["Sb"])
                      for t in order:
                          tok = slice(t * 128, (t + 1) * 128)
                          b_ = bi % 2
                          bi += 1
                          qt_, kt_, km_, vt_, gt_, ra_ = qt[b_], ktT[b_], ktm[b_], vt[b_], gt[b_], ra[b_]
                          bk = "rin%d" % b_
                          p.dma("sp", qt_[:], qTr.rearrange("j (hh d) t -> d (j hh) t", d=64)[:, :, tok], writes=[bk + "q"])
                          p.dma("sp", kt_[:], kTr.rearrange("j (hh d) t -> d (j hh) t", d=64)[:, :, tok], writes=[bk + "k"])
                          p.dma("sp", km_[:], kr[tok, :], writes=[bk + "km"])
                          p.dma("sp", vt_[:], vr[tok, :], writes=[bk + "v"])
                          p.dma("sp", gt_[:], gfb[dr, tok, :], writes=[bk + "g"])
                          if dr == 1:
                              p.dma("sp", ra_[:], racc[tok, :], reads=["racc%d" % t], writes=[bk + "ra"])
                          psS = lambda h: ps[0][:, h * 128:(h + 1) * 128] if h < 4 else ps[1][:, (h - 4) * 128:(h - 3) * 128]
                          psY = lambda h: ps[2][:, h * 128:(h + 1) * 128] if h < 4 else ps[3][:, (h - 4) * 128:(h - 3) * 128]
                          kS = lambda h: psk[0] if h < 4 else psk[1]
                          kY = lambda h: psk[2] if h < 4 else psk[3]
                          for h in range(6):
                              j, hh = h // 2, h % 2
                              p.op("pe", lambda e, h=h, j=j, hh=hh, kt_=kt_, qt_=qt_, psS=psS: e.matmul(
                                  psS(h), kt_[:, h, :], qt_[:, h, :], start=True, stop=True),
                                  [bk + "q", bk + "k"], [kS(h)])
                          p.op("dve", lambda e, dr=dr: e.tensor_tensor(
                              PTr[:, 0:4, :], ps[0][:, 0:512].rearrange("p (a b) -> p a b", b=128), maskT[:, dr, 0:4, :], ALU.mult),
                              [psk[0], "maskT"], ["PTr"])
                          p.op("dve", lambda e, dr=dr: e.tensor_tensor(
                              PTr[:, 4:6, :], ps[1][:, 0:256].rearrange("p (a b) -> p a b", b=128), maskT[:, dr, 4:6, :], ALU.mult),
                              [psk[1], "maskT"], ["PTr"])
                          if RS < 2:
                              continue
                          p.op("pool", lambda e, dr=dr, qt_=qt_: e.tensor_tensor(qd[:], qt_[:], qdec[:, dr], ALU.mult), [bk + "q", "qdec"], ["qd"])
                          p.op("pool", lambda e, dr=dr, km_=km_: e.tensor_tensor(
                              kd[:], km_[:].rearrange("p (a b) -> p a b", b=64), V(kdf, dr * 6, 128, [1, 6], [0, 64]), ALU.mult),
                              [bk + "km", "kdf"], ["kd"])
                          for h in range(6):
                              j, hh = h // 2, h % 2
                              p.op("pe", lambda e, h=h, vt_=vt_, psY=psY: e.matmul(
                                  psY(h), PTr[:, h, :], vt_[:, h * 128:(h + 1) * 128], start=True, stop=False), ["PTr", bk + "v"], [kY(h)])
                              p.op("pe", lambda e, h=h, j=j, hh=hh, psY=psY: e.matmul(
                                  psY(h), qd[:, h, :], Sb[:, h, :],
                                  start=False, stop=True), ["qd", "Sb"], [kY(h)])
                          if RS < 3:
                              continue
                          for h in range(6):
                              ub, uo = (4, h * 128) if h < 4 else (5, (h - 4) * 128)
                              p.op("pe", lambda e, h=h, ub=ub, uo=uo, vt_=vt_: e.matmul(
                                  ps[ub][0:64, uo:uo + 128], kd[:, h, :], vt_[:, h * 128:(h + 1) * 128], start=True, stop=True), ["kd", bk + "v"], [psk[ub]])
                          p.op("dve", lambda e, dr=dr: e.tensor_tensor(Stmp[:], S[:], gCt[:, dr], ALU.mult), ["S", "gCt"], ["Stmp"])
                          p.op("dve", lambda e: e.tensor_tensor(S[:, 0:4, :], Stmp[:, 0:4, :],
                                                                ps[4][0:64, 0:512].rearrange("p (a b) -> p a b", b=128), ALU.add), ["Stmp", psk[4]], ["S"])
                          p.op("dve", lambda e: e.tensor_tensor(S[:, 4:6, :], Stmp[:, 4:6, :],
                                                                ps[5][0:64, 0:256].rearrange("p (a b) -> p a b", b=128), ALU.add), ["Stmp", psk[5]], ["S"])
                          p.op("act", lambda e: e.copy(Sb[:], S[:]), ["S"], ["Sb"])
                          if RS < 4:
                              continue
                          for h in range(6):
                              p.op("dve", lambda e, h=h, psY=psY: e.bn_stats(st6[:, h, :], psY(h)), [kY(h)], ["st6"])
                          for h in range(6):
                              p.op("dve", lambda e, h=h: e.bn_aggr(mv6[:, h, :], st6[:, h, :]), ["st6"], ["mv6"])
                          p.op("dve", lambda e: e.tensor_scalar(rs6[:], V(mv6, 1, 128, [2, 6]), EPS, None, ALU.add), ["mv6"], ["rs6"])
                          p.op("act", lambda e: e.activation(rs6[:], rs6[:], AF.Sqrt), ["rs6"], ["rs6"])
                          p.op("dve", lambda e: e.reciprocal(rs6[:], rs6[:]), ["rs6"], ["rs6"])
                          p.op("dve", lambda e: e.tensor_tensor(y1[:, 0:4, :], ps[2][:, 0:512].rearrange("p (a b) -> p a b", b=128),
                                                                V(mv6, 0, 128, [2, 4], [0, 128]), ALU.subtract), [psk[2], "mv6"], ["y1"])
                          p.op("dve", lambda e: e.tensor_tensor(y1[:, 4:6, :], ps[3][:, 0:256].rearrange("p (a b) -> p a b", b=128),
                                                                V(mv6, 8, 128, [2, 2], [0, 128]), ALU.subtract), [psk[3], "mv6"], ["y1"])
                          p.op("pool", lambda e: e.tensor_tensor(y2[:], y1[:], V(rs6, 0, 128, [1, 6], [0, 128]), ALU.mult), ["y1", "rs6"], ["y2"])
                          p.op("pool", lambda e, gt_=gt_: e.tensor_tensor(y1[:].rearrange("p a b -> p (a b)"),
                                                                          y2[:].rearrange("p a b -> p (a b)"), gt_[:], ALU.mult),
                               ["y2", bk + "g"], ["y1"])
                          if RS < 5:
                              continue
                          if dr == 0:
                              p.dma("sp", racc[tok, :], y1[:].rearrange("p a b -> p (a b)"), reads=["y1"], writes=["racc%d" % t])
                          else:
                              p.op("dve", lambda e, ra_=ra_: e.tensor_tensor(yb[:], y1[:].rearrange("p a b -> p (a b)"), ra_[:], ALU.add),
                                   ["y1", bk + "ra"], ["yb"])
                              for h in range(6):
                                  p.op("pe", lambda e, h=h: e.transpose(psb[6][:, h * 128:(h + 1) * 128], yb[:, h * 128:(h + 1) * 128], identb[:]),
                                       ["yb", "identb"], [psk[6]])
                              p.op("act", lambda e: e.copy(tT2[:].rearrange("p a b -> p (a b)"), psb[6][:, 0:768]), [psk[6]], ["tT2"])
                              p.dma("sp", catT[6:12, :, tok].rearrange("h p t -> p h t"), tT2[:], reads=["tT2"])
                  p.flush()
            if "P5" in dbg:
                break

            with ExitStack() as ph:
                wo = SB("wo", [128, 16, D], BF16, ph)
                g1 = [SB("g1_%d" % m, [128, D], F32, ph) for m in range(2)]
                lnG = SB("lnG", [128, D], F32, ph)
                lnB = SB("lnB", [128, D], F32, ph)
                cT = [SB("cT%d" % i, [128, 16, 128], BF16, ph) for i in range(2)]
                xt2 = [SB("xo%d" % i, [128, D], F32, ph) for i in range(2)]
                yg = SB("yg", [128, D], F32, ph)
                B6 = dict(stt=SB("stt6", [128, 4, 6], F32, ph), mv=SB("mvo", [128, 2], F32, ph),
                          rstd=SB("rstdo", [128, 1], F32, ph), nmr=SB("nmro", [128, 1], F32, ph))
                wov = w_out[l].rearrange("(kc p) n -> p kc n", p=128)
                for q in range(4):
                    p.dma("pool", wo[:, q * 4:(q + 1) * 4, :], wov[:, q * 4:(q + 1) * 4, :], writes=["wo"])
                for m in range(2):
                    p.dma("sp", g1[m][:], gsc[l, 0, m:m + 1, :].to_broadcast([128, D]), writes=["g1"])
                p.dma("sp", lnG[:], ln_g[0][l:l + 1, :].to_broadcast([128, D]), writes=["lnG"])
                p.dma("sp", lnB[:], ln_b[0][l:l + 1, :].to_broadcast([128, D]), writes=["lnB"])
                for t in range(NT):
                    tok = slice(t * 128, (t + 1) * 128)
                    m = 1 if t < 2 else 0
                    c_, ck = cT[t % 2], "cT%d" % (t % 2)
                    x_, xk = xt2[t % 2], "xo%d" % (t % 2)
                    p.dma("sp", c_[:], catT[:, :, tok].rearrange("k p t -> p k t"), writes=[ck])
                    p.dma("sp", x_[:], blk_src(l, t), reads=["xres%d" % t], writes=[xk])
                    for oc in range(4):
                        for kc in range(16):
                            p.op("pe", lambda e, oc=oc, kc=kc, c_=c_: e.matmul(
                                ps[oc][:, :], c_[:, kc, :], wo[:, kc, oc * 512:(oc + 1) * 512], start=(kc == 0), stop=(kc == 15)),
                                [ck, "wo"], [psk[oc]])
                    for oc in range(4):
                        p.op("dve", lambda e, oc=oc, m=m: e.tensor_tensor(yg[:, oc * 512:(oc + 1) * 512], ps[oc][:, :],
                                                                          g1[m][:, oc * 512:(oc + 1) * 512], ALU.mult),
                             [psk[oc], "g1"], ["yg"])
                    p.op("dve", lambda e, x_=x_: e.scalar_tensor_tensor(yg[:], x_[:], ALPHA, yg[:], ALU.mult, ALU.add), [xk, "yg"], ["yg"])
                    ln_stats(yg, "yg", B6["stt"], B6["mv"], B6["rstd"], B6["nmr"], "o")
                    p.op("act", lambda e, x_=x_: e.activation(x_[:], yg[:], AF.Identity, bias=B6["nmr"][:], scale=B6["rstd"][:]),
                         ["yg", "onmr", "orstd"], [xk])
                    p.op("dve", lambda e, x_=x_: e.tensor_tensor(x_[:], x_[:], lnG[:], ALU.mult), [xk, "lnG"], [xk])
                    p.op("pool", lambda e, x_=x_: e.tensor_tensor(x_[:], x_[:], lnB[:], ALU.add), [xk, "lnB"], [xk])
                    p.dma("sp", xres[tok, :], x_[:], reads=[xk], writes=["xres%d" % t])
                p.flush()
            if "P6" in dbg:
                break
            NB = 2 * NT + NE
            if l == 0:
                jv_in = din("jv", [128, NB])
                pidx_in = din("pidx", [128, 1])
                h2d = dscr("h2d", [T, D], BF16)
                xslots = dscr("xslots", [NB * 128, D], BF16)
                yslots = dscr("yslots", [NB * 128, D])
                dest_i = SB("dest_i", [128, NT, 2], I32)
                gatew = SB("gatew", [128, NT, 2], F32)
                offs_i = SB("offs_i", [128, NB], I32)
            with ExitStack() as ph:
                B = dict(xt=[SB("xt%d" % i, [128, D], F32, ph) for i in range(2)], xn=SB("xn", [128, D], F32, ph),
                         stt=SB("stt", [128, 4, 6], F32, ph), mv=SB("mv", [128, 2], F32, ph),
                         rstd=SB("rstd", [128, 1], F32, ph), nmr=SB("nmr", [128, 1], F32, ph))
                scb = [SB("scb%d" % m, [128, D], F32, ph) for m in range(2)]
                shb = [SB("shb%d" % m, [128, D], F32, ph) for m in range(2)]
                h2 = SB("h2", [128, D], F32, ph)
                h2b = [SB("h2b%d" % i, [128, D], BF16, ph) for i in range(2)]
                h2T = SB("h2T", [128, 16, 128], F32, ph)
                wr = SB("wr", [128, 16, 36], F32, ph)
                brt = SB("brt", [128, 36], F32, ph)
                lg = SB("lg", [128, 36], F32, ph)
                sm = SB("sm", [128, 16], F32, ph)
                ohg = SB("ohg", [128, 4], F32, ph)
                ge = SB("ge", [128, 4], F32, ph)
                ein = SB("ein", [128, 8], F32, ph)
                ein2 = SB("ein2", [128, 8], F32, ph)
                oh8 = SB("oh8", [128, 2, 8], F32, ph)
                ohall = SB("ohall", [128, NT, 2, 32], F32, ph)
                At = SB("At", [128, 32], BF16, ph)
                Us = SB("Us", [128, 128], BF16, ph)
                base = SB("base", [128, 32], F32, ph)
                rank = SB("rank", [128, NT, 32], F32, ph)
                tmpr = SB("tmpr", [128, NT, 32], F32, ph)
                cs = [SB("cs%d" % i, [128, 32], F32, ph) for i in range(2)]
                padd = SB("padd", [128, 32], F32, ph)
                pst = SB("pst", [128, 32], F32, ph)
                dest_f = SB("dest_f", [128, NT, 2], F32, ph)
                jv = SB("jv", [128, NB], F32, ph)
                pidx = SB("pidx", [128, 1], F32, ph)
                cmp_ = SB("cmp", [128, NB, 32], F32, ph)
                be = SB("be", [128, NB], F32, ph)
                zt = SB("zt", [128, D], BF16, ph)
                p.dma("sp", jv[:], jv_in, writes=["jv"])
                p.dma("sp", pidx[:], pidx_in, writes=["pidx"])
                p.op("dve", lambda e: e.tensor_copy(Us[:], cst[:, 5, :]), ["cst"], ["Us"])
                p.op("dve", lambda e: e.memset(base[:], 0.0), [], ["base"])
                p.op("pool", lambda e: e.memset(zt[:], 0.0), [], ["zt"])
                for j in range(NB):
                    p.dma("sp", xslots[j * 128:(j + 1) * 128, :], zt[:], reads=["zt"], writes=["xslots"])
                for m in range(2):
                    p.dma("sp", shb[m][:], gsc[l, 2, m:m + 1, :].to_broadcast([128, D]), writes=["shb"])
                    p.dma("sp", scb[m][:], gsc[l, 3, m:m + 1, :].to_broadcast([128, D]), writes=["scb"])
                p.dma("sp", wr[:, :, 0:4], w_grp[l].rearrange("(kc p) n -> p kc n", p=128), writes=["wr"])
                p.dma("sp", wr[:, :, 4:36], w_exp[l].rearrange("(kc p) n -> p kc n", p=128), writes=["wr"])
                p.dma("sp", brt[:, 0:4], b_grp[l:l + 1, :].to_broadcast([128, 4]), writes=["brt"])
                p.dma("sp", brt[:, 4:36], b_exp[l:l + 1, :].to_broadcast([128, 32]), writes=["brt"])
                for t in range(NT):
                    tok = slice(t * 128, (t + 1) * 128)
                    m = 1 if t < 2 else 0
                    xt, xk = B["xt"][t % 2], "xt%d" % (t % 2)
                    hb, hbk = h2b[t % 2], "h2b%d" % (t % 2)
                    p.dma("sp", xt[:], xres[tok, :], writes=[xk])
                    ln_stats(xt, xk, B["stt"], B["mv"], B["rstd"], B["nmr"], "l")
                    xn = B["xn"]
                    p.op("act", lambda e, xt=xt: e.activation(xn[:], xt[:], AF.Identity, bias=B["nmr"][:], scale=B["rstd"][:]),
                         [xk, "lnmr", "lrstd"], ["xn"])
                    p.op("dve", lambda e, m=m: e.tensor_tensor(h2[:], xn[:], scb[m][:], ALU.mult), ["xn", "scb"], ["h2"])
                    p.op("pool", lambda e, m=m: e.tensor_tensor(h2[:], h2[:], shb[m][:], ALU.add), ["h2", "shb"], ["h2"])
                    p.op("act", lambda e, hb=hb: e.copy(hb[:], h2[:]), ["h2"], [hbk])
                    p.dma("sp", h2d[tok, :], hb[:], reads=[hbk], writes=["h2d%d" % t])
                    for q in range(4):
                        bk = 4 + q
                        for k4 in range(4):
                            kc = q * 4 + k4
                            p.op("pe", lambda e, kc=kc, k4=k4, bk=bk: e.transpose(
                                ps[bk][:, k4 * 128:(k4 + 1) * 128], h2[:, kc * 128:(kc + 1) * 128], ident[:]), ["h2", "ident"], [psk[bk]])
                        p.op("act" if q % 2 else "dve", lambda e, q=q, bk=bk: e.tensor_copy(
                            h2T[:, q * 4:(q + 1) * 4, :].rearrange("p a b -> p (a b)"), ps[bk][:, :]) if q % 2 == 0 else e.copy(
                            h2T[:, q * 4:(q + 1) * 4, :].rearrange("p a b -> p (a b)"), ps[bk][:, :]), [psk[bk]], ["h2T"])
                    for kc in range(16):
                        p.op("pe", lambda e, kc=kc: e.matmul(ps[0][:, 0:36], h2T[:, kc, :], wr[:, kc, :], start=(kc == 0), stop=(kc == 15)),
                             ["h2T", "wr"], [psk[0]])
                    p.op("dve", lambda e: e.tensor_tensor(lg[:], ps[0][:, 0:36], brt[:], ALU.add), [psk[0], "brt"], ["lg"])
                    D_ = lambda fn, r, w: p.op("dve", fn, r, w)
                    D_(lambda e: e.reduce_max(sm[:, 0:1], lg[:, 0:4], axis=AX.X), ["lg"], ["sm"])
                    D_(lambda e: e.tensor_scalar(ohg[:], lg[:, 0:4], sm[:, 0:1], None, ALU.is_equal), ["lg", "sm"], ["ohg"])
                    D_(lambda e: e.tensor_scalar(sm[:, 1:2], sm[:, 0:1], -1.0, None, ALU.mult), ["sm"], ["sm"])
                    p.op("act", lambda e: e.activation(ge[:], lg[:, 0:4], AF.Exp, bias=sm[:, 1:2], scale=1.0), ["lg", "sm"], ["ge"])
                    D_(lambda e: e.reduce_sum(sm[:, 2:3], ge[:], axis=AX.X), ["ge"], ["sm"])
                    D_(lambda e: e.reciprocal(sm[:, 3:4], sm[:, 2:3]), ["sm"], ["sm"])
                    D_(lambda e: e.tensor_scalar(ein[:], lg[:, 4:12], ohg[:, 0:1], None, ALU.mult), ["lg", "ohg"], ["ein"])
                    for g in range(1, 4):
                        D_(lambda e, g=g: e.scalar_tensor_tensor(ein[:], lg[:, 4 + g * 8:12 + g * 8], ohg[:, g:g + 1], ein[:], ALU.mult, ALU.add),
                           ["lg", "ohg", "ein"], ["ein"])
                    D_(lambda e: e.reduce_max(sm[:, 4:5], ein[:], axis=AX.X), ["ein"], ["sm"])
                    D_(lambda e: e.tensor_scalar(oh8[:, 0, :], ein[:], sm[:, 4:5], None, ALU.is_equal), ["ein", "sm"], ["oh8"])
                    D_(lambda e: e.scalar_tensor_tensor(ein2[:], oh8[:, 0, :], -1e30, ein[:], ALU.mult, ALU.add), ["oh8", "ein"], ["ein2"])
                    D_(lambda e: e.reduce_max(sm[:, 5:6], ein2[:], axis=AX.X), ["ein2"], ["sm"])
                    D_(lambda e: e.tensor_scalar(oh8[:, 1, :], ein2[:], sm[:, 5:6], None, ALU.is_equal), ["ein2", "sm"], ["oh8"])
                    D_(lambda e: e.tensor_tensor(sm[:, 6:7], sm[:, 5:6], sm[:, 4:5], ALU.subtract), ["sm"], ["sm"])
                    p.op("act", lambda e: e.activation(sm[:, 7:8], sm[:, 6:7], AF.Exp), ["sm"], ["sm"])
                    D_(lambda e: e.tensor_scalar(sm[:, 8:9], sm[:, 7:8], 1.0, None, ALU.add), ["sm"], ["sm"])
                    D_(lambda e: e.reciprocal(sm[:, 9:10], sm[:, 8:9]), ["sm"], ["sm"])
                    D_(lambda e: e.tensor_tensor(sm[:, 10:11], sm[:, 7:8], sm[:, 9:10], ALU.mult), ["sm"], ["sm"])
                    D_(lambda e, t=t: e.tensor_tensor(gatew[:, t, 0:1], sm[:, 9:10], sm[:, 3:4], ALU.mult), ["sm"], ["gatew"])
                    D_(lambda e, t=t: e.tensor_tensor(gatew[:, t, 1:2], sm[:, 10:11], sm[:, 3:4], ALU.mult), ["sm"], ["gatew"])
                    for k in range(2):
                        for g in range(4):
                            D_(lambda e, t=t, k=k, g=g: e.tensor_scalar(ohall[:, t, k, g * 8:(g + 1) * 8], oh8[:, k, :], ohg[:, g:g + 1], None, ALU.mult),
                               ["oh8", "ohg"], ["ohall"])
                    D_(lambda e, t=t: e.tensor_tensor(At[:], ohall[:, t, 0, :], ohall[:, t, 1, :], ALU.add), ["ohall"], ["At"])
                    p.op("pe", lambda e: e.matmul(ps[1][:, 0:32], Us[:], At[:], start=True, stop=True), ["Us", "At"], [psk[1]])
                    p.op("pe", lambda e: e.matmul(ps[2][:, 0:32], onesb[:], At[:], start=True, stop=True), ["onesb", "At"], [psk[2]])
                    D_(lambda e, t=t: e.tensor_tensor(rank[:, t, :], ps[1][:, 0:32], base[:], ALU.add), [psk[1], "base"], ["rank"])
                    D_(lambda e: e.tensor_tensor(base[:], ps[2][:, 0:32], base[:], ALU.add), [psk[2], "base"], ["base"])
                D_(lambda e: e.tensor_tensor(V(cmp_, 0, 128, [NB, 32], [1, NB]), V(base, 0, 128, [1, 32], [0, NB]),
                                             V(jv, 0, 128, [0, 32], [1, NB]), ALU.is_gt), ["base", "jv"], ["cmp"])
                D_(lambda e: e.reduce_sum(padd[:], V(cmp_, 0, 128, [NB, 32], [1, NB]), axis=AX.X), ["cmp"], ["padd"])
                D_(lambda e: e.tensor_scalar(padd[:], padd[:], 128.0, None, ALU.mult), ["padd"], ["padd"])
                D_(lambda e: e.tensor_copy(cs[0][:], padd[:]), ["padd"], ["cs0"])
                cur = 0
                for sh in (1, 2, 4, 8, 16):
                    a, b = cs[cur], cs[1 - cur]
                    ak, bkk = "cs%d" % cur, "cs%d" % (1 - cur)
                    D_(lambda e, a=a, b=b: e.tensor_copy(b[:], a[:]), [ak], [bkk])
                    D_(lambda e, a=a, b=b, sh=sh: e.tensor_tensor(b[:, sh:32], a[:, sh:32], a[:, 0:32 - sh], ALU.add), [ak], [bkk])
                    cur = 1 - cur
                ends, ek = cs[cur], "cs%d" % cur
                D_(lambda e: e.tensor_tensor(pst[:], ends[:], padd[:], ALU.subtract), [ek, "padd"], ["pst"])
                D_(lambda e: e.tensor_tensor(rank[:], rank[:], V(pst, 0, 128, [0, NT], [1, 32]), ALU.add), ["rank", "pst"], ["rank"])
                for k in range(2):
                    D_(lambda e, k=k: e.tensor_tensor(tmpr[:], ohall[:, :, k, :], rank[:], ALU.mult), ["ohall", "rank"], ["tmpr"])
                    D_(lambda e, k=k: e.reduce_sum(dest_f[:, :, k], tmpr[:], axis=AX.X), ["tmpr"], ["dest_f"])
                D_(lambda e: e.tensor_copy(dest_i[:], dest_f[:]), ["dest_f"], ["dest_i"])
                D_(lambda e: e.tensor_tensor(cmp_[:], V(ends, 0, 128, [0, NB], [1, 32]), V(jv, 0, 128, [1, NB], [0, 32]), ALU.is_le),
                   [ek, "jv"], ["cmp"])
                D_(lambda e: e.reduce_sum(be[:], cmp_[:], axis=AX.X), ["cmp"], ["be"])
                D_(lambda e: e.tensor_scalar(be[:], be[:], 31.0, 128.0, ALU.min, ALU.mult), ["be"], ["be"])
                D_(lambda e: e.tensor_scalar(be[:], be[:], pidx[:, 0:1], float(l * NE * 128), ALU.add, ALU.add), ["be", "pidx"], ["be"])
                D_(lambda e: e.tensor_copy(offs_i[:], be[:]), ["be"], ["offs_i"])
                p.flush()
                for t in range(NT):
                    tok = slice(t * 128, (t + 1) * 128)
                    hb, hbk = h2b[t % 2], "h2b%d" % (t % 2)
                    p.dma("sp", hb[:], h2d[tok, :], reads=["h2d%d" % t], writes=[hbk])
                    for k in range(2):
                        p.op("pool", lambda e, t=t, k=k, hb=hb: e.indirect_dma_start(
                            out=xslots, out_offset=bass.IndirectOffsetOnAxis(ap=dest_i[:, t, k:k + 1], axis=0),
                            in_=hb[:], in_offset=None), [hbk, "dest_i", "xslots"], [], dma=True)
                p.flush()

            with ExitStack() as ph:
                Wg = SB("Wg", [128, 16, EH], BF16, ph)
                Wu = SB("Wu", [128, 16, EH], BF16, ph)
                Wd = SB("Wd", [128, 8, D], BF16, ph)
                xb = [SB("xb%d" % i, [128, D], BF16, ph) for i in range(2)]
                xT = SB("xT", [128, 16, 128], BF16, ph)
                sg = SB("sg", [128, EH], F32, ph)
                act_ = SB("act", [128, EH], BF16, ph)
                actT = SB("actT", [128, 8, 128], BF16, ph)
                yb = [SB("yb%d" % i, [128, D], F32, ph) for i in range(2)]
                wg_tab = w_gate.rearrange("l e (p kc) n -> (l e p) (kc n)", kc=16)
                wu_tab = w_up.rearrange("l e (p kc) n -> (l e p) (kc n)", kc=16)
                wd_tab = w_down.rearrange("l e (p hc) n -> (l e p) (hc n)", hc=8)
                for j in range(NB):
                    x_, xk = xb[j % 2], "xb%d" % (j % 2)
                    y_, yk = yb[j % 2], "yb%d" % (j % 2)
                    for (W_, tab, wk) in ((Wg, wg_tab, "Wg"), (Wu, wu_tab, "Wu"), (Wd, wd_tab, "Wd")):
                        p.op("pool", lambda e, W_=W_, tab=tab, j=j: e.indirect_dma_start(
                            out=W_[:].rearrange("p a b -> p (a b)"), out_offset=None, in_=tab,
                            in_offset=bass.IndirectOffsetOnAxis(ap=offs_i[:, j:j + 1], axis=0)), ["offs_i"], [wk], dma=True)
                    p.dma("sp", x_[:], xslots[j * 128:(j + 1) * 128, :], writes=[xk])
                    for kc in range(16):
                        p.op("pe", lambda e, kc=kc, x_=x_: e.transpose(
                            psb[4 + kc // 8][:, (kc % 8) * 128:(kc % 8 + 1) * 128], V(x_, kc, 128, [16, 128]), identb[:]),
                            [xk, "identb"], [psk[4 + kc // 8]])
                    p.op("act", lambda e: e.copy(xT[:, 0:8, :].rearrange("p a b -> p (a b)"), psb[4][:, :]), [psk[4]], ["xT"])
                    p.op("dve", lambda e: e.tensor_copy(xT[:, 8:16, :].rearrange("p a b -> p (a b)"), psb[5][:, :]), [psk[5]], ["xT"])
                    for wi_, (W_, wk) in enumerate(((Wg, "Wg"), (Wu, "Wu"))):
                        for nh in range(2):
                            bk = wi_ * 2 + nh
                            for kc in range(16):
                                p.op("pe", lambda e, W_=W_, nh=nh, kc=kc, bk=bk: e.matmul(
                                    ps[bk][:, :], xT[:, kc, :], W_[:, kc, nh * 512:(nh + 1) * 512], start=(kc == 0), stop=(kc == 15)),
                                    ["xT", wk], [psk[bk]])
                    for nh in range(2):
                        p.op("act", lambda e, nh=nh: e.activation(sg[:, nh * 512:(nh + 1) * 512], ps[nh][:, :], AF.Silu), [psk[nh]], ["sg"])
                        p.op("dve", lambda e, nh=nh: e.tensor_tensor(act_[:, nh * 512:(nh + 1) * 512], sg[:, nh * 512:(nh + 1) * 512],
                                                                     ps[2 + nh][:, :], ALU.mult), ["sg", psk[2 + nh]], ["act"])
                    for hc in range(8):
                        p.op("pe", lambda e, hc=hc: e.transpose(psb[6][:, hc * 128:(hc + 1) * 128], V(act_, hc, 128, [8, 128]), identb[:]),
                             ["act", "identb"], [psk[6]])
                    p.op("act", lambda e: e.copy(actT[:].rearrange("p a b -> p (a b)"), psb[6][:, :]), [psk[6]], ["actT"])
                    for oc in range(4):
                        bk = oc
                        for hc in range(8):
                            p.op("pe", lambda e, oc=oc, hc=hc, bk=bk: e.matmul(
                                ps[bk][:, :], actT[:, hc, :], Wd[:, hc, oc * 512:(oc + 1) * 512], start=(hc == 0), stop=(hc == 7)),
                                ["actT", "Wd"], [psk[bk]])
                        p.op("act" if oc % 2 else "dve", (lambda e, oc=oc, bk=bk, y_=y_: e.copy(y_[:, oc * 512:(oc + 1) * 512], ps[bk][:, :])) if oc % 2
                             else (lambda e, oc=oc, bk=bk, y_=y_: e.tensor_copy(y_[:, oc * 512:(oc + 1) * 512], ps[bk][:, :])), [psk[bk]], [yk])
                    p.dma("sp", yslots[j * 128:(j + 1) * 128, :], y_[:], reads=[yk])
                p.flush()

            with ExitStack() as ph:
                g2 = [SB("g2_%d" % m, [128, D], F32, ph) for m in range(2)]
                lnG = SB("lnG2", [128, D], F32, ph)
                lnB = SB("lnB2", [128, D], F32, ph)
                ya = [SB("ya%d" % i, [128, D], F32, ph) for i in range(2)]
                yc = [SB("yc%d" % i, [128, D], F32, ph) for i in range(2)]
                xo = [SB("xq%d" % i, [128, D], F32, ph) for i in range(2)]
                B6 = dict(stt=SB("stt7", [128, 4, 6], F32, ph), mv=SB("mv7", [128, 2], F32, ph),
                          rstd=SB("rstd7", [128, 1], F32, ph), nmr=SB("nmr7", [128, 1], F32, ph))
                for m in range(2):
                    p.dma("sp", g2[m][:], gsc[l, 1, m:m + 1, :].to_broadcast([128, D]), writes=["g2"])
                p.dma("sp", lnG[:], ln_g[1][l:l + 1, :].to_broadcast([128, D]), writes=["lnG"])
                p.dma("sp", lnB[:], ln_b[1][l:l + 1, :].to_broadcast([128, D]), writes=["lnB"])
                for t in range(NT):
                    if l == DEPTH - 1 and t < 2:
                        continue
                    tok = slice(t * 128, (t + 1) * 128)
                    m = 1 if t < 2 else 0
                    i_ = t % 2
                    for k, (yy, ykk) in enumerate(((ya[i_], "ya%d" % i_), (yc[i_], "yc%d" % i_))):
                        p.op("pool", lambda e, t=t, k=k, yy=yy: e.indirect_dma_start(
                            out=yy[:], out_offset=None, in_=yslots,
                            in_offset=bass.IndirectOffsetOnAxis(ap=dest_i[:, t, k:k + 1], axis=0)), ["dest_i"], [ykk], dma=True)
                    x_, xk = xo[i_], "xq%d" % i_
                    p.dma("sp", x_[:], xres[tok, :], reads=["xres%d" % t], writes=[xk])
                    y1_, y2_ = ya[i_], yc[i_]
                    p.op("dve", lambda e, t=t, y1_=y1_: e.tensor_scalar(y1_[:], y1_[:], gatew[:, t, 0:1], None, ALU.mult), ["ya%d" % i_, "gatew"], ["ya%d" % i_])
                    p.op("dve", lambda e, t=t, y1_=y1_, y2_=y2_: e.scalar_tensor_tensor(y1_[:], y2_[:], gatew[:, t, 1:2], y1_[:], ALU.mult, ALU.add),
                         ["ya%d" % i_, "yc%d" % i_, "gatew"], ["ya%d" % i_])
                    p.op("pool", lambda e, y1_=y1_, m=m: e.tensor_tensor(y1_[:], y1_[:], g2[m][:], ALU.mult), ["ya%d" % i_, "g2"], ["ya%d" % i_])
                    p.op("dve", lambda e, x_=x_, y1_=y1_: e.scalar_tensor_tensor(y1_[:], x_[:], ALPHA, y1_[:], ALU.mult, ALU.add),
                         [xk, "ya%d" % i_], ["ya%d" % i_])
                    ln_stats(y1_, "ya%d" % i_, B6["stt"], B6["mv"], B6["rstd"], B6["nmr"], "q")
                    p.op("act", lambda e, x_=x_, y1_=y1_: e.activation(x_[:], y1_[:], AF.Identity, bias=B6["nmr"][:], scale=B6["rstd"][:]),
                         ["ya%d" % i_, "qnmr", "qrstd"], [xk])
                    p.op("dve", lambda e, x_=x_: e.tensor_tensor(x_[:], x_[:], lnG[:], ALU.mult), [xk, "lnG"], [xk])
                    p.op("pool", lambda e, x_=x_: e.tensor_tensor(x_[:], x_[:], lnB[:], ALU.add), [xk, "lnB"], [xk])
                    if l == DEPTH - 1:
                        p.dma("sp", out_d[(t - 2) * 128:(t - 1) * 128, :], x_[:], reads=[xk])
                    else:
                        p.dma("sp", xres[tok, :], x_[:], reads=[xk], writes=["xres%d" % t])
                p.flush()
            if "L0" in dbg:
                break
        p.finish()
        print("instructions:", p.n_inst)
    return nc


def _consts(L):
    T = NCTX + L
    f32 = np.float32
    t = np.arange(T)
    lat = t >= NCTX
    i = np.where(lat, t - NCTX, 0)
    row = (i // 64).astype(f32)
    col = (i % 64).astype(f32)
    pos = t.astype(f32)
    fa = (10000.0 ** (-np.arange(32, dtype=f32) / 32)).astype(f32)
    fb = (10000.0 ** (-np.arange(16, dtype=f32) / 16)).astype(f32)

    def cs(pp, fr, mask):
        ang = (pp[:, None] * fr[None, :]).astype(f32)
        c = np.cos(ang).astype(f32)
        s = np.sin(ang).astype(f32)
        if mask is not None:
            c = np.where(mask[:, None], c, 1.0).astype(f32)
            s = np.where(mask[:, None], s, 0.0).astype(f32)
        return c, s
    cr, sr = cs(row, fa, lat)
    cc, sc = cs(col, fa, lat)
    swa = [np.concatenate([cr, cr, cc, cc], 1), np.concatenate([-sr, sr, -sc, sc], 1)]
    cp, sp = cs(pos, fa, None)
    ret = [np.concatenate([cp, cp], 1), np.concatenate([-sp, sp], 1)]
    cr, sr = cs(row, fb, lat)
    cc, sc = cs(col, fb, lat)
    mla = [np.concatenate([cr, cr, cc, cc], 1), np.concatenate([-sr, sr, -sc, sc], 1)]
    rtab = np.ascontiguousarray(np.concatenate(swa + ret + mla, 1).astype(f32))
    s = np.arange(128)[:, None]
    c = np.arange(128)[None, :]
    z = 0 * s + 0 * c
    cst = np.ascontiguousarray(np.stack([(s <= c) + z, (s >= c) + z, (c - s) + z, (c + 1) + z, (128 - c) + z, (s < c) + z], 1).astype(f32))
    scol = np.ascontiguousarray(np.stack([127 - np.arange(128), np.arange(128)], 1).astype(f32))
    NB = 2 * (T // 128) + NE
    jv = np.ascontiguousarray(np.tile((np.arange(NB, dtype=f32) * 128)[None, :], (128, 1)))
    pidx = np.arange(128, dtype=f32).reshape(128, 1)
    return dict(ident=np.eye(128, dtype=f32), rtab=rtab, cst=cst, scol=scol, jv=jv, pidx=pidx)


_WKEYS = ("w_ada", "b_ada", "w_in", "swa_sink", "ret_decay", "mla_kv_norm", "mla_w_uk", "mla_w_uv", "w_out",
          "ln1_g", "ln1_b", "ln2_g", "ln2_b", "moe_w_group", "moe_b_group", "moe_w_expert", "moe_b_expert",
          "moe_w_gate", "moe_w_up", "moe_w_down")


def core_inputs(inp, b, L, consts=None, names=None):
    m = dict(consts if consts is not None else _consts(L))
    m["x"] = np.ascontiguousarray(inp["x"][b], dtype=np.float32)
    m["ctx"] = np.ascontiguousarray(inp["ctx"][b], dtype=np.float32)
    m["c2"] = np.ascontiguousarray(np.stack([inp["c"][b], inp["c_ctx"]], 0), dtype=np.float32)
    for k in _WKEYS:
        if names is None or k in names:
            m[k] = np.ascontiguousarray(inp[k], dtype=np.float32)
    return m


_NC_CACHE = {}


def kernel(**inputs):
    L = int(inputs["x"].shape[1])
    Bn = int(inputs["x"].shape[0])
    if L not in _NC_CACHE:
        _NC_CACHE[L] = build(L)
    nc = _NC_CACHE[L]
    consts = _consts(L)
    inp = {k: np.asarray(v) for k, v in inputs.items()}
    in_maps = [core_inputs(inp, b, L, consts, names=nc._in_names) for b in range(Bn)]
    res = run_bass_kernel_spmd(nc, in_maps, core_ids=list(range(Bn)))
    out = np.stack([np.asarray(res.results[b]["out"]) for b in range(Bn)], 0)
    return out.astype(np.float32)
```

```python
import math
from contextlib import ExitStack
import numpy as np
import ml_dtypes
import concourse.bass as bass
import concourse.mybir as mybir
from concourse.bass_utils import run_bass_kernel_spmd

F32 = mybir.dt.float32
BF16 = mybir.dt.bfloat16
I32 = mybir.dt.int32
AF = mybir.ActivationFunctionType
ALU = mybir.AluOpType
AX = mybir.AxisListType

D = 2048
NCTX = 256
DEPTH = 2
IN_W = 5440
ALPHA = (2 * DEPTH) ** 0.25
EPS = 1e-6
NE = 32
EH = 1024
COMPUTE = ("pe", "act", "dve", "pool")


class Prog:
    def __init__(self, nc, stack, n_dma_sems=12):
        self.nc = nc
        self.eng = {"pe": nc.tensor, "act": nc.scalar, "dve": nc.vector,
                    "pool": nc.gpsimd, "sp": nc.sync}
        self.ops = []
        self.nd = n_dma_sems
        self.csem = {e: stack.enter_context(nc.semaphore("cs_" + e)) for e in COMPUTE}
        self.ccount = {e: 0 for e in COMPUTE}
        self.sem_obj = {("c", e): self.csem[e] for e in COMPUTE}
        self.dstate = {}
        for e in ("sp", "act", "pool"):
            sems = [stack.enter_context(nc.semaphore("ds_%s_%d" % (e, k))) for k in range(n_dma_sems)]
            for k, s in enumerate(sems):
                self.sem_obj[("d", e, k)] = s
            self.dstate[e] = dict(next=0, cnt=[0] * n_dma_sems)
        self.waited = {}
        self.carry = {}
        self.pend = {e: {} for e in self.eng}
        self.n_inst = 0

    def op(self, eng, fn, reads=(), writes=(), dma=False):
        self.ops.append((eng, fn, tuple(reads), tuple(writes), dma))

    def dma(self, eng, out, in_, reads=(), writes=(), **kw):
        self.op(eng, lambda e: e.dma_start(out=out, in_=in_, **kw), reads, writes, dma=True)

    def _wait(self, eng, sk, val):
        key = (eng, sk)
        if self.waited.get(key, 0) >= val:
            return
        self.waited[key] = val
        self.eng[eng].wait_ge(self.sem_obj[sk], val)
        self.n_inst += 1

    def flush(self):
        ops = self.ops
        self.ops = []
        n = len(ops)
        last_w, readers = {}, {}
        deps = [None] * n
        last_on_eng = {}
        dma_ops = []
        for i, (eng, fn, reads, writes, dma) in enumerate(ops):
            d = set()
            for k in reads:
                if k in last_w:
                    d.add(last_w[k])
            for k in writes:
                if k in last_w:
                    d.add(last_w[k])
                r = readers.get(k)
                if r:
                    d.update(r[0].values())
                    d.update(r[1])
            d.discard(i)
            deps[i] = d
            for k in reads:
                r = readers.setdefault(k, ({}, []))
                if dma:
                    r[1].append(i)
                else:
                    r[0][eng] = i
            for k in writes:
                last_w[k] = i
                readers[k] = ({}, [])
            if dma:
                dma_ops.append(i)
            else:
                last_on_eng[eng] = i
        signal = [False] * n
        for i, (eng, fn, reads, writes, dma) in enumerate(ops):
            keep = set()
            for j in deps[i]:
                ej, _, rj, wj, dj = ops[j]
                if (not dj) and ej == eng and not dma:
                    if eng == "pe":
                        continue
                    if not (set(wj) & set(reads)):
                        continue
                keep.add(j)
            deps[i] = keep
            for j in keep:
                signal[j] = True
        for e, j in last_on_eng.items():
            signal[j] = True
        event = [None] * n
        for i, (eng, fn, reads, writes, dma) in enumerate(ops):
            need = {}
            if self.pend[eng]:
                need.update(self.pend[eng])
                self.pend[eng] = {}
            for j in deps[i]:
                sk, val = event[j]
                if need.get(sk, 0) < val:
                    need[sk] = val
            for sk, val in need.items():
                self._wait(eng, sk, val)
            if dma:
                st = self.dstate[eng]
                k = st["next"]
                st["next"] = (k + 1) % self.nd
                sk = ("d", eng, k)
                if st["cnt"][k] > 0:
                    self._wait(eng, sk, st["cnt"][k])
                st["cnt"][k] += 16
                ins = fn(self.eng[eng])
                ins.then_inc(self.sem_obj[sk], 16)
                event[i] = (sk, st["cnt"][k])
            else:
                ins = fn(self.eng[eng])
                if signal[i]:
                    self.ccount[eng] += 1
                    ins.then_inc(self.csem[eng], 1)
                    event[i] = (("c", eng), self.ccount[eng])
            self.n_inst += 1
        carry = {}
        for e in COMPUTE:
            if self.ccount[e] > 0:
                carry[("c", e)] = self.ccount[e]
        for e, st in self.dstate.items():
            for k in range(self.nd):
                if st["cnt"][k] > 0:
                    carry[("d", e, k)] = st["cnt"][k]
        for e in self.eng:
            self.pend[e] = dict(carry)

    def finish(self):
        self.flush()
        for sk, val in self.pend["sp"].items():
            self._wait("sp", sk, val)


def V(t, off, *dims):
    F = 1
    for s in t.shape[1:]:
        F *= s
    npart = dims[0]
    return bass.AP(t, off, [[F, npart]] + [list(d) for d in dims[1:]])


CHUNKS = [("sq0", 0, 512), ("sq1", 512, 256), ("sk", 768, 256), ("sv", 1024, 256),
          ("rq", 1280, 384), ("rk", 1664, 384), ("rv0", 2048, 512), ("rv1", 2560, 256),
          ("gf0", 2816, 512), ("gf1", 3328, 256), ("gb0", 3584, 512), ("gb1", 4096, 256),
          ("mq0", 4352, 384), ("mq1", 4736, 384), ("ckv", 5120, 320)]


def build(L, dbg=()):
    nc = bass.Bass("TRN2", target_bir_lowering=False)
    NT = (NCTX + L) // 128
    T = NT * 128
    NLT = L // 128

    in_names = []
    nc._in_names = in_names

    def din(name, shape, dt=F32):
        in_names.append(name)
        return nc.dram_tensor(name, list(shape), dt, kind="ExternalInput").ap()

    def dscr(name, shape, dt=F32):
        kind = "ExternalOutput" if name in dbg else "Internal"
        return nc.dram_tensor(name, list(shape), dt, kind=kind).ap()

    x_in = din("x", [L, D])
    ctx_in = din("ctx", [NCTX, D])
    c2_in = din("c2", [2, D])
    w_ada = din("w_ada", [DEPTH, D, 6 * D])
    b_ada = din("b_ada", [DEPTH, 6 * D])
    w_in = din("w_in", [DEPTH, D, IN_W])
    swa_sink = din("swa_sink", [DEPTH, 6])
    ret_decay = din("ret_decay", [DEPTH, 2, 6])
    kv_norm = din("mla_kv_norm", [DEPTH, 256])
    w_uk = din("mla_w_uk", [DEPTH, 256, 512])
    w_uv = din("mla_w_uv", [DEPTH, 256, 512])
    w_out = din("w_out", [DEPTH, D, D])
    ln_g = [din("ln1_g", [DEPTH, D]), din("ln2_g", [DEPTH, D])]
    ln_b = [din("ln1_b", [DEPTH, D]), din("ln2_b", [DEPTH, D])]
    w_grp = din("moe_w_group", [DEPTH, D, 4])
    b_grp = din("moe_b_group", [DEPTH, 4])
    w_exp = din("moe_w_expert", [DEPTH, D, NE])
    b_exp = din("moe_b_expert", [DEPTH, NE])
    if not (set(dbg) & {"P2", "P5", "P6"}):
        w_gate = din("moe_w_gate", [DEPTH, NE, D, EH])
        w_up = din("moe_w_up", [DEPTH, NE, D, EH])
        w_down = din("moe_w_down", [DEPTH, NE, EH, D])
    ident_in = din("ident", [128, 128])
    rtab_in = din("rtab", [T, 512])
    out_d = nc.dram_tensor("out", [L, D], F32, kind="ExternalOutput").ap()

    xres = dscr("xres", [T, D])
    gsc = dscr("gsc", [DEPTH, 4, 2, D])
    qTs = dscr("qTs", [6, 128, T], BF16)
    kTs = dscr("kTs", [2, 128, T], BF16)
    vs = dscr("vs", [T, 256], BF16)
    qTr = dscr("qTr", [3, 128, T], BF16)
    kTr = dscr("kTr", [3, 128, T], BF16)
    kr = dscr("kr", [T, 384], BF16)
    vr = dscr("vr", [T, 768], BF16)
    gfb = dscr("gfb", [2, T, 768])
    qnT = dscr("qnT", [4, 128, T], BF16)
    qrT = dscr("qrT", [4, 64, T], BF16)
    knT = dscr("knT", [4, 128, T], BF16)
    krT = dscr("krT", [64, T], BF16)
    vm = dscr("vm", [T, 512], BF16)
    catT = dscr("catT", [16, 128, T], BF16)

    with ExitStack() as st:
        p = Prog(nc, st)
        _uid = [0]

        def SB(name, shape, dt=F32, s=st):
            _uid[0] += 1
            return s.enter_context(nc.sbuf_tensor("s%d_%s" % (_uid[0], name), list(shape), dt))
        ps = [st.enter_context(nc.psum_tensor("ps%d" % i, [128, 512], F32)) for i in range(8)]
        psk = ["ps%d" % i for i in range(8)]
        psb = [b[:].bitcast(BF16) for b in ps]

        ident = SB("ident", [128, 128])
        identb = SB("identb", [128, 128], BF16)
        modcol = SB("modcol", [128, 96, 2])
        scT = SB("scT", [128, 16, 2], BF16)
        p.dma("sp", ident[:], ident_in, writes=["ident"])
        p.op("dve", lambda e: e.tensor_copy(identb[:], ident[:]), ["ident"], ["identb"])

        with ExitStack() as ph:
            c2 = SB("c2", [2, D], F32, ph)
            p.dma("sp", c2[:], c2_in, writes=["c2"])
            p.op("act", lambda e: e.activation(c2[:], c2[:], AF.Silu), ["c2"], ["c2"])
            for kc in range(16):
                p.op("pe", lambda e, kc=kc: e.transpose(ps[0][:, kc * 2:kc * 2 + 2], c2[:, kc * 128:(kc + 1) * 128],
                                                        ident[0:2, 0:2]), ["c2", "ident"], [psk[0]])
            p.op("dve", lambda e: e.tensor_copy(scT[:].rearrange("p a b -> p (a b)"), ps[0][:, 0:32]), [psk[0]], ["scT"])
            p.flush()

        cst_in = din("cst", [128, 6, 128])
        scol_in = din("scol", [128, 2])
        cst = SB("cst", [128, 6, 128])
        scol = SB("scol", [128, 2])
        mle = SB("mle", [128, 128], BF16)
        mge = SB("mge", [128, 128], BF16)
        onesb = SB("onesb", [128, 128], BF16)
        onesf = SB("onesf", [128, 128])
        p.dma("sp", cst[:], cst_in, writes=["cst"])
        p.dma("sp", scol[:], scol_in, writes=["scol"])
        p.op("dve", lambda e: e.tensor_copy(mle[:], cst[:, 0, :]), ["cst"], ["mle"])
        p.op("dve", lambda e: e.tensor_copy(mge[:], cst[:, 1, :]), ["cst"], ["mge"])
        p.op("dve", lambda e: e.memset(onesb[:], 1.0), [], ["onesb"])
        p.op("dve", lambda e: e.memset(onesf[:], 1.0), [], ["onesf"])
        p.flush()

        def blk_src(l, t):
            if l == 0:
                return ctx_in[t * 128:(t + 1) * 128, :] if t < 2 else x_in[(t - 2) * 128:(t - 1) * 128, :]
            return xres[t * 128:(t + 1) * 128, :]

        cnt = [0]

        def ln_stats(xt, xk, stt, mv, rstd, nmr, pre):
            for c4 in range(4):
                p.op("dve", lambda e, c4=c4: e.bn_stats(stt[:, c4, :], xt[:, c4 * 512:(c4 + 1) * 512]), [xk], [pre + "st"])
            p.op("dve", lambda e: e.bn_aggr(mv[:], stt[:].rearrange("p a b -> p (a b)")), [pre + "st"], [pre + "mv"])
            p.op("dve", lambda e: e.tensor_scalar(rstd[:], mv[:, 1:2], EPS, None, ALU.add), [pre + "mv"], [pre + "rstd"])
            p.op("act", lambda e: e.activation(rstd[:], rstd[:], AF.Sqrt), [pre + "rstd"], [pre + "rstd"])
            p.op("dve", lambda e: e.reciprocal(rstd[:], rstd[:]), [pre + "rstd"], [pre + "rstd"])
            p.op("dve", lambda e: e.scalar_tensor_tensor(nmr[:], mv[:, 0:1], -1.0, rstd[:], ALU.mult, ALU.mult),
                 [pre + "mv", pre + "rstd"], [pre + "nmr"])

        def ln_mod_T(src_ap, m, jsh, jsc, hT, hk, slot, B):
            i = cnt[0]
            cnt[0] += 1
            xt, xk = B["xt"][i % 2], "xt%d" % (i % 2)
            p.dma("sp", xt[:], src_ap, writes=[xk])
            ln_stats(xt, xk, B["stt"], B["mv"], B["rstd"], B["nmr"], "l")
            xn = B["xn"]
            p.op("act", lambda e: e.activation(xn[:], xt[:], AF.Identity, bias=B["nmr"][:], scale=B["rstd"][:]),
                 [xk, "lnmr", "lrstd"], ["xn"])
            for q in range(4):
                bk = 4 + q
                for k4 in range(4):
                    kc = q * 4 + k4
                    p.op("pe", lambda e, kc=kc, k4=k4, bk=bk: e.transpose(
                        ps[bk][:, k4 * 128:(k4 + 1) * 128], xn[:, kc * 128:(kc + 1) * 128], ident[:]),
                        ["xn", "ident"], [psk[bk]])
                for k4 in range(4):
                    kc = q * 4 + k4
                    p.op("act", lambda e, kc=kc, k4=k4, bk=bk: e.activation(
                        hT[:, kc, slot * 128:(slot + 1) * 128], ps[bk][:, k4 * 128:(k4 + 1) * 128], AF.Identity,
                        bias=modcol[:, jsh * 16 + kc, m:m + 1], scale=modcol[:, jsc * 16 + kc, m:m + 1]),
                        [psk[bk], "modcol"], [hk])

        def rope(rs, W, H, d, hb, hstride, hoff, tab, tk, cb, sb, r1, r2, rb, x=""):
            nb2 = d // (2 * hb)
            full = lambda t, o=0: V(t, hoff + o, 128, [hstride, H], [2 * hb, nb2], [1, hb])
            p.op("dve", lambda e: e.tensor_tensor(
                V(r1, hoff, 128, [hstride, H], [1, d]), V(rs, hoff, 128, [hstride, H], [1, d]),
                V(tab, cb, 128, [0, H], [1, d]), ALU.mult), ["rs" + x, tk], ["r1" + x])
            for b in range(2):
                p.op("pool", lambda e, b=b: e.tensor_tensor(
                    full(r2, b * hb), full(rs, (1 - b) * hb),
                    V(tab, sb + b * hb, 128, [0, H], [2 * hb, nb2], [1, hb]), ALU.mult), ["rs" + x, tk], ["r2" + x])
            p.op("dve", lambda e: e.tensor_tensor(
                V(rb, hoff, 128, [hstride, H], [1, d]), V(r1, hoff, 128, [hstride, H], [1, d]),
                V(r2, hoff, 128, [hstride, H], [1, d]), ALU.add), ["r1" + x, "r2" + x], ["rb" + x])

        for l in range(DEPTH):
            with ExitStack() as ph:
                wa = [SB("wa%d" % i, [128, 16, 1024], BF16, ph) for i in range(2)]
                bar = SB("bar", [96, 128], F32, ph)
                bcol = SB("bcol", [128, 96], F32, ph)
                rows2 = SB("rows2", [2, 4, D], F32, ph)
                p.dma("sp", bar[:], b_ada[l].rearrange("(a b) -> a b", b=128), writes=["bar"])
                p.op("pe", lambda e: e.transpose(ps[1][:, 0:96], bar[:], ident[0:96, 0:96]), ["bar", "ident"], [psk[1]])
                p.op("dve", lambda e: e.tensor_copy(bcol[:], ps[1][:, 0:96]), [psk[1]], ["bcol"])
                wav = w_ada[l].rearrange("(kc p) n -> p kc n", p=128)
                for g in range(12):
                    w = wa[g % 2]
                    wk = "wa%d" % (g % 2)
                    p.dma("pool", w[:], wav[:, :, g * 1024:(g + 1) * 1024], writes=[wk])
                    for nn in range(8):
                        n = g * 8 + nn
                        for kc in range(16):
                            p.op("pe", lambda e, w=w, nn=nn, n=n, kc=kc: e.matmul(
                                ps[0][:, 2 * n:2 * n + 2], w[:, kc, nn * 128:(nn + 1) * 128], scT[:, kc, :],
                                start=(kc == 0), stop=(kc == 15)), [wk, "scT"], [psk[0]])
                p.op("dve", lambda e: e.tensor_tensor(
                    modcol[:], ps[0][:, 0:192].rearrange("p (a b) -> p a b", b=2),
                    V(bcol, 0, 128, [1, 96], [0, 2]), ALU.add), [psk[0], "bcol"], ["modcol"])
                for j in (1, 4):
                    p.op("dve", lambda e, j=j: e.tensor_scalar_add(modcol[:, j * 16:(j + 1) * 16, :],
                                                                   modcol[:, j * 16:(j + 1) * 16, :], 1.0),
                         ["modcol"], ["modcol"])
                for si, j in enumerate((2, 5, 3, 4)):
                    for kc in range(16):
                        bk = 2 + kc // 4
                        p.op("pe", lambda e, j=j, kc=kc, bk=bk: e.transpose(
                            ps[bk][0:2, (kc % 4) * 128:(kc % 4 + 1) * 128], modcol[:, j * 16 + kc, :], ident[:]),
                            ["modcol", "ident"], [psk[bk]])
                    for q in range(4):
                        p.op("dve", lambda e, si=si, q=q: e.tensor_copy(rows2[:, si, q * 512:(q + 1) * 512], ps[2 + q][0:2, :]),
                             [psk[2 + q]], ["rows2"])
                p.dma("sp", gsc[l].rearrange("s m d -> m s d"), rows2[:], reads=["rows2"], writes=[])
                p.flush()

            with ExitStack() as ph:
                B = dict(xt=[SB("xt%d" % i, [128, D], F32, ph) for i in range(2)], xn=SB("xn", [128, D], F32, ph),
                         stt=SB("stt", [128, 4, 6], F32, ph), mv=SB("mv", [128, 2], F32, ph),
                         rstd=SB("rstd", [128, 1], F32, ph), nmr=SB("nmr", [128, 1], F32, ph))
                hT = SB("hT", [128, 16, 512], BF16, ph)
                wch = [SB("wch%d" % i, [128, 16, 512], BF16, ph) for i in range(2)]
                rt = [SB("rt%d" % i, [128, 512], F32, ph) for i in range(4)]
                rsL = [SB("rs%d" % i, [128, 768], F32, ph) for i in range(2)]
                r1L = [SB("r1%d" % i, [128, 768], F32, ph) for i in range(2)]
                r2L = [SB("r2%d" % i, [128, 768], F32, ph) for i in range(2)]
                rbL = [SB("rb%d" % i, [128, 768], BF16, ph) for i in range(2)]
                tTL = [SB("tT%d" % i, [128, 6, 128], BF16, ph) for i in range(2)]
                gstL = [SB("gst%d" % i, [128, 512], F32, ph) for i in range(2)]
                cnTL = [SB("cnT%d" % i, [128, 2, 128], BF16, ph) for i in range(2)]
                ssqL = [SB("ssq%d" % i, [128, 1], F32, ph) for i in range(2)]
                gkv = SB("gkv", [128, 256], F32, ph)
                wuk = SB("wuk", [128, 2, 512], BF16, ph)
                wuv = SB("wuv", [128, 2, 512], BF16, ph)
                p.dma("sp", gkv[:], kv_norm[l:l + 1, :].to_broadcast([128, 256]), writes=["gkv"])
                p.dma("pool", wuk[:], w_uk[l].rearrange("(rc p) n -> p rc n", p=128), writes=["wuk"])
                p.dma("pool", wuv[:], w_uv[l].rearrange("(rc p) n -> p rc n", p=128), writes=["wuv"])
                wiv = w_in[l].rearrange("(kc p) n -> p kc n", p=128)
                wi = 0
                mmc = [0]
                pcnt = [0]
                pending = [None]
                for g0 in range(0, NT, 4):
                    tblks = list(range(g0, min(g0 + 4, NT)))
                    for s_, t in enumerate(tblks):
                        ln_mod_T(blk_src(l, t), 1 if t < 2 else 0, 0, 1, hT, "hT", s_, B)
                        p.dma("sp", rt[s_][:], rtab_in[t * 128:(t + 1) * 128, :], writes=["rt%d" % s_])
                    for (cname, c0, cw) in CHUNKS:
                        w = wch[wi % 2]
                        wk = "wch%d" % (wi % 2)
                        wi += 1
                        p.dma("pool", w[:, :, 0:cw], wiv[:, :, c0:c0 + cw], writes=[wk])
                        for s_, t in enumerate(tblks):
                            tok = slice(t * 128, (t + 1) * 128)
                            bk = mmc[0] % 4
                            mmc[0] += 1
                            pk = psk[bk]
                            pt = ps[bk]
                            tab, tk = rt[s_], "rt%d" % s_
                            for kc in range(16):
                                p.op("pe", lambda e, kc=kc, s_=s_, w=w, pt=pt, cw=cw: e.matmul(
                                    pt[:, 0:cw], hT[:, kc, s_ * 128:(s_ + 1) * 128], w[:, kc, 0:cw],
                                    start=(kc == 0), stop=(kc == 15)), ["hT", wk], [pk])

                            def post(cname=cname, cw=cw, pt=pt, pk=pk, tab=tab, tk=tk, tok=tok, x=str(pcnt[0] % 2)):
                                rs, r1, r2, rb, tT, gst, cnT, ssq = (rsL[int(x)], r1L[int(x)], r2L[int(x)], rbL[int(x)], tTL[int(x)],
                                                                      gstL[int(x)], cnTL[int(x)], ssqL[int(x)])
                                def transp(nblk, width, dst_ap, srcoff=lambda b: b * 128):
                                    for b in range(nblk):
                                        p.op("pe", lambda e, b=b: e.transpose(
                                            psb[7][0:width, b * 128:(b + 1) * 128], rb[:, srcoff(b):srcoff(b) + width], identb[:]),
                                            ["rb" + x, "identb"], [psk[7]])
                                    p.op("act", lambda e: e.copy(tT[0:width, 0:nblk, :],
                                                                 psb[7][0:width, 0:nblk * 128].rearrange("p (a b) -> p a b", b=128)),
                                         [psk[7]], ["tT" + x])
                                    p.dma("sp", dst_ap, tT[0:width, 0:nblk, :], reads=["tT" + x], writes=[])

                                if cname in ("sq0", "sq1", "sk"):
                                    H = cw // 128
                                    sc = 128 ** -0.5 if cname != "sk" else 1.0
                                    p.op("act", lambda e, pt=pt, cw=cw, sc=sc: e.mul(rs[:, 0:cw], pt[:, 0:cw], sc), [pk], ["rs" + x])
                                    rope(rs, cw, H, 128, 32, 128, 0, tab, tk, 0, 128, r1, r2, rb, x)
                                    if cname == "sk":
                                        dst = kTs[:, :, tok]
                                    else:
                                        h0 = 0 if cname == "sq0" else 4
                                        dst = qTs[h0:h0 + H, :, tok]
                                    transp(H, 128, dst.rearrange("h p t -> p h t"))
                                elif cname in ("sv", "rv0", "rv1"):
                                    p.op("act", lambda e, pt=pt, cw=cw: e.copy(rb[:, 0:cw], pt[:, 0:cw]), [pk], ["rb" + x])
                                    if cname == "sv":
                                        dst = vs[tok, :]
                                    else:
                                        o = 0 if cname == "rv0" else 512
                                        dst = vr[tok, o:o + cw]
                                    p.dma("sp", dst, rb[:, 0:cw], reads=["rb" + x], writes=[])
                                elif cname in ("gf0", "gf1", "gb0", "gb1"):
                                    p.op("act", lambda e, pt=pt, cw=cw: e.activation(gst[:, 0:cw], pt[:, 0:cw], AF.Silu), [pk], ["gst" + x])
                                    o = 0 if cname[2] == "0" else 512
                                    p.dma("sp", gfb[0 if cname[1] == "f" else 1, tok, o:o + cw], gst[:, 0:cw], reads=["gst" + x], writes=[])
                                elif cname in ("rq", "rk"):
                                    sc = 64 ** -0.5 if cname == "rq" else 1.0
                                    p.op("act", lambda e, pt=pt, sc=sc: e.mul(rs[:, 0:384], pt[:, 0:384], sc), [pk], ["rs" + x])
                                    rope(rs, 384, 6, 64, 32, 64, 0, tab, tk, 256, 320, r1, r2, rb, x)
                                    if cname == "rk":
                                        p.dma("sp", kr[tok, :], rb[:, 0:384], reads=["rb" + x], writes=[])
                                    dst = (qTr if cname == "rq" else kTr)[:, :, tok]
                                    transp(3, 128, dst.rearrange("h p t -> p h t"))
                                elif cname in ("mq0", "mq1"):
                                    sc = 192 ** -0.5
                                    p.op("act", lambda e, pt=pt, sc=sc: e.mul(rs[:, 0:384], pt[:, 0:384], sc), [pk], ["rs" + x])
                                    p.op("dve", lambda e: e.tensor_copy(rb[:, 0:384], rs[:, 0:384]), ["rs" + x], ["rb" + x])
                                    rope(rs, 384, 2, 64, 16, 192, 128, tab, tk, 384, 448, r1, r2, rb, x)
                                    h0 = 0 if cname == "mq0" else 2
                                    transp(2, 128, qnT[h0:h0 + 2, :, tok].rearrange("h p t -> p h t"), srcoff=lambda b: b * 192)
                                    transp(2, 64, qrT[h0:h0 + 2, :, tok].rearrange("h p t -> p h t"), srcoff=lambda b: b * 192 + 128)
                                else:
                                    p.op("act", lambda e, pt=pt: e.copy(rs[:, 0:320], pt[:, 0:320]), [pk], ["rs" + x])
                                    rope(rs, 64, 1, 64, 16, 64, 256, tab, tk, 384, 448, r1, r2, rb, x)
                                    transp(1, 64, krT[:, tok].rearrange("p (a t) -> p a t", a=1), srcoff=lambda b: 256)
                                    p.op("dve", lambda e: e.tensor_tensor(r1[:, 0:256], rs[:, 0:256], rs[:, 0:256], ALU.mult), ["rs" + x], ["r1" + x])
                                    p.op("dve", lambda e: e.reduce_sum(ssq[:], r1[:, 0:256], axis=AX.X), ["r1" + x], ["ssq" + x])
                                    p.op("dve", lambda e: e.tensor_scalar(ssq[:], ssq[:], 1.0 / 256, EPS, ALU.mult, ALU.add), ["ssq" + x], ["ssq" + x])
                                    p.op("act", lambda e: e.activation(ssq[:], ssq[:], AF.Sqrt), ["ssq" + x], ["ssq" + x])
                                    p.op("dve", lambda e: e.reciprocal(ssq[:], ssq[:]), ["ssq" + x], ["ssq" + x])
                                    p.op("dve", lambda e: e.scalar_tensor_tensor(rb[:, 0:256], rs[:, 0:256], ssq[:, 0:1], gkv[:],
                                                                                 ALU.mult, ALU.mult), ["rs" + x, "ssq" + x, "gkv"], ["rb" + x])
                                    for b in range(2):
                                        p.op("pe", lambda e, b=b: e.transpose(psb[7][:, b * 128:(b + 1) * 128], rb[:, b * 128:(b + 1) * 128], identb[:]),
                                             ["rb" + x, "identb"], [psk[7]])
                                    p.op("act", lambda e: e.copy(cnT[:].rearrange("p a b -> p (a b)"), psb[7][:, 0:256]), [psk[7]], ["cnT" + x])
                                    bk2 = mmc[0] % 4
                                    mmc[0] += 1
                                    for h in range(4):
                                        for rc in range(2):
                                            p.op("pe", lambda e, h=h, rc=rc, bk2=bk2: e.matmul(
                                                ps[bk2][:, h * 128:(h + 1) * 128], wuk[:, rc, h * 128:(h + 1) * 128], cnT[:, rc, :],
                                                start=(rc == 0), stop=(rc == 1)), ["wuk", "cnT" + x], [psk[bk2]])
                                    p.op("act", lambda e, bk2=bk2: e.copy(tT[:, 0:4, :].rearrange("p a b -> p (a b)"), ps[bk2][:, 0:512]), [psk[bk2]], ["tT" + x])
                                    p.dma("sp", knT[:, :, tok].rearrange("h p t -> p h t"), tT[:, 0:4, :], reads=["tT" + x], writes=[])
                                    bk3 = mmc[0] % 4
                                    mmc[0] += 1
                                    for rc in range(2):
                                        p.op("pe", lambda e, rc=rc, bk3=bk3: e.matmul(
                                            ps[bk3][:, 0:512], cnT[:, rc, :], wuv[:, rc, :], start=(rc == 0), stop=(rc == 1)),
                                            ["wuv", "cnT" + x], [psk[bk3]])
                                    p.op("act", lambda e, bk3=bk3: e.copy(rb[:, 0:512], ps[bk3][:, 0:512]), [psk[bk3]], ["rb" + x])
                                    p.dma("sp", vm[tok, :], rb[:, 0:512], reads=["rb" + x], writes=[])
                            pcnt[0] += 1
                            if pending[0] is not None:
                                pending[0]()
                            pending[0] = post
                    if pending[0] is not None:
                        pending[0]()
                        pending[0] = None
                p.flush()
            if "P2" in dbg:
                break
            LN2 = math.log(2.0)
            import os
            SKIP = os.environ.get("KSKIP", "").split(",")
            with ExitStack() as ph:
              if "P3" not in SKIP:
                  kT = SB("kT_s", [128, T], BF16, ph)
                  vv = SB("v_s", [128, NT, 128], BF16, ph)
                  q3 = [SB("q3_%d" % i, [128, 3, 128], BF16, ph) for i in range(2)]
                  PT = [SB("PT%d" % i, [128, 384], BF16, ph) for i in range(2)]
                  sink6 = SB("sink6", [1, 6], F32, ph)
                  esrow = SB("esrow", [1, 768], BF16, ph)
                  rec = SB("rec", [128, 384], F32, ph)
                  oT = SB("oT", [128, 384], BF16, ph)
                  p.dma("sp", sink6[:], swa_sink[l:l + 1, :], writes=["sink6"])
                  p.op("act", lambda e: e.activation(sink6[:], sink6[:], AF.Exp), ["sink6"], ["sink6"])
                  for h in range(6):
                      p.op("dve", lambda e, h=h: e.tensor_scalar(esrow[0:1, h * 128:(h + 1) * 128], onesf[0:1, 0:128],
                                                                 sink6[0:1, h:h + 1], None, ALU.mult), ["sink6", "onesf"], ["esrow"])
                  qi = 0
                  pi = 0
                  for hk in range(2):
                      p.dma("sp", kT[:], kTs[hk], writes=["kT"])
                      p.dma("sp", vv[:], vs[:, hk * 128:(hk + 1) * 128].rearrange("(n p) d -> p n d", p=128), writes=["vv"])
                      for t in range(NT):
                          keys = [(0, None), (1, None)]
                          if t >= 2:
                              n = t - 2
                              if n > 0:
                                  keys.append((t - 1, mge))
                              keys.append((t, None))
                              if n < NLT - 1:
                                  keys.append((t + 1, mle))
                          q = q3[qi % 2]
                          qk = "q3_%d" % (qi % 2)
                          dn, nm = (2, 3) if qi % 2 == 0 else (4, 5)
                          qi += 1
                          tok = slice(t * 128, (t + 1) * 128)
                          p.dma("sp", q[:], qTs[hk * 3:(hk + 1) * 3, :, tok].rearrange("h p t -> p h t"), writes=[qk])
                          pend_ = []
                          for ki, (kt, mk) in enumerate(keys):
                              sb_ = pi % 2
                              P_ = PT[pi % 2]
                              Pk = "PT%d" % (pi % 2)
                              pi += 1
                              p.op("pe", lambda e, sb_=sb_, kt=kt, q=q: e.matmul(
                                  ps[sb_][:, 0:384], kT[:, kt * 128:(kt + 1) * 128], q[:].rearrange("p a b -> p (a b)"),
                                  start=True, stop=True), ["kT", qk], [psk[sb_]])
                              p.op("act", lambda e, sb_=sb_, P_=P_: e.activation(P_[:], ps[sb_][:, 0:384], AF.Exp), [psk[sb_]], [Pk])
                              if mk is not None:
                                  p.op("dve", lambda e, P_=P_, mk=mk: e.tensor_tensor(
                                      V(P_, 0, 128, [128, 3], [1, 128]), V(P_, 0, 128, [128, 3], [1, 128]),
                                      V(mk, 0, 128, [0, 3], [1, 128]), ALU.mult), [Pk, "mle", "mge"], [Pk])

                              def fin(P_=P_, Pk=Pk, ki=ki, kt=kt, dn=dn, nm=nm, last=(ki == len(keys) - 1)):
                                  p.op("pe", lambda e: e.matmul(ps[dn][:, 0:384], onesb[:], P_[:], start=(ki == 0), stop=False),
                                       [Pk, "onesb"], [psk[dn]])
                                  p.op("pe", lambda e: e.matmul(ps[nm][:, 0:384], vv[:, kt, :], P_[:], start=(ki == 0), stop=last),
                                       [Pk, "vv"], [psk[nm]])
                              for f_ in pend_:
                                  f_()
                              pend_ = [fin]
                          for f_ in pend_:
                              f_()
                          p.op("pe", lambda e, dn=dn, hk=hk: e.matmul(
                              ps[dn][:, 0:384], onesb[0:1, :], esrow[0:1, hk * 384:(hk + 1) * 384], start=False, stop=True),
                              ["esrow", "onesb"], [psk[dn]])
                          p.op("dve", lambda e, dn=dn: e.reciprocal(rec[:], ps[dn][:, 0:384]), [psk[dn]], ["rec"])
                          p.op("dve", lambda e, nm=nm: e.tensor_tensor(oT[:], ps[nm][:, 0:384], rec[:], ALU.mult), [psk[nm], "rec"], ["oT"])
                          p.dma("sp", catT[hk * 3:(hk + 1) * 3, :, tok].rearrange("h p t -> p h t"),
                                oT[:].rearrange("p (a b) -> p a b", b=128), reads=["oT"])
                  p.flush()

            with ExitStack() as ph:
              if "P5" not in SKIP:
                  knS = SB("knS", [128, T], BF16, ph)
                  krS = SB("krS", [64, T], BF16, ph)
                  vS = SB("vS", [128, NT, 128], BF16, ph)
                  qn = [SB("qn%d" % i, [128, 512], BF16, ph) for i in range(2)]
                  qr = [SB("qr%d" % i, [64, 512], BF16, ph) for i in range(2)]
                  PT = [SB("PTm%d" % i, [128, 512], BF16, ph) for i in range(2)]
                  rec = SB("recm", [128, 512], F32, ph)
                  oT = SB("oTm", [128, 512], BF16, ph)
                  p.dma("sp", krS[:], krT, writes=["krS"])
                  groups = [(0, 2, [0, 1])] + [(g0, min(4, NT - g0), list(range(NT))) for g0 in range(2, NT, 4)]
                  qi = 0
                  pi = 0
                  for h in range(4):
                      p.dma("sp", knS[:], knT[h], writes=["knS"])
                      p.dma("sp", vS[:], vm[:, h * 128:(h + 1) * 128].rearrange("(n p) d -> p n d", p=128), writes=["vS"])
                      for (g0, ng, kblks) in groups:
                          N = ng * 128
                          cols = slice(g0 * 128, g0 * 128 + N)
                          qn_, qr_ = qn[qi % 2], qr[qi % 2]
                          qk = "qm%d" % (qi % 2)
                          dn, nm = (2, 3) if qi % 2 == 0 else (4, 5)
                          qi += 1
                          p.dma("sp", qn_[:, 0:N], qnT[h][:, cols], writes=[qk])
                          p.dma("sp", qr_[:, 0:N], qrT[h][:, cols], writes=[qk])
                          pend_ = []
                          for ki, kt in enumerate(kblks):
                              sb_ = pi % 2
                              P_ = PT[pi % 2]
                              Pk = "PTm%d" % (pi % 2)
                              pi += 1
                              p.op("pe", lambda e, sb_=sb_, kt=kt, qn_=qn_, N=N: e.matmul(
                                  ps[sb_][:, 0:N], knS[:, kt * 128:(kt + 1) * 128], qn_[:, 0:N], start=True, stop=False),
                                  ["knS", qk], [psk[sb_]])
                              p.op("pe", lambda e, sb_=sb_, kt=kt, qr_=qr_, N=N: e.matmul(
                                  ps[sb_][:, 0:N], krS[:, kt * 128:(kt + 1) * 128], qr_[:, 0:N], start=False, stop=True),
                                  ["krS", qk], [psk[sb_]])
                              p.op("act", lambda e, sb_=sb_, P_=P_, N=N: e.activation(P_[:, 0:N], ps[sb_][:, 0:N], AF.Exp), [psk[sb_]], [Pk])

                              def fin(P_=P_, Pk=Pk, ki=ki, kt=kt, dn=dn, nm=nm, N=N, last=(ki == len(kblks) - 1)):
                                  p.op("pe", lambda e: e.matmul(ps[dn][:, 0:N], onesb[:], P_[:, 0:N], start=(ki == 0), stop=last),
                                       [Pk, "onesb"], [psk[dn]])
                                  p.op("pe", lambda e: e.matmul(ps[nm][:, 0:N], vS[:, kt, :], P_[:, 0:N], start=(ki == 0), stop=last),
                                       [Pk, "vS"], [psk[nm]])
                              for f_ in pend_:
                                  f_()
                              pend_ = [fin]
                          for f_ in pend_:
                              f_()
                          p.op("dve", lambda e, dn=dn, N=N: e.reciprocal(rec[:, 0:N], ps[dn][:, 0:N]), [psk[dn]], ["recm"])
                          p.op("dve", lambda e, nm=nm, N=N: e.tensor_tensor(oT[:, 0:N], ps[nm][:, 0:N], rec[:, 0:N], ALU.mult),
                               [psk[nm], "recm"], ["oTm"])
                          p.dma("sp", catT[12 + h][:, cols], oT[:, 0:N], reads=["oTm"])
                  p.flush()

            with ExitStack() as ph:
              if "P4" not in SKIP:
                  lgb = SB("lgb", [128, 2, 6], F32, ph)
                  nlgb = SB("nlgb", [128, 2, 6], F32, ph)
                  lgcol = SB("lgcol", [128, 2, 3], F32, ph)
                  maskT = SB("maskT", [128, 2, 6, 128], F32, ph)
                  mtmp = SB("mtmp", [128, 128], F32, ph)
                  qdec = SB("qdec", [64, 2, 6, 128], F32, ph)
                  kdf = SB("kdf", [128, 2, 6], F32, ph)
                  gc = SB("gc", [64, 2, 6], F32, ph)
                  gCt = SB("gCt", [64, 2, 6, 128], F32, ph)
                  S = SB("S", [64, 6, 128], F32, ph)
                  Stmp = SB("Stmp", [64, 6, 128], F32, ph)
                  Sb = SB("Sb", [64, 6, 128], BF16, ph)
                  qt = [SB("qt%d" % i, [64, 6, 128], BF16, ph) for i in range(2)]
                  ktT = [SB("ktT%d" % i, [64, 6, 128], BF16, ph) for i in range(2)]
                  ktm = [SB("ktm%d" % i, [128, 384], BF16, ph) for i in range(2)]
                  vt = [SB("vt%d" % i, [128, 768], BF16, ph) for i in range(2)]
                  gt = [SB("gt%d" % i, [128, 768], F32, ph) for i in range(2)]
                  ra = [SB("ra%d" % i, [128, 768], F32, ph) for i in range(2)]
                  PTr = SB("PTr", [128, 6, 128], BF16, ph)
                  qd = SB("qd", [64, 6, 128], BF16, ph)
                  kd = SB("kd", [128, 6, 64], BF16, ph)
                  st6 = SB("st6", [128, 6, 6], F32, ph)
                  mv6 = SB("mv6", [128, 6, 2], F32, ph)
                  rs6 = SB("rs6", [128, 6], F32, ph)
                  y1 = SB("y1", [128, 6, 128], F32, ph)
                  y2 = SB("y2", [128, 6, 128], F32, ph)
                  yb = SB("yb", [128, 768], BF16, ph)
                  tT2 = SB("tT2", [128, 6, 128], BF16, ph)
                  racc = dscr("racc%d" % l, [T, 768])
                  p.dma("sp", lgb[:].rearrange("p a b -> p (a b)"),
                        ret_decay[l:l + 1].rearrange("o a b -> o (a b)").to_broadcast([128, 12]), writes=["lgb"])
                  p.op("act", lambda e: e.activation(lgb[:], lgb[:], AF.Exp, scale=-LN2), ["lgb"], ["lgb"])
                  p.op("act", lambda e: e.activation(lgb[:], lgb[:], AF.Ln, scale=-1.0, bias=1.0), ["lgb"], ["lgb"])
                  p.op("dve", lambda e: e.tensor_scalar(nlgb[:], lgb[:], -1.0, None, ALU.mult), ["lgb"], ["nlgb"])
                  for dr in range(2):
                      for j in range(3):
                          for hh in range(2):
                              p.op("dve", lambda e, dr=dr, j=j, hh=hh: e.tensor_copy(
                                  lgcol[hh * 64:(hh + 1) * 64, dr, j:j + 1], lgb[hh * 64:(hh + 1) * 64, dr, 2 * j + hh:2 * j + hh + 1]),
                                  ["lgb"], ["lgcol"])
                  for dr in range(2):
                      for h in range(6):
                          src = lgb if dr == 0 else nlgb
                          p.op("act", lambda e, dr=dr, h=h, src=src: e.activation(mtmp[:], cst[:, 2, :], AF.Exp, scale=src[:, dr, h:h + 1]),
                               ["cst", "lgb", "nlgb"], ["mtmp"])
                          p.op("dve", lambda e, dr=dr, h=h: e.tensor_tensor(maskT[:, dr, h, :], mtmp[:], cst[:, dr, :], ALU.mult),
                               ["mtmp", "cst"], ["maskT"])
                      for h in range(6):
                          p.op("act", lambda e, dr=dr, h=h: e.activation(qdec[:, dr, h, :], cst[0:64, 3 + dr, :], AF.Exp,
                                                                         scale=lgb[0:64, dr, h:h + 1]), ["cst", "lgb"], ["qdec"])
                      p.op("dve", lambda e, dr=dr: e.tensor_scalar(kdf[:, dr, :], lgb[:, dr, :], scol[:, dr:dr + 1], None, ALU.mult),
                           ["lgb", "scol"], ["kdf"])
                      p.op("act", lambda e, dr=dr: e.activation(kdf[:, dr, :], kdf[:, dr, :], AF.Exp), ["kdf"], ["kdf"])
                      p.op("act", lambda e, dr=dr: e.activation(gc[:, dr, :], lgb[0:64, dr, :], AF.Exp, scale=128.0), ["lgb"], ["gc"])
                      p.op("dve", lambda e, dr=dr: e.tensor_copy(gCt[:, dr], V(gc, dr * 6, 64, [1, 6], [0, 128])), ["gc"], ["gCt"])
                  bi = 0
                  RS = int(os.environ.get("RS", "9"))
                  for dr in range(2 if RS > 0 else 0):
                      order = list(range(NT)) if dr == 0 else [1, 0] + list(range(NT - 1, 1, -1))
                      p.op("dve", lambda e: e.memset(S[:], 0.0), [], ["S"])
                      p.op("dve", lambda e: e.memset(Sb[:], 0.0), [], ["Sb"])
                      for t in order:
                          tok = slice(t * 128, (t + 1) * 128)
                          b_ = bi % 2
                          bi += 1
                          qt_, kt_, km_, vt_, gt_, ra_ = qt[b_], ktT[b_], ktm[b_], vt[b_], gt[b_], ra[b_]
                          bk = "rin%d" % b_
                          p.dma("sp", qt_[:], qTr.rearrange("j (hh d) t -> d (j hh) t", d=64)[:, :, tok], writes=[bk + "q"])
                          p.dma("sp", kt_[:], kTr.rearrange("j (hh d) t -> d (j hh) t", d=64)[:, :, tok], writes=[bk + "k"])
                          p.dma("sp", km_[:], kr[tok, :], writes=[bk + "km"])
                          p.dma("sp", vt_[:], vr[tok, :], writes=[bk + "v"])
                          p.dma("sp", gt_[:], gfb[dr, tok, :], writes=[bk + "g"])
                          if dr == 1:
                              p.dma("sp", ra_[:], racc[tok, :], reads=["racc%d" % t], writes=[bk + "ra"])
                          psS = lambda h: ps[0][:, h * 128:(h + 1) * 128] if h < 4 else ps[1][:, (h - 4) * 128:(h - 3) * 128]
                          psY = lambda h: ps[2][:, h * 128:(h + 1) * 128] if h < 4 else ps[3][:, (h - 4) * 128:(h - 3) * 128]
                          kS = lambda h: psk[0] if h < 4 else psk[1]
                          kY = lambda h: psk[2] if h < 4 else psk[3]
                          for h in range(6):
                              j, hh = h // 2, h % 2
                              p.op("pe", lambda e, h=h, j=j, hh=hh, kt_=kt_, qt_=qt_, psS=psS: e.matmul(
                                  psS(h), kt_[:, h, :], qt_[:, h, :], start=True, stop=True),
                                  [bk + "q", bk + "k"], [kS(h)])
                          p.op("dve", lambda e, dr=dr: e.tensor_tensor(
                              PTr[:, 0:4, :], ps[0][:, 0:512].rearrange("p (a b) -> p a b", b=128), maskT[:, dr, 0:4, :], ALU.mult),
                              [psk[0], "maskT"], ["PTr"])
                          p.op("dve", lambda e, dr=dr: e.tensor_tensor(
                              PTr[:, 4:6, :], ps[1][:, 0:256].rearrange("p (a b) -> p a b", b=128), maskT[:, dr, 4:6, :], ALU.mult),
                              [psk[1], "maskT"], ["PTr"])
                          if RS < 2:
                              continue
                          p.op("pool", lambda e, dr=dr, qt_=qt_: e.tensor_tensor(qd[:], qt_[:], qdec[:, dr], ALU.mult), [bk + "q", "qdec"], ["qd"])
                          p.op("pool", lambda e, dr=dr, km_=km_: e.tensor_tensor(
                              kd[:], km_[:].rearrange("p (a b) -> p a b", b=64), V(kdf, dr * 6, 128, [1, 6], [0, 64]), ALU.mult),
                              [bk + "km", "kdf"], ["kd"])
                          for h in range(6):
                              j, hh = h // 2, h % 2
                              p.op("pe", lambda e, h=h, vt_=vt_, psY=psY: e.matmul(
                                  psY(h), PTr[:, h, :], vt_[:, h * 128:(h + 1) * 128], start=True, stop=False), ["PTr", bk + "v"], [kY(h)])
                              p.op("pe", lambda e, h=h, j=j, hh=hh, psY=psY: e.matmul(
                                  psY(h), qd[:, h, :], Sb[:, h, :],
                                  start=False, stop=True), ["qd", "Sb"], [kY(h)])
                          if RS < 3:
                              continue
                          for h in range(6):
                              ub, uo = (4, h * 128) if h < 4 else (5, (h - 4) * 128)
                              p.op("pe", lambda e, h=h, ub=ub, uo=uo, vt_=vt_: e.matmul(
                                  ps[ub][0:64, uo:uo + 128], kd[:, h, :], vt_[:, h * 128:(h + 1) * 128], start=True, stop=True), ["kd", bk + "v"], [psk[ub]])
                          p.op("dve", lambda e, dr=dr: e.tensor_tensor(Stmp[:], S[:], gCt[:, dr], ALU.mult), ["S", "gCt"], ["Stmp"])
                          p.op("dve", lambda e: e.tensor_tensor(S[:, 0:4, :], Stmp[:, 0:4, :],
                                                                ps[4][0:64, 0:512].rearrange("p (a b) -> p a b", b=128), ALU.add), ["Stmp", psk[4]], ["S"])
                          p.op("dve", lambda e: e.tensor_tensor(S[:, 4:6, :], Stmp[:, 4:6, :],
                                                                ps[5][0:64, 0:256].rearrange("p (a b) -> p a b", b=128), ALU.add), ["Stmp", psk[5]], ["S"])
                          p.op("act", lambda e: e.copy(Sb[:], S[:]), ["S"], ["Sb"])
                          if RS < 4:
                              continue
                          for h in range(6):
                              p.op("dve", lambda e, h=h, psY=psY: e.bn_stats(st6[:, h, :], psY(h)), [kY(h)], ["st6"])
                          for h in range(6):
                              p.op("dve", lambda e, h=h: e.bn_aggr(mv6[:, h, :], st6[:, h, :]), ["st6"], ["mv6"])
                          p.op("dve", lambda e: e.tensor_scalar(rs6[:], V(mv6, 1, 128, [2, 6]), EPS, None, ALU.add), ["mv6"], ["rs6"])
                          p.op("act", lambda e: e.activation(rs6[:], rs6[:], AF.Sqrt), ["rs6"], ["rs6"])
                          p.op("dve", lambda e: e.reciprocal(rs6[:], rs6[:]), ["rs6"], ["rs6"])
                          p.op("dve", lambda e: e.tensor_tensor(y1[:, 0:4, :], ps[2][:, 0:512].rearrange("p (a b) -> p a b", b=128),
                                                                V(mv6, 0, 128, [2, 4], [0, 128]), ALU.subtract), [psk[2], "mv6"], ["y1"])
                          p.op("dve", lambda e: e.tensor_tensor(y1[:, 4:6, :], ps[3][:, 0:256].rearrange("p (a b) -> p a b", b=128),
                                                                V(mv6, 8, 128, [2, 2], [0, 128]), ALU.subtract), [psk[3], "mv6"], ["y1"])
                          p.op("pool", lambda e: e.tensor_tensor(y2[:], y1[:], V(rs6, 0, 128, [1, 6], [0, 128]), ALU.mult), ["y1", "rs6"], ["y2"])
                          p.op("pool", lambda e, gt_=gt_: e.tensor_tensor(y1[:].rearrange("p a b -> p (a b)"),
                                                                          y2[:].rearrange("p a b -> p (a b)"), gt_[:], ALU.mult),
                               ["y2", bk + "g"], ["y1"])
                          if RS < 5:
                              continue
                          if dr == 0:
                              p.dma("sp", racc[tok, :], y1[:].rearrange("p a b -> p (a b)"), reads=["y1"], writes=["racc%d" % t])
                          else:
                              p.op("dve", lambda e, ra_=ra_: e.tensor_tensor(yb[:], y1[:].rearrange("p a b -> p (a b)"), ra_[:], ALU.add),
                                   ["y1", bk + "ra"], ["yb"])
                              for h in range(6):
                                  p.op("pe", lambda e, h=h: e.transpose(psb[6][:, h * 128:(h + 1) * 128], yb[:, h * 128:(h + 1) * 128], identb[:]),
                                       ["yb", "identb"], [psk[6]])
                              p.op("act", lambda e: e.copy(tT2[:].rearrange("p a b -> p (a b)"), psb[6][:, 0:768]), [psk[6]], ["tT2"])
                              p.dma("sp", catT[6:12, :, tok].rearrange("h p t -> p h t"), tT2[:], reads=["tT2"])
                  p.flush()
            if "P5" in dbg:
                break

            with ExitStack() as ph:
                wo = SB("wo", [128, 16, D], BF16, ph)
                g1 = [SB("g1_%d" % m, [128, D], F32, ph) for m in range(2)]
                lnG = SB("lnG", [128, D], F32, ph)
                lnB = SB("lnB", [128, D], F32, ph)
                cT = [SB("cT%d" % i, [128, 16, 128], BF16, ph) for i in range(2)]
                xt2 = [SB("xo%d" % i, [128, D], F32, ph) for i in range(2)]
                yg = SB("yg", [128, D], F32, ph)
                B6 = dict(stt=SB("stt6", [128, 4, 6], F32, ph), mv=SB("mvo", [128, 2], F32, ph),
                          rstd=SB("rstdo", [128, 1], F32, ph), nmr=SB("nmro", [128, 1], F32, ph))
                wov = w_out[l].rearrange("(kc p) n -> p kc n", p=128)
                for q in range(4):
                    p.dma("pool", wo[:, q * 4:(q + 1) * 4, :], wov[:, q * 4:(q + 1) * 4, :], writes=["wo"])
                for m in range(2):
                    p.dma("sp", g1[m][:], gsc[l, 0, m:m + 1, :].to_broadcast([128, D]), writes=["g1"])
                p.dma("sp", lnG[:], ln_g[0][l:l + 1, :].to_broadcast([128, D]), writes=["lnG"])
                p.dma("sp", lnB[:], ln_b[0][l:l + 1, :].to_broadcast([128, D]), writes=["lnB"])
                for t in range(NT):
                    tok = slice(t * 128, (t + 1) * 128)
                    m = 1 if t < 2 else 0
                    c_, ck = cT[t % 2], "cT%d" % (t % 2)
                    x_, xk = xt2[t % 2], "xo%d" % (t % 2)
                    p.dma("sp", c_[:], catT[:, :, tok].rearrange("k p t -> p k t"), writes=[ck])
                    p.dma("sp", x_[:], blk_src(l, t), reads=["xres%d" % t], writes=[xk])
                    for oc in range(4):
                        for kc in range(16):
                            p.op("pe", lambda e, oc=oc, kc=kc, c_=c_: e.matmul(
                                ps[oc][:, :], c_[:, kc, :], wo[:, kc, oc * 512:(oc + 1) * 512], start=(kc == 0), stop=(kc == 15)),
                                [ck, "wo"], [psk[oc]])
                    for oc in range(4):
                        p.op("dve", lambda e, oc=oc, m=m: e.tensor_tensor(yg[:, oc * 512:(oc + 1) * 512], ps[oc][:, :],
                                                                          g1[m][:, oc * 512:(oc + 1) * 512], ALU.mult),
                             [psk[oc], "g1"], ["yg"])
                    p.op("dve", lambda e, x_=x_: e.scalar_tensor_tensor(yg[:], x_[:], ALPHA, yg[:], ALU.mult, ALU.add), [xk, "yg"], ["yg"])
                    ln_stats(yg, "yg", B6["stt"], B6["mv"], B6["rstd"], B6["nmr"], "o")
                    p.op("act", lambda e, x_=x_: e.activation(x_[:], yg[:], AF.Identity, bias=B6["nmr"][:], scale=B6["rstd"][:]),
                         ["yg", "onmr", "orstd"], [xk])
                    p.op("dve", lambda e, x_=x_: e.tensor_tensor(x_[:], x_[:], lnG[:], ALU.mult), [xk, "lnG"], [xk])
                    p.op("pool", lambda e, x_=x_: e.tensor_tensor(x_[:], x_[:], lnB[:], ALU.add), [xk, "lnB"], [xk])
                    p.dma("sp", xres[tok, :], x_[:], reads=[xk], writes=["xres%d" % t])
                p.flush()
            if "P6" in dbg:
                break
            NB = 2 * NT + NE
            if l == 0:
                jv_in = din("jv", [128, NB])
                pidx_in = din("pidx", [128, 1])
                h2d = dscr("h2d", [T, D], BF16)
                xslots = dscr("xslots", [NB * 128, D], BF16)
                yslots = dscr("yslots", [NB * 128, D])
                dest_i = SB("dest_i", [128, NT, 2], I32)
                gatew = SB("gatew", [128, NT, 2], F32)
                offs_i = SB("offs_i", [128, NB], I32)
                breg = st.enter_context(nc.gpsimd.register("bndreg"))
                nc.gpsimd.reg_mov(breg, DEPTH * NE * 128 - 1)
                bnd_val = [nc.gpsimd.snap(breg)]
            with ExitStack() as ph:
                B = dict(xt=[SB("xt%d" % i, [128, D], F32, ph) for i in range(2)], xn=SB("xn", [128, D], F32, ph),
                         stt=SB("stt", [128, 4, 6], F32, ph), mv=SB("mv", [128, 2], F32, ph),
                         rstd=SB("rstd", [128, 1], F32, ph), nmr=SB("nmr", [128, 1], F32, ph))
                scb = [SB("scb%d" % m, [128, D], F32, ph) for m in range(2)]
                shb = [SB("shb%d" % m, [128, D], F32, ph) for m in range(2)]
                h2 = SB("h2", [128, D], F32, ph)
                h2b = [SB("h2b%d" % i, [128, D], BF16, ph) for i in range(2)]
                h2T = SB("h2T", [128, 16, 128], F32, ph)
                wr = SB("wr", [128, 16, 36], F32, ph)
                brt = SB("brt", [128, 36], F32, ph)
                lg = SB("lg", [128, 36], F32, ph)
                sm = SB("sm", [128, 16], F32, ph)
                ohg = SB("ohg", [128, 4], F32, ph)
                ge = SB("ge", [128, 4], F32, ph)
                ein = SB("ein", [128, 8], F32, ph)
                ein2 = SB("ein2", [128, 8], F32, ph)
                oh8 = SB("oh8", [128, 2, 8], F32, ph)
                ohall = SB("ohall", [128, NT, 2, 32], F32, ph)
                At = SB("At", [128, 32], BF16, ph)
                Us = SB("Us", [128, 128], BF16, ph)
                base = SB("base", [128, 32], F32, ph)
                rank = SB("rank", [128, NT, 32], F32, ph)
                tmpr = SB("tmpr", [128, NT, 32], F32, ph)
                cs = [SB("cs%d" % i, [128, 32], F32, ph) for i in range(2)]
                padd = SB("padd", [128, 32], F32, ph)
                pst = SB("pst", [128, 32], F32, ph)
                dest_f = SB("dest_f", [128, NT, 2], F32, ph)
                jv = SB("jv", [128, NB], F32, ph)
                pidx = SB("pidx", [128, 1], F32, ph)
                cmp_ = SB("cmp", [128, NB, 32], F32, ph)
                be = SB("be", [128, NB], F32, ph)
                same = SB("same", [128, NB], F32, ph)
                zt = SB("zt", [128, D], BF16, ph)
                p.dma("sp", jv[:], jv_in, writes=["jv"])
                p.dma("sp", pidx[:], pidx_in, writes=["pidx"])
                p.op("dve", lambda e: e.tensor_copy(Us[:], cst[:, 5, :]), ["cst"], ["Us"])
                p.op("dve", lambda e: e.memset(base[:], 0.0), [], ["base"])
                p.op("pool", lambda e: e.memset(zt[:], 0.0), [], ["zt"])
                for j in range(NB):
                    p.dma("sp", xslots[j * 128:(j + 1) * 128, :], zt[:], reads=["zt"], writes=["xslots"])
                for m in range(2):
                    p.dma("sp", shb[m][:], gsc[l, 2, m:m + 1, :].to_broadcast([128, D]), writes=["shb"])
                    p.dma("sp", scb[m][:], gsc[l, 3, m:m + 1, :].to_broadcast([128, D]), writes=["scb"])
                p.dma("sp", wr[:, :, 0:4], w_grp[l].rearrange("(kc p) n -> p kc n", p=128), writes=["wr"])
                p.dma("sp", wr[:, :, 4:36], w_exp[l].rearrange("(kc p) n -> p kc n", p=128), writes=["wr"])
                p.dma("sp", brt[:, 0:4], b_grp[l:l + 1, :].to_broadcast([128, 4]), writes=["brt"])
                p.dma("sp", brt[:, 4:36], b_exp[l:l + 1, :].to_broadcast([128, 32]), writes=["brt"])
                for t in range(NT):
                    tok = slice(t * 128, (t + 1) * 128)
                    m = 1 if t < 2 else 0
                    xt, xk = B["xt"][t % 2], "xt%d" % (t % 2)
                    hb, hbk = h2b[t % 2], "h2b%d" % (t % 2)
                    p.dma("sp", xt[:], xres[tok, :], writes=[xk])
                    ln_stats(xt, xk, B["stt"], B["mv"], B["rstd"], B["nmr"], "l")
                    xn = B["xn"]
                    p.op("act", lambda e, xt=xt: e.activation(xn[:], xt[:], AF.Identity, bias=B["nmr"][:], scale=B["rstd"][:]),
                         [xk, "lnmr", "lrstd"], ["xn"])
                    p.op("dve", lambda e, m=m: e.tensor_tensor(h2[:], xn[:], scb[m][:], ALU.mult), ["xn", "scb"], ["h2"])
                    p.op("pool", lambda e, m=m: e.tensor_tensor(h2[:], h2[:], shb[m][:], ALU.add), ["h2", "shb"], ["h2"])
                    p.op("act", lambda e, hb=hb: e.copy(hb[:], h2[:]), ["h2"], [hbk])
                    p.dma("sp", h2d[tok, :], hb[:], reads=[hbk], writes=["h2d%d" % t])
                    for q in range(4):
                        bk = 4 + q
                        for k4 in range(4):
                            kc = q * 4 + k4
                            p.op("pe", lambda e, kc=kc, k4=k4, bk=bk: e.transpose(
                                ps[bk][:, k4 * 128:(k4 + 1) * 128], h2[:, kc * 128:(kc + 1) * 128], ident[:]), ["h2", "ident"], [psk[bk]])
                        p.op("act" if q % 2 else "dve", lambda e, q=q, bk=bk: e.tensor_copy(
                            h2T[:, q * 4:(q + 1) * 4, :].rearrange("p a b -> p (a b)"), ps[bk][:, :]) if q % 2 == 0 else e.copy(
                            h2T[:, q * 4:(q + 1) * 4, :].rearrange("p a b -> p (a b)"), ps[bk][:, :]), [psk[bk]], ["h2T"])
                    for kc in range(16):
                        p.op("pe", lambda e, kc=kc: e.matmul(ps[0][:, 0:36], h2T[:, kc, :], wr[:, kc, :], start=(kc == 0), stop=(kc == 15)),
                             ["h2T", "wr"], [psk[0]])
                    p.op("dve", lambda e: e.tensor_tensor(lg[:], ps[0][:, 0:36], brt[:], ALU.add), [psk[0], "brt"], ["lg"])
                    D_ = lambda fn, r, w: p.op("dve", fn, r, w)
                    D_(lambda e: e.reduce_max(sm[:, 0:1], lg[:, 0:4], axis=AX.X), ["lg"], ["sm"])
                    D_(lambda e: e.tensor_scalar(ohg[:], lg[:, 0:4], sm[:, 0:1], None, ALU.is_equal), ["lg", "sm"], ["ohg"])
                    D_(lambda e: e.tensor_scalar(sm[:, 1:2], sm[:, 0:1], -1.0, None, ALU.mult), ["sm"], ["sm"])
                    p.op("act", lambda e: e.activation(ge[:], lg[:, 0:4], AF.Exp, bias=sm[:, 1:2], scale=1.0), ["lg", "sm"], ["ge"])
                    D_(lambda e: e.reduce_sum(sm[:, 2:3], ge[:], axis=AX.X), ["ge"], ["sm"])
                    D_(lambda e: e.reciprocal(sm[:, 3:4], sm[:, 2:3]), ["sm"], ["sm"])
                    D_(lambda e: e.tensor_scalar(ein[:], lg[:, 4:12], ohg[:, 0:1], None, ALU.mult), ["lg", "ohg"], ["ein"])
                    for g in range(1, 4):
                        D_(lambda e, g=g: e.scalar_tensor_tensor(ein[:], lg[:, 4 + g * 8:12 + g * 8], ohg[:, g:g + 1], ein[:], ALU.mult, ALU.add),
                           ["lg", "ohg", "ein"], ["ein"])
                    D_(lambda e: e.reduce_max(sm[:, 4:5], ein[:], axis=AX.X), ["ein"], ["sm"])
                    D_(lambda e: e.tensor_scalar(oh8[:, 0, :], ein[:], sm[:, 4:5], None, ALU.is_equal), ["ein", "sm"], ["oh8"])
                    D_(lambda e: e.scalar_tensor_tensor(ein2[:], oh8[:, 0, :], -1e30, ein[:], ALU.mult, ALU.add), ["oh8", "ein"], ["ein2"])
                    D_(lambda e: e.reduce_max(sm[:, 5:6], ein2[:], axis=AX.X), ["ein2"], ["sm"])
                    D_(lambda e: e.tensor_scalar(oh8[:, 1, :], ein2[:], sm[:, 5:6], None, ALU.is_equal), ["ein2", "sm"], ["oh8"])
                    D_(lambda e: e.tensor_tensor(sm[:, 6:7], sm[:, 5:6], sm[:, 4:5], ALU.subtract), ["sm"], ["sm"])
                    p.op("act", lambda e: e.activation(sm[:, 7:8], sm[:, 6:7], AF.Exp), ["sm"], ["sm"])
                    D_(lambda e: e.tensor_scalar(sm[:, 8:9], sm[:, 7:8], 1.0, None, ALU.add), ["sm"], ["sm"])
                    D_(lambda e: e.reciprocal(sm[:, 9:10], sm[:, 8:9]), ["sm"], ["sm"])
                    D_(lambda e: e.tensor_tensor(sm[:, 10:11], sm[:, 7:8], sm[:, 9:10], ALU.mult), ["sm"], ["sm"])
                    D_(lambda e, t=t: e.tensor_tensor(gatew[:, t, 0:1], sm[:, 9:10], sm[:, 3:4], ALU.mult), ["sm"], ["gatew"])
                    D_(lambda e, t=t: e.tensor_tensor(gatew[:, t, 1:2], sm[:, 10:11], sm[:, 3:4], ALU.mult), ["sm"], ["gatew"])
                    for k in range(2):
                        for g in range(4):
                            D_(lambda e, t=t, k=k, g=g: e.tensor_scalar(ohall[:, t, k, g * 8:(g + 1) * 8], oh8[:, k, :], ohg[:, g:g + 1], None, ALU.mult),
                               ["oh8", "ohg"], ["ohall"])
                    D_(lambda e, t=t: e.tensor_tensor(At[:], ohall[:, t, 0, :], ohall[:, t, 1, :], ALU.add), ["ohall"], ["At"])
                    p.op("pe", lambda e: e.matmul(ps[1][:, 0:32], Us[:], At[:], start=True, stop=True), ["Us", "At"], [psk[1]])
                    p.op("pe", lambda e: e.matmul(ps[2][:, 0:32], onesb[:], At[:], start=True, stop=True), ["onesb", "At"], [psk[2]])
                    D_(lambda e, t=t: e.tensor_tensor(rank[:, t, :], ps[1][:, 0:32], base[:], ALU.add), [psk[1], "base"], ["rank"])
                    D_(lambda e: e.tensor_tensor(base[:], ps[2][:, 0:32], base[:], ALU.add), [psk[2], "base"], ["base"])
                D_(lambda e: e.tensor_tensor(V(cmp_, 0, 128, [NB, 32], [1, NB]), V(base, 0, 128, [1, 32], [0, NB]),
                                             V(jv, 0, 128, [0, 32], [1, NB]), ALU.is_gt), ["base", "jv"], ["cmp"])
                D_(lambda e: e.reduce_sum(padd[:], V(cmp_, 0, 128, [NB, 32], [1, NB]), axis=AX.X), ["cmp"], ["padd"])
                D_(lambda e: e.tensor_scalar(padd[:], padd[:], 128.0, None, ALU.mult), ["padd"], ["padd"])
                D_(lambda e: e.tensor_copy(cs[0][:], padd[:]), ["padd"], ["cs0"])
                cur = 0
                for sh in (1, 2, 4, 8, 16):
                    a, b = cs[cur], cs[1 - cur]
                    ak, bkk = "cs%d" % cur, "cs%d" % (1 - cur)
                    D_(lambda e, a=a, b=b: e.tensor_copy(b[:], a[:]), [ak], [bkk])
                    D_(lambda e, a=a, b=b, sh=sh: e.tensor_tensor(b[:, sh:32], a[:, sh:32], a[:, 0:32 - sh], ALU.add), [ak], [bkk])
                    cur = 1 - cur
                ends, ek = cs[cur], "cs%d" % cur
                D_(lambda e: e.tensor_tensor(pst[:], ends[:], padd[:], ALU.subtract), [ek, "padd"], ["pst"])
                D_(lambda e: e.tensor_tensor(rank[:], rank[:], V(pst, 0, 128, [0, NT], [1, 32]), ALU.add), ["rank", "pst"], ["rank"])
                for k in range(2):
                    D_(lambda e, k=k: e.tensor_tensor(tmpr[:], ohall[:, :, k, :], rank[:], ALU.mult), ["ohall", "rank"], ["tmpr"])
                    D_(lambda e, k=k: e.reduce_sum(dest_f[:, :, k], tmpr[:], axis=AX.X), ["tmpr"], ["dest_f"])
                D_(lambda e: e.tensor_copy(dest_i[:], dest_f[:]), ["dest_f"], ["dest_i"])
                D_(lambda e: e.tensor_tensor(cmp_[:], V(ends, 0, 128, [0, NB], [1, 32]), V(jv, 0, 128, [1, NB], [0, 32]), ALU.is_le),
                   [ek, "jv"], ["cmp"])
                D_(lambda e: e.reduce_sum(be[:], cmp_[:], axis=AX.X), ["cmp"], ["be"])
                D_(lambda e: e.tensor_scalar(be[:], be[:], 31.0, None, ALU.min), ["be"], ["be"])
                D_(lambda e: e.memset(same[:], 0.0), [], ["same"])
                D_(lambda e: e.tensor_tensor(same[:, 1:NB], be[:, 1:NB], be[:, 0:NB - 1], ALU.is_equal), ["be"], ["same"])
                D_(lambda e: e.tensor_scalar(be[:], be[:], 128.0, None, ALU.mult), ["be"], ["be"])
                D_(lambda e: e.scalar_tensor_tensor(be[:], same[:], 1.0e6, be[:], ALU.mult, ALU.add), ["be", "same"], ["be"])
                D_(lambda e: e.tensor_scalar(be[:], be[:], pidx[:, 0:1], float(l * NE * 128), ALU.add, ALU.add), ["be", "pidx"], ["be"])
                D_(lambda e: e.tensor_copy(offs_i[:], be[:]), ["be"], ["offs_i"])
                p.flush()
                for t in range(NT):
                    tok = slice(t * 128, (t + 1) * 128)
                    hb, hbk = h2b[t % 2], "h2b%d" % (t % 2)
                    p.dma("sp", hb[:], h2d[tok, :], reads=["h2d%d" % t], writes=[hbk])
                    for k in range(2):
                        p.op("pool", lambda e, t=t, k=k, hb=hb: e.indirect_dma_start(
                            out=xslots, out_offset=bass.IndirectOffsetOnAxis(ap=dest_i[:, t, k:k + 1], axis=0),
                            in_=hb[:], in_offset=None), [hbk, "dest_i", "xslots"], [], dma=True)
                p.flush()

            with ExitStack() as ph:
                Wg = SB("Wg", [128, 16, EH], BF16, ph)
                Wu = SB("Wu", [128, 16, EH], BF16, ph)
                Wd = SB("Wd", [128, 8, D], BF16, ph)
                xb = [SB("xb%d" % i, [128, D], BF16, ph) for i in range(2)]
                xT = SB("xT", [128, 16, 128], BF16, ph)
                sg = SB("sg", [128, EH], F32, ph)
                act_ = SB("act", [128, EH], BF16, ph)
                actT = SB("actT", [128, 8, 128], BF16, ph)
                yb = [SB("yb%d" % i, [128, D], F32, ph) for i in range(2)]
                wg_tab = w_gate.rearrange("l e (p kc) n -> (l e p) (kc n)", kc=16)
                wu_tab = w_up.rearrange("l e (p kc) n -> (l e p) (kc n)", kc=16)
                wd_tab = w_down.rearrange("l e (p hc) n -> (l e p) (hc n)", hc=8)
                for j in range(NB):
                    x_, xk = xb[j % 2], "xb%d" % (j % 2)
                    y_, yk = yb[j % 2], "yb%d" % (j % 2)
                    for (W_, tab, wk) in ((Wg, wg_tab, "Wg"), (Wu, wu_tab, "Wu"), (Wd, wd_tab, "Wd")):
                        p.op("pool", lambda e, W_=W_, tab=tab, j=j: e.indirect_dma_start(
                            out=W_[:].rearrange("p a b -> p (a b)"), out_offset=None, in_=tab,
                            in_offset=bass.IndirectOffsetOnAxis(ap=offs_i[:, j:j + 1], axis=0),
                            bounds_check=bnd_val[0], oob_is_err=False), ["offs_i"], [wk], dma=True)
                    p.dma("sp", x_[:], xslots[j * 128:(j + 1) * 128, :], writes=[xk])
                    for kc in range(16):
                        p.op("pe", lambda e, kc=kc, x_=x_: e.transpose(
                            psb[4 + kc // 8][:, (kc % 8) * 128:(kc % 8 + 1) * 128], V(x_, kc, 128, [16, 128]), identb[:]),
                            [xk, "identb"], [psk[4 + kc // 8]])
                    p.op("act", lambda e: e.copy(xT[:, 0:8, :].rearrange("p a b -> p (a b)"), psb[4][:, :]), [psk[4]], ["xT"])
                    p.op("dve", lambda e: e.tensor_copy(xT[:, 8:16, :].rearrange("p a b -> p (a b)"), psb[5][:, :]), [psk[5]], ["xT"])
                    for wi_, (W_, wk) in enumerate(((Wg, "Wg"), (Wu, "Wu"))):
                        for nh in range(2):
                            bk = wi_ * 2 + nh
                            for kc in range(16):
                                p.op("pe", lambda e, W_=W_, nh=nh, kc=kc, bk=bk: e.matmul(
                                    ps[bk][:, :], xT[:, kc, :], W_[:, kc, nh * 512:(nh + 1) * 512], start=(kc == 0), stop=(kc == 15)),
                                    ["xT", wk], [psk[bk]])
                    for nh in range(2):
                        p.op("act", lambda e, nh=nh: e.activation(sg[:, nh * 512:(nh + 1) * 512], ps[nh][:, :], AF.Silu), [psk[nh]], ["sg"])
                        p.op("dve", lambda e, nh=nh: e.tensor_tensor(act_[:, nh * 512:(nh + 1) * 512], sg[:, nh * 512:(nh + 1) * 512],
                                                                     ps[2 + nh][:, :], ALU.mult), ["sg", psk[2 + nh]], ["act"])
                    for hc in range(8):
                        p.op("pe", lambda e, hc=hc: e.transpose(psb[6][:, hc * 128:(hc + 1) * 128], V(act_, hc, 128, [8, 128]), identb[:]),
                             ["act", "identb"], [psk[6]])
                    p.op("act", lambda e: e.copy(actT[:].rearrange("p a b -> p (a b)"), psb[6][:, :]), [psk[6]], ["actT"])
                    for oc in range(4):
                        bk = oc
                        for hc in range(8):
                            p.op("pe", lambda e, oc=oc, hc=hc, bk=bk: e.matmul(
                                ps[bk][:, :], actT[:, hc, :], Wd[:, hc, oc * 512:(oc + 1) * 512], start=(hc == 0), stop=(hc == 7)),
                                ["actT", "Wd"], [psk[bk]])
                        p.op("act" if oc % 2 else "dve", (lambda e, oc=oc, bk=bk, y_=y_: e.copy(y_[:, oc * 512:(oc + 1) * 512], ps[bk][:, :])) if oc % 2
                             else (lambda e, oc=oc, bk=bk, y_=y_: e.tensor_copy(y_[:, oc * 512:(oc + 1) * 512], ps[bk][:, :])), [psk[bk]], [yk])
                    p.dma("sp", yslots[j * 128:(j + 1) * 128, :], y_[:], reads=[yk])
                p.flush()

            with ExitStack() as ph:
                g2 = [SB("g2_%d" % m, [128, D], F32, ph) for m in range(2)]
                lnG = SB("lnG2", [128, D], F32, ph)
                lnB = SB("lnB2", [128, D], F32, ph)
                ya = [SB("ya%d" % i, [128, D], F32, ph) for i in range(2)]
                yc = [SB("yc%d" % i, [128, D], F32, ph) for i in range(2)]
                xo = [SB("xq%d" % i, [128, D], F32, ph) for i in range(2)]
                B6 = dict(stt=SB("stt7", [128, 4, 6], F32, ph), mv=SB("mv7", [128, 2], F32, ph),
                          rstd=SB("rstd7", [128, 1], F32, ph), nmr=SB("nmr7", [128, 1], F32, ph))
                for m in range(2):
                    p.dma("sp", g2[m][:], gsc[l, 1, m:m + 1, :].to_broadcast([128, D]), writes=["g2"])
                p.dma("sp", lnG[:], ln_g[1][l:l + 1, :].to_broadcast([128, D]), writes=["lnG"])
                p.dma("sp", lnB[:], ln_b[1][l:l + 1, :].to_broadcast([128, D]), writes=["lnB"])
                for t in range(NT):
                    if l == DEPTH - 1 and t < 2:
                        continue
                    tok = slice(t * 128, (t + 1) * 128)
                    m = 1 if t < 2 else 0
                    i_ = t % 2
                    for k, (yy, ykk) in enumerate(((ya[i_], "ya%d" % i_), (yc[i_], "yc%d" % i_))):
                        p.op("pool", lambda e, t=t, k=k, yy=yy: e.indirect_dma_start(
                            out=yy[:], out_offset=None, in_=yslots,
                            in_offset=bass.IndirectOffsetOnAxis(ap=dest_i[:, t, k:k + 1], axis=0)), ["dest_i"], [ykk], dma=True)
                    x_, xk = xo[i_], "xq%d" % i_
                    p.dma("sp", x_[:], xres[tok, :], reads=["xres%d" % t], writes=[xk])
                    y1_, y2_ = ya[i_], yc[i_]
                    p.op("dve", lambda e, t=t, y1_=y1_: e.tensor_scalar(y1_[:], y1_[:], gatew[:, t, 0:1], None, ALU.mult), ["ya%d" % i_, "gatew"], ["ya%d" % i_])
                    p.op("dve", lambda e, t=t, y1_=y1_, y2_=y2_: e.scalar_tensor_tensor(y1_[:], y2_[:], gatew[:, t, 1:2], y1_[:], ALU.mult, ALU.add),
                         ["ya%d" % i_, "yc%d" % i_, "gatew"], ["ya%d" % i_])
                    p.op("pool", lambda e, y1_=y1_, m=m: e.tensor_tensor(y1_[:], y1_[:], g2[m][:], ALU.mult), ["ya%d" % i_, "g2"], ["ya%d" % i_])
                    p.op("dve", lambda e, x_=x_, y1_=y1_: e.scalar_tensor_tensor(y1_[:], x_[:], ALPHA, y1_[:], ALU.mult, ALU.add),
                         [xk, "ya%d" % i_], ["ya%d" % i_])
                    ln_stats(y1_, "ya%d" % i_, B6["stt"], B6["mv"], B6["rstd"], B6["nmr"], "q")
                    p.op("act", lambda e, x_=x_, y1_=y1_: e.activation(x_[:], y1_[:], AF.Identity, bias=B6["nmr"][:], scale=B6["rstd"][:]),
                         ["ya%d" % i_, "qnmr", "qrstd"], [xk])
                    p.op("dve", lambda e, x_=x_: e.tensor_tensor(x_[:], x_[:], lnG[:], ALU.mult), [xk, "lnG"], [xk])
                    p.op("pool", lambda e, x_=x_: e.tensor_tensor(x_[:], x_[:], lnB[:], ALU.add), [xk, "lnB"], [xk])
                    if l == DEPTH - 1:
                        p.dma("sp", out_d[(t - 2) * 128:(t - 1) * 128, :], x_[:], reads=[xk])
                    else:
                        p.dma("sp", xres[tok, :], x_[:], reads=[xk], writes=["xres%d" % t])
                p.flush()
            if "L0" in dbg:
                break
        p.finish()
        print("instructions:", p.n_inst)
    return nc


def _consts(L):
    T = NCTX + L
    f32 = np.float32
    t = np.arange(T)
    lat = t >= NCTX
    i = np.where(lat, t - NCTX, 0)
    row = (i // 64).astype(f32)
    col = (i % 64).astype(f32)
    pos = t.astype(f32)
    fa = (10000.0 ** (-np.arange(32, dtype=f32) / 32)).astype(f32)
    fb = (10000.0 ** (-np.arange(16, dtype=f32) / 16)).astype(f32)

    def cs(pp, fr, mask):
        ang = (pp[:, None] * fr[None, :]).astype(f32)
        c = np.cos(ang).astype(f32)
        s = np.sin(ang).astype(f32)
        if mask is not None:
            c = np.where(mask[:, None], c, 1.0).astype(f32)
            s = np.where(mask[:, None], s, 0.0).astype(f32)
        return c, s
    cr, sr = cs(row, fa, lat)
    cc, sc = cs(col, fa, lat)
    swa = [np.concatenate([cr, cr, cc, cc], 1), np.concatenate([-sr, sr, -sc, sc], 1)]
    cp, sp = cs(pos, fa, None)
    ret = [np.concatenate([cp, cp], 1), np.concatenate([-sp, sp], 1)]
    cr, sr = cs(row, fb, lat)
    cc, sc = cs(col, fb, lat)
    mla = [np.concatenate([cr, cr, cc, cc], 1), np.concatenate([-sr, sr, -sc, sc], 1)]
    rtab = np.ascontiguousarray(np.concatenate(swa + ret + mla, 1).astype(f32))
    s = np.arange(128)[:, None]
    c = np.arange(128)[None, :]
    z = 0 * s + 0 * c
    cst = np.ascontiguousarray(np.stack([(s <= c) + z, (s >= c) + z, (c - s) + z, (c + 1) + z, (128 - c) + z, (s < c) + z], 1).astype(f32))
    scol = np.ascontiguousarray(np.stack([127 - np.arange(128), np.arange(128)], 1).astype(f32))
    NB = 2 * (T // 128) + NE
    jv = np.ascontiguousarray(np.tile((np.arange(NB, dtype=f32) * 128)[None, :], (128, 1)))
    pidx = np.arange(128, dtype=f32).reshape(128, 1)
    return dict(ident=np.eye(128, dtype=f32), rtab=rtab, cst=cst, scol=scol, jv=jv, pidx=pidx)


_WKEYS = ("w_ada", "b_ada", "w_in", "swa_sink", "ret_decay", "mla_kv_norm", "mla_w_uk", "mla_w_uv", "w_out",
          "ln1_g", "ln1_b", "ln2_g", "ln2_b", "moe_w_group", "moe_b_group", "moe_w_expert", "moe_b_expert",
          "moe_w_gate", "moe_w_up", "moe_w_down")


def core_inputs(inp, b, L, consts=None, names=None):
    m = dict(consts if consts is not None else _consts(L))
    m["x"] = np.ascontiguousarray(inp["x"][b], dtype=np.float32)
    m["ctx"] = np.ascontiguousarray(inp["ctx"][b], dtype=np.float32)
    m["c2"] = np.ascontiguousarray(np.stack([inp["c"][b], inp["c_ctx"]], 0), dtype=np.float32)
    for k in _WKEYS:
        if names is None or k in names:
            m[k] = np.ascontiguousarray(inp[k], dtype=np.float32)
    return m


_NC_CACHE = {}


def kernel(**inputs):
    L = int(inputs["x"].shape[1])
    Bn = int(inputs["x"].shape[0])
    if L not in _NC_CACHE:
        _NC_CACHE[L] = build(L)
    nc = _NC_CACHE[L]
    consts = _consts(L)
    inp = {k: np.asarray(v) for k, v in inputs.items()}
    in_maps = [core_inputs(inp, b, L, consts, names=nc._in_names) for b in range(Bn)]
    res = run_bass_kernel_spmd(nc, in_maps, core_ids=list(range(Bn)))
    out = np.stack([np.asarray(res.results[b]["out"]) for b in range(Bn)], 0)
    return out.astype(np.float32)
```

```python
import math
from contextlib import ExitStack
import numpy as np
import ml_dtypes
import concourse.bass as bass
import concourse.mybir as mybir
from concourse.bass_utils import run_bass_kernel_spmd

F32 = mybir.dt.float32
BF16 = mybir.dt.bfloat16
I32 = mybir.dt.int32
AF = mybir.ActivationFunctionType
ALU = mybir.AluOpType
AX = mybir.AxisListType

D = 2048
NCTX = 256
DEPTH = 2
IN_W = 5440
ALPHA = (2 * DEPTH) ** 0.25
EPS = 1e-6
NE = 32
EH = 1024
COMPUTE = ("pe", "act", "dve", "pool")


class Prog:
    def __init__(self, nc, stack, n_dma_sems=12):
        self.nc = nc
        self.eng = {"pe": nc.tensor, "act": nc.scalar, "dve": nc.vector,
                    "pool": nc.gpsimd, "sp": nc.sync}
        self.ops = []
        self.nd = n_dma_sems
        self.csem = {e: stack.enter_context(nc.semaphore("cs_" + e)) for e in COMPUTE}
        self.ccount = {e: 0 for e in COMPUTE}
        self.sem_obj = {("c", e): self.csem[e] for e in COMPUTE}
        self.dstate = {}
        for e in ("sp", "act", "pool"):
            sems = [stack.enter_context(nc.semaphore("ds_%s_%d" % (e, k))) for k in range(n_dma_sems)]
            for k, s in enumerate(sems):
                self.sem_obj[("d", e, k)] = s
            self.dstate[e] = dict(next=0, cnt=[0] * n_dma_sems)
        self.waited = {}
        self.carry = {}
        self.pend = {e: {} for e in self.eng}
        self.n_inst = 0

    def op(self, eng, fn, reads=(), writes=(), dma=False):
        self.ops.append((eng, fn, tuple(reads), tuple(writes), dma))

    def dma(self, eng, out, in_, reads=(), writes=(), **kw):
        self.op(eng, lambda e: e.dma_start(out=out, in_=in_, **kw), reads, writes, dma=True)

    def _wait(self, eng, sk, val):
        key = (eng, sk)
        if self.waited.get(key, 0) >= val:
            return
        self.waited[key] = val
        self.eng[eng].wait_ge(self.sem_obj[sk], val)
        self.n_inst += 1

    def flush(self):
        ops = self.ops
        self.ops = []
        n = len(ops)
        last_w, readers = {}, {}
        deps = [None] * n
        last_on_eng = {}
        dma_ops = []
        for i, (eng, fn, reads, writes, dma) in enumerate(ops):
            d = set()
            for k in reads:
                if k in last_w:
                    d.add(last_w[k])
            for k in writes:
                if k in last_w:
                    d.add(last_w[k])
                r = readers.get(k)
                if r:
                    d.update(r[0].values())
                    d.update(r[1])
            d.discard(i)
            deps[i] = d
            for k in reads:
                r = readers.setdefault(k, ({}, []))
                if dma:
                    r[1].append(i)
                else:
                    r[0][eng] = i
            for k in writes:
                last_w[k] = i
                readers[k] = ({}, [])
            if dma:
                dma_ops.append(i)
            else:
                last_on_eng[eng] = i
        signal = [False] * n
        for i, (eng, fn, reads, writes, dma) in enumerate(ops):
            keep = set()
            for j in deps[i]:
                ej, _, rj, wj, dj = ops[j]
                if (not dj) and ej == eng and not dma:
                    if eng == "pe":
                        continue
                    if not (set(wj) & set(reads)):
                        continue
                keep.add(j)
            deps[i] = keep
            for j in keep:
                signal[j] = True
        for e, j in last_on_eng.items():
            signal[j] = True
        event = [None] * n
        for i, (eng, fn, reads, writes, dma) in enumerate(ops):
            need = {}
            if self.pend[eng]:
                need.update(self.pend[eng])
                self.pend[eng] = {}
            for j in deps[i]:
                sk, val = event[j]
                if need.get(sk, 0) < val:
                    need[sk] = val
            for sk, val in need.items():
                self._wait(eng, sk, val)
            if dma:
                st = self.dstate[eng]
                k = st["next"]
                st["next"] = (k + 1) % self.nd
                sk = ("d", eng, k)
                if st["cnt"][k] > 0:
                    self._wait(eng, sk, st["cnt"][k])
                st["cnt"][k] += 16
                ins = fn(self.eng[eng])
                ins.then_inc(self.sem_obj[sk], 16)
                event[i] = (sk, st["cnt"][k])
            else:
                ins = fn(self.eng[eng])
                if signal[i]:
                    self.ccount[eng] += 1
                    ins.then_inc(self.csem[eng], 1)
                    event[i] = (("c", eng), self.ccount[eng])
            self.n_inst += 1
        carry = {}
        for e in COMPUTE:
            if self.ccount[e] > 0:
                carry[("c", e)] = self.ccount[e]
        for e, st in self.dstate.items():
            for k in range(self.nd):
                if st["cnt"][k] > 0:
                    carry[("d", e, k)] = st["cnt"][k]
        for e in self.eng:
            self.pend[e] = dict(carry)

    def finish(self):
        self.flush()
        for sk, val in self.pend["sp"].items():
            self._wait("sp", sk, val)


def V(t, off, *dims):
    F = 1
    for s in t.shape[1:]:
        F *= s
    npart = dims[0]
    return bass.AP(t, off, [[F, npart]] + [list(d) for d in dims[1:]])


CHUNKS = [("sq0", 0, 512), ("sq1", 512, 256), ("sk", 768, 256), ("sv", 1024, 256),
          ("rq", 1280, 384), ("rk", 1664, 384), ("rv0", 2048, 512), ("rv1", 2560, 256),
          ("gf0", 2816, 512), ("gf1", 3328, 256), ("gb0", 3584, 512), ("gb1", 4096, 256),
          ("mq0", 4352, 384), ("mq1", 4736, 384), ("ckv", 5120, 320)]


def build(L, dbg=()):
    nc = bass.Bass("TRN2", target_bir_lowering=False)
    NT = (NCTX + L) // 128
    T = NT * 128
    NLT = L // 128

    in_names = []
    nc._in_names = in_names

    def din(name, shape, dt=F32):
        in_names.append(name)
        return nc.dram_tensor(name, list(shape), dt, kind="ExternalInput").ap()

    def dscr(name, shape, dt=F32):
        kind = "ExternalOutput" if name in dbg else "Internal"
        return nc.dram_tensor(name, list(shape), dt, kind=kind).ap()

    x_in = din("x", [L, D])
    ctx_in = din("ctx", [NCTX, D])
    c2_in = din("c2", [2, D])
    w_ada = din("w_ada", [DEPTH, D, 6 * D])
    b_ada = din("b_ada", [DEPTH, 6 * D])
    w_in = din("w_in", [DEPTH, D, IN_W])
    swa_sink = din("swa_sink", [DEPTH, 6])
    ret_decay = din("ret_decay", [DEPTH, 2, 6])
    kv_norm = din("mla_kv_norm", [DEPTH, 256])
    w_uk = din("mla_w_uk", [DEPTH, 256, 512])
    w_uv = din("mla_w_uv", [DEPTH, 256, 512])
    w_out = din("w_out", [DEPTH, D, D])
    ln_g = [din("ln1_g", [DEPTH, D]), din("ln2_g", [DEPTH, D])]
    ln_b = [din("ln1_b", [DEPTH, D]), din("ln2_b", [DEPTH, D])]
    w_grp = din("moe_w_group", [DEPTH, D, 4])
    b_grp = din("moe_b_group", [DEPTH, 4])
    w_exp = din("moe_w_expert", [DEPTH, D, NE])
    b_exp = din("moe_b_expert", [DEPTH, NE])
    if not (set(dbg) & {"P2", "P5", "P6"}):
        w_gate = din("moe_w_gate", [DEPTH, NE, D, EH])
        w_up = din("moe_w_up", [DEPTH, NE, D, EH])
        w_down = din("moe_w_down", [DEPTH, NE, EH, D])
    ident_in = din("ident", [128, 128])
    rtab_in = din("rtab", [T, 512])
    out_d = nc.dram_tensor("out", [L, D], F32, kind="ExternalOutput").ap()

    xres = dscr("xres", [T, D])
    gsc = dscr("gsc", [DEPTH, 4, 2, D])
    qTs = dscr("qTs", [6, 128, T], BF16)
    kTs = dscr("kTs", [2, 128, T], BF16)
    vs = dscr("vs", [T, 256], BF16)
    qTr = dscr("qTr", [3, 128, T], BF16)
    kTr = dscr("kTr", [3, 128, T], BF16)
    kr = dscr("kr", [T, 384], BF16)
    vr = dscr("vr", [T, 768], BF16)
    gfb = dscr("gfb", [2, T, 768])
    qnT = dscr("qnT", [4, 128, T], BF16)
    qrT = dscr("qrT", [4, 64, T], BF16)
    knT = dscr("knT", [4, 128, T], BF16)
    krT = dscr("krT", [64, T], BF16)
    vm = dscr("vm", [T, 512], BF16)
    catT = dscr("catT", [16, 128, T], BF16)

    with ExitStack() as st:
        p = Prog(nc, st)
        _uid = [0]

        def SB(name, shape, dt=F32, s=st):
            _uid[0] += 1
            return s.enter_context(nc.sbuf_tensor("s%d_%s" % (_uid[0], name), list(shape), dt))
        ps = [st.enter_context(nc.psum_tensor("ps%d" % i, [128, 512], F32)) for i in range(8)]
        psk = ["ps%d" % i for i in range(8)]
        psb = [b[:].bitcast(BF16) for b in ps]

        ident = SB("ident", [128, 128])
        identb = SB("identb", [128, 128], BF16)
        modcol = SB("modcol", [128, 96, 2])
        scT = SB("scT", [128, 16, 2], BF16)
        p.dma("sp", ident[:], ident_in, writes=["ident"])
        p.op("dve", lambda e: e.tensor_copy(identb[:], ident[:]), ["ident"], ["identb"])

        with ExitStack() as ph:
            c2 = SB("c2", [2, D], F32, ph)
            p.dma("sp", c2[:], c2_in, writes=["c2"])
            p.op("act", lambda e: e.activation(c2[:], c2[:], AF.Silu), ["c2"], ["c2"])
            for kc in range(16):
                p.op("pe", lambda e, kc=kc: e.transpose(ps[0][:, kc * 2:kc * 2 + 2], c2[:, kc * 128:(kc + 1) * 128],
                                                        ident[0:2, 0:2]), ["c2", "ident"], [psk[0]])
            p.op("dve", lambda e: e.tensor_copy(scT[:].rearrange("p a b -> p (a b)"), ps[0][:, 0:32]), [psk[0]], ["scT"])
            p.flush()

        cst_in = din("cst", [128, 6, 128])
        scol_in = din("scol", [128, 2])
        cst = SB("cst", [128, 6, 128])
        scol = SB("scol", [128, 2])
        mle = SB("mle", [128, 128], BF16)
        mge = SB("mge", [128, 128], BF16)
        onesb = SB("onesb", [128, 128], BF16)
        onesf = SB("onesf", [128, 128])
        p.dma("sp", cst[:], cst_in, writes=["cst"])
        p.dma("sp", scol[:], scol_in, writes=["scol"])
        p.op("dve", lambda e: e.tensor_copy(mle[:], cst[:, 0, :]), ["cst"], ["mle"])
        p.op("dve", lambda e: e.tensor_copy(mge[:], cst[:, 1, :]), ["cst"], ["mge"])
        p.op("dve", lambda e: e.memset(onesb[:], 1.0), [], ["onesb"])
        p.op("dve", lambda e: e.memset(onesf[:], 1.0), [], ["onesf"])
        p.flush()

        def blk_src(l, t):
            if l == 0:
                return ctx_in[t * 128:(t + 1) * 128, :] if t < 2 else x_in[(t - 2) * 128:(t - 1) * 128, :]
            return xres[t * 128:(t + 1) * 128, :]

        cnt = [0]

        def ln_stats(xt, xk, stt, mv, rstd, nmr, pre):
            for c4 in range(4):
                p.op("dve", lambda e, c4=c4: e.bn_stats(stt[:, c4, :], xt[:, c4 * 512:(c4 + 1) * 512]), [xk], [pre + "st"])
            p.op("dve", lambda e: e.bn_aggr(mv[:], stt[:].rearrange("p a b -> p (a b)")), [pre + "st"], [pre + "mv"])
            p.op("dve", lambda e: e.tensor_scalar(rstd[:], mv[:, 1:2], EPS, None, ALU.add), [pre + "mv"], [pre + "rstd"])
            p.op("act", lambda e: e.activation(rstd[:], rstd[:], AF.Sqrt), [pre + "rstd"], [pre + "rstd"])
            p.op("dve", lambda e: e.reciprocal(rstd[:], rstd[:]), [pre + "rstd"], [pre + "rstd"])
            p.op("dve", lambda e: e.scalar_tensor_tensor(nmr[:], mv[:, 0:1], -1.0, rstd[:], ALU.mult, ALU.mult),
                 [pre + "mv", pre + "rstd"], [pre + "nmr"])

        def ln_mod_T(src_ap, m, jsh, jsc, hT, hk, slot, B):
            i = cnt[0]
            cnt[0] += 1
            xt, xk = B["xt"][i % 2], "xt%d" % (i % 2)
            p.dma("sp", xt[:], src_ap, writes=[xk])
            ln_stats(xt, xk, B["stt"], B["mv"], B["rstd"], B["nmr"], "l")
            xn = B["xn"]
            p.op("act", lambda e: e.activation(xn[:], xt[:], AF.Identity, bias=B["nmr"][:], scale=B["rstd"][:]),
                 [xk, "lnmr", "lrstd"], ["xn"])
            for q in range(4):
                bk = 4 + q
                for k4 in range(4):
                    kc = q * 4 + k4
                    p.op("pe", lambda e, kc=kc, k4=k4, bk=bk: e.transpose(
                        ps[bk][:, k4 * 128:(k4 + 1) * 128], xn[:, kc * 128:(kc + 1) * 128], ident[:]),
                        ["xn", "ident"], [psk[bk]])
                for k4 in range(4):
                    kc = q * 4 + k4
                    p.op("act", lambda e, kc=kc, k4=k4, bk=bk: e.activation(
                        hT[:, kc, slot * 128:(slot + 1) * 128], ps[bk][:, k4 * 128:(k4 + 1) * 128], AF.Identity,
                        bias=modcol[:, jsh * 16 + kc, m:m + 1], scale=modcol[:, jsc * 16 + kc, m:m + 1]),
                        [psk[bk], "modcol"], [hk])

        def rope(rs, W, H, d, hb, hstride, hoff, tab, tk, cb, sb, r1, r2, rb, x=""):
            nb2 = d // (2 * hb)
            full = lambda t, o=0: V(t, hoff + o, 128, [hstride, H], [2 * hb, nb2], [1, hb])
            p.op("dve", lambda e: e.tensor_tensor(
                V(r1, hoff, 128, [hstride, H], [1, d]), V(rs, hoff, 128, [hstride, H], [1, d]),
                V(tab, cb, 128, [0, H], [1, d]), ALU.mult), ["rs" + x, tk], ["r1" + x])
            for b in range(2):
                p.op("dve", lambda e, b=b: e.tensor_tensor(
                    full(r2, b * hb), full(rs, (1 - b) * hb),
                    V(tab, sb + b * hb, 128, [0, H], [2 * hb, nb2], [1, hb]), ALU.mult), ["rs" + x, tk], ["r2" + x])
            p.op("dve", lambda e: e.tensor_tensor(
                V(rb, hoff, 128, [hstride, H], [1, d]), V(r1, hoff, 128, [hstride, H], [1, d]),
                V(r2, hoff, 128, [hstride, H], [1, d]), ALU.add), ["r1" + x, "r2" + x], ["rb" + x])

        for l in range(DEPTH):
            with ExitStack() as ph:
                wa = [SB("wa%d" % i, [128, 16, 1024], BF16, ph) for i in range(2)]
                bar = SB("bar", [96, 128], F32, ph)
                bcol = SB("bcol", [128, 96], F32, ph)
                rows2 = SB("rows2", [2, 4, D], F32, ph)
                p.dma("sp", bar[:], b_ada[l].rearrange("(a b) -> a b", b=128), writes=["bar"])
                p.op("pe", lambda e: e.transpose(ps[1][:, 0:96], bar[:], ident[0:96, 0:96]), ["bar", "ident"], [psk[1]])
                p.op("dve", lambda e: e.tensor_copy(bcol[:], ps[1][:, 0:96]), [psk[1]], ["bcol"])
                wav = w_ada[l].rearrange("(kc p) n -> p kc n", p=128)
                for g in range(12):
                    w = wa[g % 2]
                    wk = "wa%d" % (g % 2)
                    p.dma("pool", w[:], wav[:, :, g * 1024:(g + 1) * 1024], writes=[wk])
                    for nn in range(8):
                        n = g * 8 + nn
                        for kc in range(16):
                            p.op("pe", lambda e, w=w, nn=nn, n=n, kc=kc: e.matmul(
                                ps[0][:, 2 * n:2 * n + 2], w[:, kc, nn * 128:(nn + 1) * 128], scT[:, kc, :],
                                start=(kc == 0), stop=(kc == 15)), [wk, "scT"], [psk[0]])
                p.op("dve", lambda e: e.tensor_tensor(
                    modcol[:], ps[0][:, 0:192].rearrange("p (a b) -> p a b", b=2),
                    V(bcol, 0, 128, [1, 96], [0, 2]), ALU.add), [psk[0], "bcol"], ["modcol"])
                for j in (1, 4):
                    p.op("dve", lambda e, j=j: e.tensor_scalar_add(modcol[:, j * 16:(j + 1) * 16, :],
                                                                   modcol[:, j * 16:(j + 1) * 16, :], 1.0),
                         ["modcol"], ["modcol"])
                for si, j in enumerate((2, 5, 3, 4)):
                    for kc in range(16):
                        bk = 2 + kc // 4
                        p.op("pe", lambda e, j=j, kc=kc, bk=bk: e.transpose(
                            ps[bk][0:2, (kc % 4) * 128:(kc % 4 + 1) * 128], modcol[:, j * 16 + kc, :], ident[:]),
                            ["modcol", "ident"], [psk[bk]])
                    for q in range(4):
                        p.op("dve", lambda e, si=si, q=q: e.tensor_copy(rows2[:, si, q * 512:(q + 1) * 512], ps[2 + q][0:2, :]),
                             [psk[2 + q]], ["rows2"])
                p.dma("sp", gsc[l].rearrange("s m d -> m s d"), rows2[:], reads=["rows2"], writes=[])
                p.flush()

            with ExitStack() as ph:
                B = dict(xt=[SB("xt%d" % i, [128, D], F32, ph) for i in range(2)], xn=SB("xn", [128, D], F32, ph),
                         stt=SB("stt", [128, 4, 6], F32, ph), mv=SB("mv", [128, 2], F32, ph),
                         rstd=SB("rstd", [128, 1], F32, ph), nmr=SB("nmr", [128, 1], F32, ph))
                hT = SB("hT", [128, 16, 1024], BF16, ph)
                wch = [SB("wch%d" % i, [128, 16, 512], BF16, ph) for i in range(2)]
                rt = [SB("rt%d" % i, [128, 512], F32, ph) for i in range(8)]
                rsL = [SB("rs%d" % i, [128, 768], F32, ph) for i in range(3)]
                r1L = [SB("r1%d" % i, [128, 768], F32, ph) for i in range(3)]
                r2L = [SB("r2%d" % i, [128, 768], F32, ph) for i in range(3)]
                rbL = [SB("rb%d" % i, [128, 768], BF16, ph) for i in range(3)]
                tTL = [SB("tT%d" % i, [128, 6, 128], BF16, ph) for i in range(3)]
                gstL = [SB("gst%d" % i, [128, 512], F32, ph) for i in range(3)]
                cnTL = [SB("cnT%d" % i, [128, 2, 128], BF16, ph) for i in range(3)]
                ssqL = [SB("ssq%d" % i, [128, 1], F32, ph) for i in range(3)]
                gkv = SB("gkv", [128, 256], F32, ph)
                wuk = SB("wuk", [128, 2, 512], BF16, ph)
                wuv = SB("wuv", [128, 2, 512], BF16, ph)
                p.dma("sp", gkv[:], kv_norm[l:l + 1, :].to_broadcast([128, 256]), writes=["gkv"])
                p.dma("pool", wuk[:], w_uk[l].rearrange("(rc p) n -> p rc n", p=128), writes=["wuk"])
                p.dma("pool", wuv[:], w_uv[l].rearrange("(rc p) n -> p rc n", p=128), writes=["wuv"])
                wiv = w_in[l].rearrange("(kc p) n -> p kc n", p=128)
                wi = 0
                mmc = [0]
                pcnt = [0]
                pendq = []
                for g0 in range(0, NT, 8):
                    tblks = list(range(g0, min(g0 + 8, NT)))
                    for s_, t in enumerate(tblks):
                        ln_mod_T(blk_src(l, t), 1 if t < 2 else 0, 0, 1, hT, "hT", s_, B)
                        p.dma("sp", rt[s_][:], rtab_in[t * 128:(t + 1) * 128, :], writes=["rt%d" % s_])
                    for (cname, c0, cw) in CHUNKS:
                        w = wch[wi % 2]
                        wk = "wch%d" % (wi % 2)
                        wi += 1
                        p.dma("pool", w[:, :, 0:cw], wiv[:, :, c0:c0 + cw], writes=[wk])
                        for s_, t in enumerate(tblks):
                            tok = slice(t * 128, (t + 1) * 128)
                            bk = mmc[0] % 3
                            mmc[0] += 1
                            pk = psk[bk]
                            pt = ps[bk]
                            tab, tk = rt[s_], "rt%d" % s_
                            for kc in range(16):
                                p.op("pe", lambda e, kc=kc, s_=s_, w=w, pt=pt, cw=cw: e.matmul(
                                    pt[:, 0:cw], hT[:, kc, s_ * 128:(s_ + 1) * 128], w[:, kc, 0:cw],
                                    start=(kc == 0), stop=(kc == 15)), ["hT", wk], [pk])

                            def post(cname=cname, cw=cw, pt=pt, pk=pk, tab=tab, tk=tk, tok=tok, x=str(pcnt[0] % 3)):
                                rs, r1, r2, rb, tT, gst, cnT, ssq = (rsL[int(x)], r1L[int(x)], r2L[int(x)], rbL[int(x)], tTL[int(x)],
                                                                      gstL[int(x)], cnTL[int(x)], ssqL[int(x)])
                                def transp(nblk, width, dst_ap, srcoff=lambda b: b * 128):
                                    for b in range(nblk):
                                        p.op("pe", lambda e, b=b: e.transpose(
                                            psb[7][0:width, b * 128:(b + 1) * 128], rb[:, srcoff(b):srcoff(b) + width], identb[:]),
                                            ["rb" + x, "identb"], [psk[7]])
                                    p.op("act", lambda e: e.copy(tT[0:width, 0:nblk, :],
                                                                 psb[7][0:width, 0:nblk * 128].rearrange("p (a b) -> p a b", b=128)),
                                         [psk[7]], ["tT" + x])
                                    p.dma("sp", dst_ap, tT[0:width, 0:nblk, :], reads=["tT" + x], writes=[])

                                if cname in ("sq0", "sq1", "sk"):
                                    H = cw // 128
                                    sc = 128 ** -0.5 if cname != "sk" else 1.0
                                    p.op("act", lambda e, pt=pt, cw=cw, sc=sc: e.mul(rs[:, 0:cw], pt[:, 0:cw], sc), [pk], ["rs" + x])
                                    rope(rs, cw, H, 128, 32, 128, 0, tab, tk, 0, 128, r1, r2, rb, x)
                                    if cname == "sk":
                                        dst = kTs[:, :, tok]
                                    else:
                                        h0 = 0 if cname == "sq0" else 4
                                        dst = qTs[h0:h0 + H, :, tok]
                                    transp(H, 128, dst.rearrange("h p t -> p h t"))
                                elif cname in ("sv", "rv0", "rv1"):
                                    p.op("act", lambda e, pt=pt, cw=cw: e.copy(rb[:, 0:cw], pt[:, 0:cw]), [pk], ["rb" + x])
                                    if cname == "sv":
                                        dst = vs[tok, :]
                                    else:
                                        o = 0 if cname == "rv0" else 512
                                        dst = vr[tok, o:o + cw]
                                    p.dma("sp", dst, rb[:, 0:cw], reads=["rb" + x], writes=[])
                                elif cname in ("gf0", "gf1", "gb0", "gb1"):
                                    p.op("act", lambda e, pt=pt, cw=cw: e.activation(gst[:, 0:cw], pt[:, 0:cw], AF.Silu), [pk], ["gst" + x])
                                    o = 0 if cname[2] == "0" else 512
                                    p.dma("sp", gfb[0 if cname[1] == "f" else 1, tok, o:o + cw], gst[:, 0:cw], reads=["gst" + x], writes=[])
                                elif cname in ("rq", "rk"):
                                    sc = 64 ** -0.5 if cname == "rq" else 1.0
                                    p.op("act", lambda e, pt=pt, sc=sc: e.mul(rs[:, 0:384], pt[:, 0:384], sc), [pk], ["rs" + x])
                                    rope(rs, 384, 6, 64, 32, 64, 0, tab, tk, 256, 320, r1, r2, rb, x)
                                    if cname == "rk":
                                        p.dma("sp", kr[tok, :], rb[:, 0:384], reads=["rb" + x], writes=[])
                                    dst = (qTr if cname == "rq" else kTr)[:, :, tok]
                                    transp(3, 128, dst.rearrange("h p t -> p h t"))
                                elif cname in ("mq0", "mq1"):
                                    sc = 192 ** -0.5
                                    p.op("act", lambda e, pt=pt, sc=sc: e.mul(rs[:, 0:384], pt[:, 0:384], sc), [pk], ["rs" + x])
                                    p.op("dve", lambda e: e.tensor_copy(rb[:, 0:384], rs[:, 0:384]), ["rs" + x], ["rb" + x])
                                    rope(rs, 384, 2, 64, 16, 192, 128, tab, tk, 384, 448, r1, r2, rb, x)
                                    h0 = 0 if cname == "mq0" else 2
                                    transp(2, 128, qnT[h0:h0 + 2, :, tok].rearrange("h p t -> p h t"), srcoff=lambda b: b * 192)
                                    transp(2, 64, qrT[h0:h0 + 2, :, tok].rearrange("h p t -> p h t"), srcoff=lambda b: b * 192 + 128)
                                else:
                                    p.op("act", lambda e, pt=pt: e.copy(rs[:, 0:320], pt[:, 0:320]), [pk], ["rs" + x])
                                    rope(rs, 64, 1, 64, 16, 64, 256, tab, tk, 384, 448, r1, r2, rb, x)
                                    transp(1, 64, krT[:, tok].rearrange("p (a t) -> p a t", a=1), srcoff=lambda b: 256)
                                    p.op("dve", lambda e: e.tensor_tensor(r1[:, 0:256], rs[:, 0:256], rs[:, 0:256], ALU.mult), ["rs" + x], ["r1" + x])
                                    p.op("dve", lambda e: e.reduce_sum(ssq[:], r1[:, 0:256], axis=AX.X), ["r1" + x], ["ssq" + x])
                                    p.op("dve", lambda e: e.tensor_scalar(ssq[:], ssq[:], 1.0 / 256, EPS, ALU.mult, ALU.add), ["ssq" + x], ["ssq" + x])
                                    p.op("act", lambda e: e.activation(ssq[:], ssq[:], AF.Sqrt), ["ssq" + x], ["ssq" + x])
                                    p.op("dve", lambda e: e.reciprocal(ssq[:], ssq[:]), ["ssq" + x], ["ssq" + x])
                                    p.op("dve", lambda e: e.scalar_tensor_tensor(rb[:, 0:256], rs[:, 0:256], ssq[:, 0:1], gkv[:],
                                                                                 ALU.mult, ALU.mult), ["rs" + x, "ssq" + x, "gkv"], ["rb" + x])
                                    for b in range(2):
                                        p.op("pe", lambda e, b=b: e.transpose(psb[7][:, b * 128:(b + 1) * 128], rb[:, b * 128:(b + 1) * 128], identb[:]),
                                             ["rb" + x, "identb"], [psk[7]])
                                    p.op("act", lambda e: e.copy(cnT[:].rearrange("p a b -> p (a b)"), psb[7][:, 0:256]), [psk[7]], ["cnT" + x])
                                    bk2 = 3
                                    for h in range(4):
                                        for rc in range(2):
                                            p.op("pe", lambda e, h=h, rc=rc, bk2=bk2: e.matmul(
                                                ps[bk2][:, h * 128:(h + 1) * 128], wuk[:, rc, h * 128:(h + 1) * 128], cnT[:, rc, :],
                                                start=(rc == 0), stop=(rc == 1)), ["wuk", "cnT" + x], [psk[bk2]])
                                    p.op("act", lambda e, bk2=bk2: e.copy(tT[:, 0:4, :].rearrange("p a b -> p (a b)"), ps[bk2][:, 0:512]), [psk[bk2]], ["tT" + x])
                                    p.dma("sp", knT[:, :, tok].rearrange("h p t -> p h t"), tT[:, 0:4, :], reads=["tT" + x], writes=[])
                                    bk3 = 6
                                    for rc in range(2):
                                        p.op("pe", lambda e, rc=rc, bk3=bk3: e.matmul(
                                            ps[bk3][:, 0:512], cnT[:, rc, :], wuv[:, rc, :], start=(rc == 0), stop=(rc == 1)),
                                            ["wuv", "cnT" + x], [psk[bk3]])
                                    p.op("act", lambda e, bk3=bk3: e.copy(rb[:, 0:512], ps[bk3][:, 0:512]), [psk[bk3]], ["rb" + x])
                                    p.dma("sp", vm[tok, :], rb[:, 0:512], reads=["rb" + x], writes=[])
                            pcnt[0] += 1
                            pendq.append(post)
                            if len(pendq) > 2:
                                pendq.pop(0)()
                    while pendq:
                        pendq.pop(0)()
                p.flush()
            if "P2" in dbg:
                break
            LN2 = math.log(2.0)
            import os
            SKIP = os.environ.get("KSKIP", "").split(",")
            with ExitStack() as ph:
              if "P3" not in SKIP:
                  kT = SB("kT_s", [128, T], BF16, ph)
                  vv = SB("v_s", [128, NT, 128], BF16, ph)
                  q3 = [SB("q3_%d" % i, [128, 3, 128], BF16, ph) for i in range(2)]
                  PT = [SB("PT%d" % i, [128, 384], BF16, ph) for i in range(2)]
                  sink6 = SB("sink6", [1, 6], F32, ph)
                  esrow = SB("esrow", [1, 768], BF16, ph)
                  rec = SB("rec", [128, 384], F32, ph)
                  oT = SB("oT", [128, 384], BF16, ph)
                  p.dma("sp", sink6[:], swa_sink[l:l + 1, :], writes=["sink6"])
                  p.op("act", lambda e: e.activation(sink6[:], sink6[:], AF.Exp), ["sink6"], ["sink6"])
                  for h in range(6):
                      p.op("dve", lambda e, h=h: e.tensor_scalar(esrow[0:1, h * 128:(h + 1) * 128], onesf[0:1, 0:128],
                                                                 sink6[0:1, h:h + 1], None, ALU.mult), ["sink6", "onesf"], ["esrow"])
                  qi = 0
                  pi = 0
                  for hk in range(2):
                      p.dma("sp", kT[:], kTs[hk], writes=["kT"])
                      p.dma("sp", vv[:], vs[:, hk * 128:(hk + 1) * 128].rearrange("(n p) d -> p n d", p=128), writes=["vv"])
                      for t in range(NT):
                          keys = [(0, None), (1, None)]
                          if t >= 2:
                              n = t - 2
                              if n > 0:
                                  keys.append((t - 1, mge))
                              keys.append((t, None))
                              if n < NLT - 1:
                                  keys.append((t + 1, mle))
                          q = q3[qi % 2]
                          qk = "q3_%d" % (qi % 2)
                          dn, nm = (2, 3) if qi % 2 == 0 else (4, 5)
                          qi += 1
                          tok = slice(t * 128, (t + 1) * 128)
                          p.dma("sp", q[:], qTs[hk * 3:(hk + 1) * 3, :, tok].rearrange("h p t -> p h t"), writes=[qk])
                          pend_ = []
                          for ki, (kt, mk) in enumerate(keys):
                              sb_ = pi % 2
                              P_ = PT[pi % 2]
                              Pk = "PT%d" % (pi % 2)
                              pi += 1
                              p.op("pe", lambda e, sb_=sb_, kt=kt, q=q: e.matmul(
                                  ps[sb_][:, 0:384], kT[:, kt * 128:(kt + 1) * 128], q[:].rearrange("p a b -> p (a b)"),
                                  start=True, stop=True), ["kT", qk], [psk[sb_]])
                              p.op("act", lambda e, sb_=sb_, P_=P_: e.activation(P_[:], ps[sb_][:, 0:384], AF.Exp), [psk[sb_]], [Pk])
                              if mk is not None:
                                  p.op("dve", lambda e, P_=P_, mk=mk: e.tensor_tensor(
                                      V(P_, 0, 128, [128, 3], [1, 128]), V(P_, 0, 128, [128, 3], [1, 128]),
                                      V(mk, 0, 128, [0, 3], [1, 128]), ALU.mult), [Pk, "mle", "mge"], [Pk])

                              def fin(P_=P_, Pk=Pk, ki=ki, kt=kt, dn=dn, nm=nm, last=(ki == len(keys) - 1)):
                                  p.op("pe", lambda e: e.matmul(ps[dn][:, 0:384], onesb[:], P_[:], start=(ki == 0), stop=False),
                                       [Pk, "onesb"], [psk[dn]])
                                  p.op("pe", lambda e: e.matmul(ps[nm][:, 0:384], vv[:, kt, :], P_[:], start=(ki == 0), stop=last),
                                       [Pk, "vv"], [psk[nm]])
                              for f_ in pend_:
                                  f_()
                              pend_ = [fin]
                          for f_ in pend_:
                              f_()
                          p.op("pe", lambda e, dn=dn, hk=hk: e.matmul(
                              ps[dn][:, 0:384], onesb[0:1, :], esrow[0:1, hk * 384:(hk + 1) * 384], start=False, stop=True),
                              ["esrow", "onesb"], [psk[dn]])
                          p.op("dve", lambda e, dn=dn: e.reciprocal(rec[:], ps[dn][:, 0:384]), [psk[dn]], ["rec"])
                          p.op("dve", lambda e, nm=nm: e.tensor_tensor(oT[:], ps[nm][:, 0:384], rec[:], ALU.mult), [psk[nm], "rec"], ["oT"])
                          p.dma("sp", catT[hk * 3:(hk + 1) * 3, :, tok].rearrange("h p t -> p h t"),
                                oT[:].rearrange("p (a b) -> p a b", b=128), reads=["oT"])
                  p.flush()

            with ExitStack() as ph:
              if "P5" not in SKIP:
                  knS = SB("knS", [128, T], BF16, ph)
                  krS = SB("krS", [64, T], BF16, ph)
                  vS = SB("vS", [128, NT, 128], BF16, ph)
                  qn = [SB("qn%d" % i, [128, 512], BF16, ph) for i in range(2)]
                  qr = [SB("qr%d" % i, [64, 512], BF16, ph) for i in range(2)]
                  PT = [SB("PTm%d" % i, [128, 512], BF16, ph) for i in range(2)]
                  rec = SB("recm", [128, 512], F32, ph)
                  oT = SB("oTm", [128, 512], BF16, ph)
                  p.dma("sp", krS[:], krT, writes=["krS"])
                  groups = [(0, 2, [0, 1])] + [(g0, min(4, NT - g0), list(range(NT))) for g0 in range(2, NT, 4)]
                  qi = 0
                  pi = 0
                  for h in range(4):
                      p.dma("sp", knS[:], knT[h], writes=["knS"])
                      p.dma("sp", vS[:], vm[:, h * 128:(h + 1) * 128].rearrange("(n p) d -> p n d", p=128), writes=["vS"])
                      for (g0, ng, kblks) in groups:
                          N = ng * 128
                          cols = slice(g0 * 128, g0 * 128 + N)
                          qn_, qr_ = qn[qi % 2], qr[qi % 2]
                          qk = "qm%d" % (qi % 2)
                          dn, nm = (2, 3) if qi % 2 == 0 else (4, 5)
                          qi += 1
                          p.dma("sp", qn_[:, 0:N], qnT[h][:, cols], writes=[qk])
                          p.dma("sp", qr_[:, 0:N], qrT[h][:, cols], writes=[qk])
                          pend_ = []
                          for ki, kt in enumerate(kblks):
                              sb_ = pi % 2
                              P_ = PT[pi % 2]
                              Pk = "PTm%d" % (pi % 2)
                              pi += 1
                              p.op("pe", lambda e, sb_=sb_, kt=kt, qn_=qn_, N=N: e.matmul(
                                  ps[sb_][:, 0:N], knS[:, kt * 128:(kt + 1) * 128], qn_[:, 0:N], start=True, stop=False),
                                  ["knS", qk], [psk[sb_]])
                              p.op("pe", lambda e, sb_=sb_, kt=kt, qr_=qr_, N=N: e.matmul(
                                  ps[sb_][:, 0:N], krS[:, kt * 128:(kt + 1) * 128], qr_[:, 0:N], start=False, stop=True),
                                  ["krS", qk], [psk[sb_]])
                              p.op("act", lambda e, sb_=sb_, P_=P_, N=N: e.activation(P_[:, 0:N], ps[sb_][:, 0:N], AF.Exp), [psk[sb_]], [Pk])

                              def fin(P_=P_, Pk=Pk, ki=ki, kt=kt, dn=dn, nm=nm, N=N, last=(ki == len(kblks) - 1)):
                                  p.op("pe", lambda e: e.matmul(ps[dn][:, 0:N], onesb[:], P_[:, 0:N], start=(ki == 0), stop=last),
                                       [Pk, "onesb"], [psk[dn]])
                                  p.op("pe", lambda e: e.matmul(ps[nm][:, 0:N], vS[:, kt, :], P_[:, 0:N], start=(ki == 0), stop=last),
                                       [Pk, "vS"], [psk[nm]])
                              for f_ in pend_:
                                  f_()
                              pend_ = [fin]
                          for f_ in pend_:
                              f_()
                          p.op("dve", lambda e, dn=dn, N=N: e.reciprocal(rec[:, 0:N], ps[dn][:, 0:N]), [psk[dn]], ["recm"])
                          p.op("dve", lambda e, nm=nm, N=N: e.tensor_tensor(oT[:, 0:N], ps[nm][:, 0:N], rec[:, 0:N], ALU.mult),
                               [psk[nm], "recm"], ["oTm"])
                          p.dma("sp", catT[12 + h][:, cols], oT[:, 0:N], reads=["oTm"])
                  p.flush()

            with ExitStack() as ph:
              if "P4" not in SKIP:
                  lgb = SB("lgb", [128, 2, 6], F32, ph)
                  nlgb = SB("nlgb", [128, 2, 6], F32, ph)
                  lgcol = SB("lgcol", [128, 2, 3], F32, ph)
                  maskT = SB("maskT", [128, 2, 6, 128], F32, ph)
                  mtmp = SB("mtmp", [128, 128], F32, ph)
                  qdec = SB("qdec", [64, 2, 6, 128], F32, ph)
                  kdf = SB("kdf", [128, 2, 6], F32, ph)
                  gc = SB("gc", [64, 2, 6], F32, ph)
                  gCt = SB("gCt", [64, 2, 6, 128], F32, ph)
                  S = SB("S", [64, 6, 128], F32, ph)
                  Stmp = SB("Stmp", [64, 6, 128], F32, ph)
                  Sb = SB("Sb", [64, 6, 128], BF16, ph)
                  qt = [SB("qt%d" % i, [64, 6, 128], BF16, ph) for i in range(2)]
                  ktT = [SB("ktT%d" % i, [64, 6, 128], BF16, ph) for i in range(2)]
                  ktm = [SB("ktm%d" % i, [128, 384], BF16, ph) for i in range(2)]
                  vt = [SB("vt%d" % i, [128, 768], BF16, ph) for i in range(2)]
                  gt = [SB("gt%d" % i, [128, 768], F32, ph) for i in range(2)]
                  ra = [SB("ra%d" % i, [128, 768], F32, ph) for i in range(2)]
                  PTr = SB("PTr", [128, 6, 128], BF16, ph)
                  qd = SB("qd", [64, 6, 128], BF16, ph)
                  kd = SB("kd", [128, 6, 64], BF16, ph)
                  st6 = SB("st6", [128, 6, 6], F32, ph)
                  mv6 = SB("mv6", [128, 6, 2], F32, ph)
                  rs6 = SB("rs6", [128, 6], F32, ph)
                  y1 = SB("y1", [128, 6, 128], F32, ph)
                  y2 = SB("y2", [128, 6, 128], F32, ph)
                  yb = SB("yb", [128, 768], BF16, ph)
                  tT2 = SB("tT2", [128, 6, 128], BF16, ph)
                  racc = dscr("racc%d" % l, [T, 768])
                  p.dma("sp", lgb[:].rearrange("p a b -> p (a b)"),
                        ret_decay[l:l + 1].rearrange("o a b -> o (a b)").to_broadcast([128, 12]), writes=["lgb"])
                  p.op("act", lambda e: e.activation(lgb[:], lgb[:], AF.Exp, scale=-LN2), ["lgb"], ["lgb"])
                  p.op("act", lambda e: e.activation(lgb[:], lgb[:], AF.Ln, scale=-1.0, bias=1.0), ["lgb"], ["lgb"])
                  p.op("dve", lambda e: e.tensor_scalar(nlgb[:], lgb[:], -1.0, None, ALU.mult), ["lgb"], ["nlgb"])
                  for dr in range(2):
                      for j in range(3):
                          for hh in range(2):
                              p.op("dve", lambda e, dr=dr, j=j, hh=hh: e.tensor_copy(
                                  lgcol[hh * 64:(hh + 1) * 64, dr, j:j + 1], lgb[hh * 64:(hh + 1) * 64, dr, 2 * j + hh:2 * j + hh + 1]),
                                  ["lgb"], ["lgcol"])
                  for dr in range(2):
                      for h in range(6):
                          src = lgb if dr == 0 else nlgb
                          p.op("act", lambda e, dr=dr, h=h, src=src: e.activation(mtmp[:], cst[:, 2, :], AF.Exp, scale=src[:, dr, h:h + 1]),
                               ["cst", "lgb", "nlgb"], ["mtmp"])
                          p.op("dve", lambda e, dr=dr, h=h: e.tensor_tensor(maskT[:, dr, h, :], mtmp[:], cst[:, dr, :], ALU.mult),
                               ["mtmp", "cst"], ["maskT"])
                      for h in range(6):
                          p.op("act", lambda e, dr=dr, h=h: e.activation(qdec[:, dr, h, :], cst[0:64, 3 + dr, :], AF.Exp,
                                                                         scale=lgb[0:64, dr, h:h + 1]), ["cst", "lgb"], ["qdec"])
                      p.op("dve", lambda e, dr=dr: e.tensor_scalar(kdf[:, dr, :], lgb[:, dr, :], scol[:, dr:dr + 1], None, ALU.mult),
                           ["lgb", "scol"], ["kdf"])
                      p.op("act", lambda e, dr=dr: e.activation(kdf[:, dr, :], kdf[:, dr, :], AF.Exp), ["kdf"], ["kdf"])
                      p.op("act", lambda e, dr=dr: e.activation(gc[:, dr, :], lgb[0:64, dr, :], AF.Exp, scale=128.0), ["lgb"], ["gc"])
                      p.op("dve", lambda e, dr=dr: e.tensor_copy(gCt[:, dr], V(gc, dr * 6, 64, [1, 6], [0, 128])), ["gc"], ["gCt"])
                  bi = 0
                  RS = int(os.environ.get("RS", "9"))
                  for dr in range(2 if RS > 0 else 0):
                      order = list(range(NT)) if dr == 0 else [1, 0] + list(range(NT - 1, 1, -1))
                      p.op("dve", lambda e: e.memset(S[:], 0.0), [], ["S"])
                      p.op("dve", lambda e: e.memset(Sb[:], 0.0), [], ["Sb"])
                      for t in order:
                          tok = slice(t * 128, (t + 1) * 128)
                          b_ = bi % 2
                          bi += 1
                          qt_, kt_, km_, vt_, gt_, ra_ = qt[b_], ktT[b_], ktm[b_], vt[b_], gt[b_], ra[b_]
                          bk = "rin%d" % b_
                          p.dma("sp", qt_[:], qTr.rearrange("j (hh d) t -> d (j hh) t", d=64)[:, :, tok], writes=[bk + "q"])
                          p.dma("sp", kt_[:], kTr.rearrange("j (hh d) t -> d (j hh) t", d=64)[:, :, tok], writes=[bk + "k"])
                          p.dma("sp", km_[:], kr[tok, :], writes=[bk + "km"])
                          p.dma("sp", vt_[:], vr[tok, :], writes=[bk + "v"])
                          p.dma("sp", gt_[:], gfb[dr, tok, :], writes=[bk + "g"])
                          if dr == 1:
                              p.dma("sp", ra_[:], racc[tok, :], reads=["racc%d" % t], writes=[bk + "ra"])
                          psS = lambda h: ps[0][:, h * 128:(h + 1) * 128] if h < 4 else ps[1][:, (h - 4) * 128:(h - 3) * 128]
                          psY = lambda h: ps[2][:, h * 128:(h + 1) * 128] if h < 4 else ps[3][:, (h - 4) * 128:(h - 3) * 128]
                          kS = lambda h: psk[0] if h < 4 else psk[1]
                          kY = lambda h: psk[2] if h < 4 else psk[3]
                          for h in range(6):
                              j, hh = h // 2, h % 2
                              p.op("pe", lambda e, h=h, j=j, hh=hh, kt_=kt_, qt_=qt_, psS=psS: e.matmul(
                                  psS(h), kt_[:, h, :], qt_[:, h, :], start=True, stop=True),
                                  [bk + "q", bk + "k"], [kS(h)])
                          p.op("dve", lambda e, dr=dr: e.tensor_tensor(
                              PTr[:, 0:4, :], ps[0][:, 0:512].rearrange("p (a b) -> p a b", b=128), maskT[:, dr, 0:4, :], ALU.mult),
                              [psk[0], "maskT"], ["PTr"])
                          p.op("dve", lambda e, dr=dr: e.tensor_tensor(
                              PTr[:, 4:6, :], ps[1][:, 0:256].rearrange("p (a b) -> p a b", b=128), maskT[:, dr, 4:6, :], ALU.mult),
                              [psk[1], "maskT"], ["PTr"])
                          if RS < 2:
                              continue
                          p.op("pool", lambda e, dr=dr, qt_=qt_: e.tensor_tensor(qd[:], qt_[:], qdec[:, dr], ALU.mult), [bk + "q", "qdec"], ["qd"])
                          p.op("pool", lambda e, dr=dr, km_=km_: e.tensor_tensor(
                              kd[:], km_[:].rearrange("p (a b) -> p a b", b=64), V(kdf, dr * 6, 128, [1, 6], [0, 64]), ALU.mult),
                              [bk + "km", "kdf"], ["kd"])
                          for h in range(6):
                              j, hh = h // 2, h % 2
                              p.op("pe", lambda e, h=h, vt_=vt_, psY=psY: e.matmul(
                                  psY(h), PTr[:, h, :], vt_[:, h * 128:(h + 1) * 128], start=True, stop=False), ["PTr", bk + "v"], [kY(h)])
                              p.op("pe", lambda e, h=h, j=j, hh=hh, psY=psY: e.matmul(
                                  psY(h), qd[:, h, :], Sb[:, h, :],
                                  start=False, stop=True), ["qd", "Sb"], [kY(h)])
                          if RS < 3:
                              continue
                          for h in range(6):
                              ub, uo = (4, h * 128) if h < 4 else (5, (h - 4) * 128)
                              p.op("pe", lambda e, h=h, ub=ub, uo=uo, vt_=vt_: e.matmul(
                                  ps[ub][0:64, uo:uo + 128], kd[:, h, :], vt_[:, h * 128:(h + 1) * 128], start=True, stop=True), ["kd", bk + "v"], [psk[ub]])
                          p.op("dve", lambda e, dr=dr: e.tensor_tensor(Stmp[:], S[:], gCt[:, dr], ALU.mult), ["S", "gCt"], ["Stmp"])
                          p.op("dve", lambda e: e.tensor_tensor(S[:, 0:4, :], Stmp[:, 0:4, :],
                                                                ps[4][0:64, 0:512].rearrange("p (a b) -> p a b", b=128), ALU.add), ["Stmp", psk[4]], ["S"])
                          p.op("dve", lambda e: e.tensor_tensor(S[:, 4:6, :], Stmp[:, 4:6, :],
                                                                ps[5][0:64, 0:256].rearrange("p (a b) -> p a b", b=128), ALU.add), ["Stmp", psk[5]], ["S"])
                          p.op("act", lambda e: e.copy(Sb[:], S[:]), ["S"], ["Sb"])
                          if RS < 4:
                              continue
                          for h in range(6):
                              p.op("dve", lambda e, h=h, psY=psY: e.bn_stats(st6[:, h, :], psY(h)), [kY(h)], ["st6"])
                          for h in range(6):
                              p.op("dve", lambda e, h=h: e.bn_aggr(mv6[:, h, :], st6[:, h, :]), ["st6"], ["mv6"])
                          p.op("dve", lambda e: e.tensor_scalar(rs6[:], V(mv6, 1, 128, [2, 6]), EPS, None, ALU.add), ["mv6"], ["rs6"])
                          p.op("act", lambda e: e.activation(rs6[:], rs6[:], AF.Sqrt), ["rs6"], ["rs6"])
                          p.op("dve", lambda e: e.reciprocal(rs6[:], rs6[:]), ["rs6"], ["rs6"])
                          p.op("dve", lambda e: e.tensor_tensor(y1[:, 0:4, :], ps[2][:, 0:512].rearrange("p (a b) -> p a b", b=128),
                                                                V(mv6, 0, 128, [2, 4], [0, 128]), ALU.subtract), [psk[2], "mv6"], ["y1"])
                          p.op("dve", lambda e: e.tensor_tensor(y1[:, 4:6, :], ps[3][:, 0:256].rearrange("p (a b) -> p a b", b=128),
                                                                V(mv6, 8, 128, [2, 2], [0, 128]), ALU.subtract), [psk[3], "mv6"], ["y1"])
                          p.op("pool", lambda e: e.tensor_tensor(y2[:], y1[:], V(rs6, 0, 128, [1, 6], [0, 128]), ALU.mult), ["y1", "rs6"], ["y2"])
                          p.op("pool", lambda e, gt_=gt_: e.tensor_tensor(y1[:].rearrange("p a b -> p (a b)"),
                                                                          y2[:].rearrange("p a b -> p (a b)"), gt_[:], ALU.mult),
                               ["y2", bk + "g"], ["y1"])
                          if RS < 5:
                              continue
                          if dr == 0:
                              p.dma("sp", racc[tok, :], y1[:].rearrange("p a b -> p (a b)"), reads=["y1"], writes=["racc%d" % t])
                          else:
                              p.op("dve", lambda e, ra_=ra_: e.tensor_tensor(yb[:], y1[:].rearrange("p a b -> p (a b)"), ra_[:], ALU.add),
                                   ["y1", bk + "ra"], ["yb"])
                              for h in range(6):
                                  p.op("pe", lambda e, h=h: e.transpose(psb[6][:, h * 128:(h + 1) * 128], yb[:, h * 128:(h + 1) * 128], identb[:]),
                                       ["yb", "identb"], [psk[6]])
                              p.op("act", lambda e: e.copy(tT2[:].rearrange("p a b -> p (a b)"), psb[6][:, 0:768]), [psk[6]], ["tT2"])
                              p.dma("sp", catT[6:12, :, tok].rearrange("h p t -> p h t"), tT2[:], reads=["tT2"])
                  p.flush()
            if "P5" in dbg:
                break

            with ExitStack() as ph:
                wo = SB("wo", [128, 16, D], BF16, ph)
                g1 = [SB("g1_%d" % m, [128, D], F32, ph) for m in range(2)]
                lnG = SB("lnG", [128, D], F32, ph)
                lnB = SB("lnB", [128, D], F32, ph)
                cT = [SB("cT%d" % i, [128, 16, 128], BF16, ph) for i in range(2)]
                xt2 = [SB("xo%d" % i, [128, D], F32, ph) for i in range(2)]
                ygL = [SB("yg%d" % i, [128, D], F32, ph) for i in range(2)]
                B6 = dict(stt=SB("stt6", [128, 4, 6], F32, ph), mv=SB("mvo", [128, 2], F32, ph),
                          rstd=SB("rstdo", [128, 1], F32, ph), nmr=SB("nmro", [128, 1], F32, ph))
                wov = w_out[l].rearrange("(kc p) n -> p kc n", p=128)
                for q in range(4):
                    p.dma("pool", wo[:, q * 4:(q + 1) * 4, :], wov[:, q * 4:(q + 1) * 4, :], writes=["wo"])
                for m in range(2):
                    p.dma("sp", g1[m][:], gsc[l, 0, m:m + 1, :].to_broadcast([128, D]), writes=["g1"])
                p.dma("sp", lnG[:], ln_g[0][l:l + 1, :].to_broadcast([128, D]), writes=["lnG"])
                p.dma("sp", lnB[:], ln_b[0][l:l + 1, :].to_broadcast([128, D]), writes=["lnB"])
                for t in range(NT):
                    tok = slice(t * 128, (t + 1) * 128)
                    m = 1 if t < 2 else 0
                    c_, ck = cT[t % 2], "cT%d" % (t % 2)
                    x_, xk = xt2[t % 2], "xo%d" % (t % 2)
                    yg, ygk = ygL[t % 2], "yg%d" % (t % 2)
                    p.dma("sp", c_[:], catT[:, :, tok].rearrange("k p t -> p k t"), writes=[ck])
                    p.dma("sp", x_[:], blk_src(l, t), reads=["xres%d" % t], writes=[xk])
                    for oc in range(4):
                        for kc in range(16):
                            p.op("pe", lambda e, oc=oc, kc=kc, c_=c_: e.matmul(
                                ps[oc][:, :], c_[:, kc, :], wo[:, kc, oc * 512:(oc + 1) * 512], start=(kc == 0), stop=(kc == 15)),
                                [ck, "wo"], [psk[oc]])
                    for oc in range(4):
                        p.op("dve", lambda e, oc=oc, m=m, yg=yg: e.tensor_tensor(yg[:, oc * 512:(oc + 1) * 512], ps[oc][:, :],
                                                                          g1[m][:, oc * 512:(oc + 1) * 512], ALU.mult),
                             [psk[oc], "g1"], [ygk])
                    p.op("dve", lambda e, x_=x_, yg=yg: e.scalar_tensor_tensor(yg[:], x_[:], ALPHA, yg[:], ALU.mult, ALU.add), [xk, ygk], [ygk])
                    ln_stats(yg, ygk, B6["stt"], B6["mv"], B6["rstd"], B6["nmr"], "o")
                    p.op("act", lambda e, x_=x_, yg=yg: e.activation(x_[:], yg[:], AF.Identity, bias=B6["nmr"][:], scale=B6["rstd"][:]),
                         [ygk, "onmr", "orstd"], [xk])
                    p.op("dve", lambda e, x_=x_: e.tensor_tensor(x_[:], x_[:], lnG[:], ALU.mult), [xk, "lnG"], [xk])
                    p.op("pool", lambda e, x_=x_: e.tensor_tensor(x_[:], x_[:], lnB[:], ALU.add), [xk, "lnB"], [xk])
                    p.dma("sp", xres[tok, :], x_[:], reads=[xk], writes=["xres%d" % t])
                p.flush()
            if "P6" in dbg:
                break
            NB = 2 * NT + NE
            if l == 0:
                jv_in = din("jv", [128, NB])
                pidx_in = din("pidx", [128, 1])
                h2d = dscr("h2d", [T, D], BF16)
                xslots = dscr("xslots", [NB * 128, D], BF16)
                yslots = dscr("yslots", [NB * 128, D])
                dest_i = SB("dest_i", [128, NT, 2], I32)
                gatew = SB("gatew", [128, NT, 2], F32)
                offs_i = SB("offs_i", [128, NB], I32)
                breg = st.enter_context(nc.gpsimd.register("bndreg"))
                nc.gpsimd.reg_mov(breg, DEPTH * NE * 128 - 1)
                bnd_val = [nc.gpsimd.snap(breg)]
            with ExitStack() as ph:
                B = dict(xt=[SB("xt%d" % i, [128, D], F32, ph) for i in range(2)], xn=SB("xn", [128, D], F32, ph),
                         stt=SB("stt", [128, 4, 6], F32, ph), mv=SB("mv", [128, 2], F32, ph),
                         rstd=SB("rstd", [128, 1], F32, ph), nmr=SB("nmr", [128, 1], F32, ph))
                scb = [SB("scb%d" % m, [128, D], F32, ph) for m in range(2)]
                shb = [SB("shb%d" % m, [128, D], F32, ph) for m in range(2)]
                h2 = SB("h2", [128, D], F32, ph)
                h2b = [SB("h2b%d" % i, [128, D], BF16, ph) for i in range(2)]
                h2T = SB("h2T", [128, 16, 128], F32, ph)
                wr = SB("wr", [128, 16, 36], F32, ph)
                brt = SB("brt", [128, 36], F32, ph)
                lg = SB("lg", [128, 36], F32, ph)
                sm = SB("sm", [128, 16], F32, ph)
                ohg = SB("ohg", [128, 4], F32, ph)
                ge = SB("ge", [128, 4], F32, ph)
                ein = SB("ein", [128, 8], F32, ph)
                ein2 = SB("ein2", [128, 8], F32, ph)
                oh8 = SB("oh8", [128, 2, 8], F32, ph)
                ohall = SB("ohall", [128, NT, 2, 32], F32, ph)
                At = SB("At", [128, 32], BF16, ph)
                Us = SB("Us", [128, 128], BF16, ph)
                base = SB("base", [128, 32], F32, ph)
                rank = SB("rank", [128, NT, 32], F32, ph)
                tmpr = SB("tmpr", [128, NT, 32], F32, ph)
                cs = [SB("cs%d" % i, [128, 32], F32, ph) for i in range(2)]
                padd = SB("padd", [128, 32], F32, ph)
                pst = SB("pst", [128, 32], F32, ph)
                dest_f = SB("dest_f", [128, NT, 2], F32, ph)
                jv = SB("jv", [128, NB], F32, ph)
                pidx = SB("pidx", [128, 1], F32, ph)
                cmp_ = SB("cmp", [128, NB, 32], F32, ph)
                be = SB("be", [128, NB], F32, ph)
                same = SB("same", [128, NB], F32, ph)
                zt = SB("zt", [128, D], BF16, ph)
                p.dma("sp", jv[:], jv_in, writes=["jv"])
                p.dma("sp", pidx[:], pidx_in, writes=["pidx"])
                p.op("dve", lambda e: e.tensor_copy(Us[:], cst[:, 5, :]), ["cst"], ["Us"])
                p.op("dve", lambda e: e.memset(base[:], 0.0), [], ["base"])
                p.op("pool", lambda e: e.memset(zt[:], 0.0), [], ["zt"])
                for j in range(NB):
                    p.dma("sp", xslots[j * 128:(j + 1) * 128, :], zt[:], reads=["zt"], writes=["xslots"])
                for m in range(2):
                    p.dma("sp", shb[m][:], gsc[l, 2, m:m + 1, :].to_broadcast([128, D]), writes=["shb"])
                    p.dma("sp", scb[m][:], gsc[l, 3, m:m + 1, :].to_broadcast([128, D]), writes=["scb"])
                p.dma("sp", wr[:, :, 0:4], w_grp[l].rearrange("(kc p) n -> p kc n", p=128), writes=["wr"])
                p.dma("sp", wr[:, :, 4:36], w_exp[l].rearrange("(kc p) n -> p kc n", p=128), writes=["wr"])
                p.dma("sp", brt[:, 0:4], b_grp[l:l + 1, :].to_broadcast([128, 4]), writes=["brt"])
                p.dma("sp", brt[:, 4:36], b_exp[l:l + 1, :].to_broadcast([128, 32]), writes=["brt"])
                for t in range(NT):
                    tok = slice(t * 128, (t + 1) * 128)
                    m = 1 if t < 2 else 0
                    xt, xk = B["xt"][t % 2], "xt%d" % (t % 2)
                    hb, hbk = h2b[t % 2], "h2b%d" % (t % 2)
                    p.dma("sp", xt[:], xres[tok, :], writes=[xk])
                    ln_stats(xt, xk, B["stt"], B["mv"], B["rstd"], B["nmr"], "l")
                    xn = B["xn"]
                    p.op("act", lambda e, xt=xt: e.activation(xn[:], xt[:], AF.Identity, bias=B["nmr"][:], scale=B["rstd"][:]),
                         [xk, "lnmr", "lrstd"], ["xn"])
                    p.op("dve", lambda e, m=m: e.tensor_tensor(h2[:], xn[:], scb[m][:], ALU.mult), ["xn", "scb"], ["h2"])
                    p.op("pool", lambda e, m=m: e.tensor_tensor(h2[:], h2[:], shb[m][:], ALU.add), ["h2", "shb"], ["h2"])
                    p.op("act", lambda e, hb=hb: e.copy(hb[:], h2[:]), ["h2"], [hbk])
                    p.dma("sp", h2d[tok, :], hb[:], reads=[hbk], writes=["h2d%d" % t])
                    for q in range(4):
                        bk = 4 + q
                        for k4 in range(4):
                            kc = q * 4 + k4
                            p.op("pe", lambda e, kc=kc, k4=k4, bk=bk: e.transpose(
                                ps[bk][:, k4 * 128:(k4 + 1) * 128], h2[:, kc * 128:(kc + 1) * 128], ident[:]), ["h2", "ident"], [psk[bk]])
                        p.op("act" if q % 2 else "dve", lambda e, q=q, bk=bk: e.tensor_copy(
                            h2T[:, q * 4:(q + 1) * 4, :].rearrange("p a b -> p (a b)"), ps[bk][:, :]) if q % 2 == 0 else e.copy(
                            h2T[:, q * 4:(q + 1) * 4, :].rearrange("p a b -> p (a b)"), ps[bk][:, :]), [psk[bk]], ["h2T"])
                    for kc in range(16):
                        p.op("pe", lambda e, kc=kc: e.matmul(ps[0][:, 0:36], h2T[:, kc, :], wr[:, kc, :], start=(kc == 0), stop=(kc == 15)),
                             ["h2T", "wr"], [psk[0]])
                    p.op("dve", lambda e: e.tensor_tensor(lg[:], ps[0][:, 0:36], brt[:], ALU.add), [psk[0], "brt"], ["lg"])
                    D_ = lambda fn, r, w: p.op("dve", fn, r, w)
                    D_(lambda e: e.reduce_max(sm[:, 0:1], lg[:, 0:4], axis=AX.X), ["lg"], ["sm"])
                    D_(lambda e: e.tensor_scalar(ohg[:], lg[:, 0:4], sm[:, 0:1], None, ALU.is_equal), ["lg", "sm"], ["ohg"])
                    D_(lambda e: e.tensor_scalar(sm[:, 1:2], sm[:, 0:1], -1.0, None, ALU.mult), ["sm"], ["sm"])
                    p.op("act", lambda e: e.activation(ge[:], lg[:, 0:4], AF.Exp, bias=sm[:, 1:2], scale=1.0), ["lg", "sm"], ["ge"])
                    D_(lambda e: e.reduce_sum(sm[:, 2:3], ge[:], axis=AX.X), ["ge"], ["sm"])
                    D_(lambda e: e.reciprocal(sm[:, 3:4], sm[:, 2:3]), ["sm"], ["sm"])
                    D_(lambda e: e.tensor_scalar(ein[:], lg[:, 4:12], ohg[:, 0:1], None, ALU.mult), ["lg", "ohg"], ["ein"])
                    for g in range(1, 4):
                        D_(lambda e, g=g: e.scalar_tensor_tensor(ein[:], lg[:, 4 + g * 8:12 + g * 8], ohg[:, g:g + 1], ein[:], ALU.mult, ALU.add),
                           ["lg", "ohg", "ein"], ["ein"])
                    D_(lambda e: e.reduce_max(sm[:, 4:5], ein[:], axis=AX.X), ["ein"], ["sm"])
                    D_(lambda e: e.tensor_scalar(oh8[:, 0, :], ein[:], sm[:, 4:5], None, ALU.is_equal), ["ein", "sm"], ["oh8"])
                    D_(lambda e: e.scalar_tensor_tensor(ein2[:], oh8[:, 0, :], -1e30, ein[:], ALU.mult, ALU.add), ["oh8", "ein"], ["ein2"])
                    D_(lambda e: e.reduce_max(sm[:, 5:6], ein2[:], axis=AX.X), ["ein2"], ["sm"])
                    D_(lambda e: e.tensor_scalar(oh8[:, 1, :], ein2[:], sm[:, 5:6], None, ALU.is_equal), ["ein2", "sm"], ["oh8"])
                    D_(lambda e: e.tensor_tensor(sm[:, 6:7], sm[:, 5:6], sm[:, 4:5], ALU.subtract), ["sm"], ["sm"])
                    p.op("act", lambda e: e.activation(sm[:, 7:8], sm[:, 6:7], AF.Exp), ["sm"], ["sm"])
                    D_(lambda e: e.tensor_scalar(sm[:, 8:9], sm[:, 7:8], 1.0, None, ALU.add), ["sm"], ["sm"])
                    D_(lambda e: e.reciprocal(sm[:, 9:10], sm[:, 8:9]), ["sm"], ["sm"])
                    D_(lambda e: e.tensor_tensor(sm[:, 10:11], sm[:, 7:8], sm[:, 9:10], ALU.mult), ["sm"], ["sm"])
                    D_(lambda e, t=t: e.tensor_tensor(gatew[:, t, 0:1], sm[:, 9:10], sm[:, 3:4], ALU.mult), ["sm"], ["gatew"])
                    D_(lambda e, t=t: e.tensor_tensor(gatew[:, t, 1:2], sm[:, 10:11], sm[:, 3:4], ALU.mult), ["sm"], ["gatew"])
                    for k in range(2):
                        for g in range(4):
                            D_(lambda e, t=t, k=k, g=g: e.tensor_scalar(ohall[:, t, k, g * 8:(g + 1) * 8], oh8[:, k, :], ohg[:, g:g + 1], None, ALU.mult),
                               ["oh8", "ohg"], ["ohall"])
                    D_(lambda e, t=t: e.tensor_tensor(At[:], ohall[:, t, 0, :], ohall[:, t, 1, :], ALU.add), ["ohall"], ["At"])
                    p.op("pe", lambda e: e.matmul(ps[1][:, 0:32], Us[:], At[:], start=True, stop=True), ["Us", "At"], [psk[1]])
                    p.op("pe", lambda e: e.matmul(ps[2][:, 0:32], onesb[:], At[:], start=True, stop=True), ["onesb", "At"], [psk[2]])
                    D_(lambda e, t=t: e.tensor_tensor(rank[:, t, :], ps[1][:, 0:32], base[:], ALU.add), [psk[1], "base"], ["rank"])
                    D_(lambda e: e.tensor_tensor(base[:], ps[2][:, 0:32], base[:], ALU.add), [psk[2], "base"], ["base"])
                D_(lambda e: e.tensor_tensor(V(cmp_, 0, 128, [NB, 32], [1, NB]), V(base, 0, 128, [1, 32], [0, NB]),
                                             V(jv, 0, 128, [0, 32], [1, NB]), ALU.is_gt), ["base", "jv"], ["cmp"])
                D_(lambda e: e.reduce_sum(padd[:], V(cmp_, 0, 128, [NB, 32], [1, NB]), axis=AX.X), ["cmp"], ["padd"])
                D_(lambda e: e.tensor_scalar(padd[:], padd[:], 128.0, None, ALU.mult), ["padd"], ["padd"])
                D_(lambda e: e.tensor_copy(cs[0][:], padd[:]), ["padd"], ["cs0"])
                cur = 0
                for sh in (1, 2, 4, 8, 16):
                    a, b = cs[cur], cs[1 - cur]
                    ak, bkk = "cs%d" % cur, "cs%d" % (1 - cur)
                    D_(lambda e, a=a, b=b: e.tensor_copy(b[:], a[:]), [ak], [bkk])
                    D_(lambda e, a=a, b=b, sh=sh: e.tensor_tensor(b[:, sh:32], a[:, sh:32], a[:, 0:32 - sh], ALU.add), [ak], [bkk])
                    cur = 1 - cur
                ends, ek = cs[cur], "cs%d" % cur
                D_(lambda e: e.tensor_tensor(pst[:], ends[:], padd[:], ALU.subtract), [ek, "padd"], ["pst"])
                D_(lambda e: e.tensor_tensor(rank[:], rank[:], V(pst, 0, 128, [0, NT], [1, 32]), ALU.add), ["rank", "pst"], ["rank"])
                for k in range(2):
                    D_(lambda e, k=k: e.tensor_tensor(tmpr[:], ohall[:, :, k, :], rank[:], ALU.mult), ["ohall", "rank"], ["tmpr"])
                    D_(lambda e, k=k: e.reduce_sum(dest_f[:, :, k], tmpr[:], axis=AX.X), ["tmpr"], ["dest_f"])
                D_(lambda e: e.tensor_copy(dest_i[:], dest_f[:]), ["dest_f"], ["dest_i"])
                D_(lambda e: e.tensor_tensor(cmp_[:], V(ends, 0, 128, [0, NB], [1, 32]), V(jv, 0, 128, [1, NB], [0, 32]), ALU.is_le),
                   [ek, "jv"], ["cmp"])
                D_(lambda e: e.reduce_sum(be[:], cmp_[:], axis=AX.X), ["cmp"], ["be"])
                D_(lambda e: e.tensor_scalar(be[:], be[:], 31.0, None, ALU.min), ["be"], ["be"])
                D_(lambda e: e.memset(same[:], 0.0), [], ["same"])
                D_(lambda e: e.tensor_tensor(same[:, 1:NB], be[:, 1:NB], be[:, 0:NB - 1], ALU.is_equal), ["be"], ["same"])
                D_(lambda e: e.tensor_scalar(be[:], be[:], 128.0, None, ALU.mult), ["be"], ["be"])
                D_(lambda e: e.scalar_tensor_tensor(be[:], same[:], 1.0e6, be[:], ALU.mult, ALU.add), ["be", "same"], ["be"])
                D_(lambda e: e.tensor_scalar(be[:], be[:], pidx[:, 0:1], float(l * NE * 128), ALU.add, ALU.add), ["be", "pidx"], ["be"])
                D_(lambda e: e.tensor_copy(offs_i[:], be[:]), ["be"], ["offs_i"])
                p.flush()
                for t in range(NT):
                    tok = slice(t * 128, (t + 1) * 128)
                    hb, hbk = h2b[t % 2], "h2b%d" % (t % 2)
                    p.dma("sp", hb[:], h2d[tok, :], reads=["h2d%d" % t], writes=[hbk])
                    for k in range(2):
                        p.op("pool", lambda e, t=t, k=k, hb=hb: e.indirect_dma_start(
                            out=xslots, out_offset=bass.IndirectOffsetOnAxis(ap=dest_i[:, t, k:k + 1], axis=0),
                            in_=hb[:], in_offset=None), [hbk, "dest_i", "xslots"], [], dma=True)
                p.flush()

            with ExitStack() as ph:
                Wg = SB("Wg", [128, 16, EH], BF16, ph)
                Wu = SB("Wu", [128, 16, EH], BF16, ph)
                Wd = SB("Wd", [128, 8, D], BF16, ph)
                xb = [SB("xb%d" % i, [128, D], BF16, ph) for i in range(2)]
                xT = SB("xT", [128, 16, 128], BF16, ph)
                sg = SB("sg", [128, EH], F32, ph)
                act_ = SB("act", [128, EH], BF16, ph)
                actT = SB("actT", [128, 8, 128], BF16, ph)
                yb = [SB("yb%d" % i, [128, D], F32, ph) for i in range(2)]
                wg_tab = w_gate.rearrange("l e (p kc) n -> (l e p) (kc n)", kc=16)
                wu_tab = w_up.rearrange("l e (p kc) n -> (l e p) (kc n)", kc=16)
                wd_tab = w_down.rearrange("l e (p hc) n -> (l e p) (hc n)", hc=8)
                for j in range(NB):
                    x_, xk = xb[j % 2], "xb%d" % (j % 2)
                    y_, yk = yb[j % 2], "yb%d" % (j % 2)
                    for (W_, tab, wk) in ((Wg, wg_tab, "Wg"), (Wu, wu_tab, "Wu"), (Wd, wd_tab, "Wd")):
                        p.op("pool", lambda e, W_=W_, tab=tab, j=j: e.indirect_dma_start(
                            out=W_[:].rearrange("p a b -> p (a b)"), out_offset=None, in_=tab,
                            in_offset=bass.IndirectOffsetOnAxis(ap=offs_i[:, j:j + 1], axis=0),
                            bounds_check=bnd_val[0], oob_is_err=False), ["offs_i"], [wk], dma=True)
                    p.dma("sp", x_[:], xslots[j * 128:(j + 1) * 128, :], writes=[xk])
                    for kc in range(16):
                        p.op("pe", lambda e, kc=kc, x_=x_: e.transpose(
                            psb[4 + kc // 8][:, (kc % 8) * 128:(kc % 8 + 1) * 128], V(x_, kc, 128, [16, 128]), identb[:]),
                            [xk, "identb"], [psk[4 + kc // 8]])
                    p.op("act", lambda e: e.copy(xT[:, 0:8, :].rearrange("p a b -> p (a b)"), psb[4][:, :]), [psk[4]], ["xT"])
                    p.op("dve", lambda e: e.tensor_copy(xT[:, 8:16, :].rearrange("p a b -> p (a b)"), psb[5][:, :]), [psk[5]], ["xT"])
                    for wi_, (W_, wk) in enumerate(((Wg, "Wg"), (Wu, "Wu"))):
                        for nh in range(2):
                            bk = wi_ * 2 + nh
                            for kc in range(16):
                                p.op("pe", lambda e, W_=W_, nh=nh, kc=kc, bk=bk: e.matmul(
                                    ps[bk][:, :], xT[:, kc, :], W_[:, kc, nh * 512:(nh + 1) * 512], start=(kc == 0), stop=(kc == 15)),
                                    ["xT", wk], [psk[bk]])
                    for nh in range(2):
                        p.op("act", lambda e, nh=nh: e.activation(sg[:, nh * 512:(nh + 1) * 512], ps[nh][:, :], AF.Silu), [psk[nh]], ["sg"])
                        p.op("dve", lambda e, nh=nh: e.tensor_tensor(act_[:, nh * 512:(nh + 1) * 512], sg[:, nh * 512:(nh + 1) * 512],
                                                                     ps[2 + nh][:, :], ALU.mult), ["sg", psk[2 + nh]], ["act"])
                    for hc in range(8):
                        p.op("pe", lambda e, hc=hc: e.transpose(psb[6][:, hc * 128:(hc + 1) * 128], V(act_, hc, 128, [8, 128]), identb[:]),
                             ["act", "identb"], [psk[6]])
                    p.op("act", lambda e: e.copy(actT[:].rearrange("p a b -> p (a b)"), psb[6][:, :]), [psk[6]], ["actT"])
                    for oc in range(4):
                        bk = oc
                        for hc in range(8):
                            p.op("pe", lambda e, oc=oc, hc=hc, bk=bk: e.matmul(
                                ps[bk][:, :], actT[:, hc, :], Wd[:, hc, oc * 512:(oc + 1) * 512], start=(hc == 0), stop=(hc == 7)),
                                ["actT", "Wd"], [psk[bk]])
                        p.op("act" if oc % 2 else "dve", (lambda e, oc=oc, bk=bk, y_=y_: e.copy(y_[:, oc * 512:(oc + 1) * 512], ps[bk][:, :])) if oc % 2
                             else (lambda e, oc=oc, bk=bk, y_=y_: e.tensor_copy(y_[:, oc * 512:(oc + 1) * 512], ps[bk][:, :])), [psk[bk]], [yk])
                    p.dma("sp", yslots[j * 128:(j + 1) * 128, :], y_[:], reads=[yk])
                p.flush()

            with ExitStack() as ph:
                g2 = [SB("g2_%d" % m, [128, D], F32, ph) for m in range(2)]
                lnG = SB("lnG2", [128, D], F32, ph)
                lnB = SB("lnB2", [128, D], F32, ph)
                ya = [SB("ya%d" % i, [128, D], F32, ph) for i in range(2)]
                yc = [SB("yc%d" % i, [128, D], F32, ph) for i in range(2)]
                xo = [SB("xq%d" % i, [128, D], F32, ph) for i in range(2)]
                B6 = dict(stt=SB("stt7", [128, 4, 6], F32, ph), mv=SB("mv7", [128, 2], F32, ph),
                          rstd=SB("rstd7", [128, 1], F32, ph), nmr=SB("nmr7", [128, 1], F32, ph))
                for m in range(2):
                    p.dma("sp", g2[m][:], gsc[l, 1, m:m + 1, :].to_broadcast([128, D]), writes=["g2"])
                p.dma("sp", lnG[:], ln_g[1][l:l + 1, :].to_broadcast([128, D]), writes=["lnG"])
                p.dma("sp", lnB[:], ln_b[1][l:l + 1, :].to_broadcast([128, D]), writes=["lnB"])
                for t in range(NT):
                    if l == DEPTH - 1 and t < 2:
                        continue
                    tok = slice(t * 128, (t + 1) * 128)
                    m = 1 if t < 2 else 0
                    i_ = t % 2
                    for k, (yy, ykk) in enumerate(((ya[i_], "ya%d" % i_), (yc[i_], "yc%d" % i_))):
                        p.op("pool", lambda e, t=t, k=k, yy=yy: e.indirect_dma_start(
                            out=yy[:], out_offset=None, in_=yslots,
                            in_offset=bass.IndirectOffsetOnAxis(ap=dest_i[:, t, k:k + 1], axis=0)), ["dest_i"], [ykk], dma=True)
                    x_, xk = xo[i_], "xq%d" % i_
                    p.dma("sp", x_[:], xres[tok, :], reads=["xres%d" % t], writes=[xk])
                    y1_, y2_ = ya[i_], yc[i_]
                    p.op("dve", lambda e, t=t, y1_=y1_: e.tensor_scalar(y1_[:], y1_[:], gatew[:, t, 0:1], None, ALU.mult), ["ya%d" % i_, "gatew"], ["ya%d" % i_])
                    p.op("dve", lambda e, t=t, y1_=y1_, y2_=y2_: e.scalar_tensor_tensor(y1_[:], y2_[:], gatew[:, t, 1:2], y1_[:], ALU.mult, ALU.add),
                         ["ya%d" % i_, "yc%d" % i_, "gatew"], ["ya%d" % i_])
                    p.op("pool", lambda e, y1_=y1_, m=m: e.tensor_tensor(y1_[:], y1_[:], g2[m][:], ALU.mult), ["ya%d" % i_, "g2"], ["ya%d" % i_])
                    p.op("dve", lambda e, x_=x_, y1_=y1_: e.scalar_tensor_tensor(y1_[:], x_[:], ALPHA, y1_[:], ALU.mult, ALU.add),
                         [xk, "ya%d" % i_], ["ya%d" % i_])
                    ln_stats(y1_, "ya%d" % i_, B6["stt"], B6["mv"], B6["rstd"], B6["nmr"], "q")
                    p.op("act", lambda e, x_=x_, y1_=y1_: e.activation(x_[:], y1_[:], AF.Identity, bias=B6["nmr"][:], scale=B6["rstd"][:]),
                         ["ya%d" % i_, "qnmr", "qrstd"], [xk])
                    p.op("dve", lambda e, x_=x_: e.tensor_tensor(x_[:], x_[:], lnG[:], ALU.mult), [xk, "lnG"], [xk])
                    p.op("pool", lambda e, x_=x_: e.tensor_tensor(x_[:], x_[:], lnB[:], ALU.add), [xk, "lnB"], [xk])
                    if l == DEPTH - 1:
                        p.dma("sp", out_d[(t - 2) * 128:(t - 1) * 128, :], x_[:], reads=[xk])
                    else:
                        p.dma("sp", xres[tok, :], x_[:], reads=[xk], writes=["xres%d" % t])
                p.flush()
            if "L0" in dbg:
                break
        p.finish()
        print("instructions:", p.n_inst)
    return nc


def _consts(L):
    T = NCTX + L
    f32 = np.float32
    t = np.arange(T)
    lat = t >= NCTX
    i = np.where(lat, t - NCTX, 0)
    row = (i // 64).astype(f32)
    col = (i % 64).astype(f32)
    pos = t.astype(f32)
    fa = (10000.0 ** (-np.arange(32, dtype=f32) / 32)).astype(f32)
    fb = (10000.0 ** (-np.arange(16, dtype=f32) / 16)).astype(f32)

    def cs(pp, fr, mask):
        ang = (pp[:, None] * fr[None, :]).astype(f32)
        c = np.cos(ang).astype(f32)
        s = np.sin(ang).astype(f32)
        if mask is not None:
            c = np.where(mask[:, None], c, 1.0).astype(f32)
            s = np.where(mask[:, None], s, 0.0).astype(f32)
        return c, s
    cr, sr = cs(row, fa, lat)
    cc, sc = cs(col, fa, lat)
    swa = [np.concatenate([cr, cr, cc, cc], 1), np.concatenate([-sr, sr, -sc, sc], 1)]
    cp, sp = cs(pos, fa, None)
    ret = [np.concatenate([cp, cp], 1), np.concatenate([-sp, sp], 1)]
    cr, sr = cs(row, fb, lat)
    cc, sc = cs(col, fb, lat)
    mla = [np.concatenate([cr, cr, cc, cc], 1), np.concatenate([-sr, sr, -sc, sc], 1)]
    rtab = np.ascontiguousarray(np.concatenate(swa + ret + mla, 1).astype(f32))
    s = np.arange(128)[:, None]
    c = np.arange(128)[None, :]
    z = 0 * s + 0 * c
    cst = np.ascontiguousarray(np.stack([(s <= c) + z, (s >= c) + z, (c - s) + z, (c + 1) + z, (128 - c) + z, (s < c) + z], 1).astype(f32))
    scol = np.ascontiguousarray(np.stack([127 - np.arange(128), np.arange(128)], 1).astype(f32))
    NB = 2 * (T // 128) + NE
    jv = np.ascontiguousarray(np.tile((np.arange(NB, dtype=f32) * 128)[None, :], (128, 1)))
    pidx = np.arange(128, dtype=f32).reshape(128, 1)
    return dict(ident=np.eye(128, dtype=f32), rtab=rtab, cst=cst, scol=scol, jv=jv, pidx=pidx)


_WKEYS = ("w_ada", "b_ada", "w_in", "swa_sink", "ret_decay", "mla_kv_norm", "mla_w_uk", "mla_w_uv", "w_out",
          "ln1_g", "ln1_b", "ln2_g", "ln2_b", "moe_w_group", "moe_b_group", "moe_w_expert", "moe_b_expert",
          "moe_w_gate", "moe_w_up", "moe_w_down")


def core_inputs(inp, b, L, consts=None, names=None):
    m = dict(consts if consts is not None else _consts(L))
    m["x"] = np.ascontiguousarray(inp["x"][b], dtype=np.float32)
    m["ctx"] = np.ascontiguousarray(inp["ctx"][b], dtype=np.float32)
    m["c2"] = np.ascontiguousarray(np.stack([inp["c"][b], inp["c_ctx"]], 0), dtype=np.float32)
    for k in _WKEYS:
        if names is None or k in names:
            m[k] = np.ascontiguousarray(inp[k], dtype=np.float32)
    return m


_NC_CACHE = {}


def kernel(**inputs):
    L = int(inputs["x"].shape[1])
    Bn = int(inputs["x"].shape[0])
    if L not in _NC_CACHE:
        _NC_CACHE[L] = build(L)
    nc = _NC_CACHE[L]
    consts = _consts(L)
    inp = {k: np.asarray(v) for k, v in inputs.items()}
    in_maps = [core_inputs(inp, b, L, consts, names=nc._in_names) for b in range(Bn)]
    res = run_bass_kernel_spmd(nc, in_maps, core_ids=list(range(Bn)))
    out = np.stack([np.asarray(res.results[b]["out"]) for b in range(Bn)], 0)
    return out.astype(np.float32)
```

```python
import math
from contextlib import ExitStack
import numpy as np
import ml_dtypes
import concourse.bass as bass
import concourse.mybir as mybir
from concourse.bass_utils import run_bass_kernel_spmd

F32 = mybir.dt.float32
BF16 = mybir.dt.bfloat16
I32 = mybir.dt.int32
AF = mybir.ActivationFunctionType
ALU = mybir.AluOpType
AX = mybir.AxisListType

D = 2048
NCTX = 256
DEPTH = 2
IN_W = 5440
ALPHA = (2 * DEPTH) ** 0.25
EPS = 1e-6
NE = 32
EH = 1024
COMPUTE = ("pe", "act", "dve", "pool")


class Prog:
    def __init__(self, nc, stack, n_dma_sems=24):
        self.nc = nc
        self.eng = {"pe": nc.tensor, "act": nc.scalar, "dve": nc.vector,
                    "pool": nc.gpsimd, "sp": nc.sync}
        self.ops = []
        self.nd = n_dma_sems
        self.csem = {e: stack.enter_context(nc.semaphore("cs_" + e)) for e in COMPUTE}
        self.ccount = {e: 0 for e in COMPUTE}
        self.sem_obj = {("c", e): self.csem[e] for e in COMPUTE}
        self.dstate = {}
        for e in ("sp", "act", "pool"):
            sems = [stack.enter_context(nc.semaphore("ds_%s_%d" % (e, k))) for k in range(n_dma_sems)]
            for k, s in enumerate(sems):
                self.sem_obj[("d", e, k)] = s
            self.dstate[e] = dict(next=0, cnt=[0] * n_dma_sems)
        self.waited = {}
        self.carry = {}
        self.pend = {e: {} for e in self.eng}
        self.n_inst = 0

    def op(self, eng, fn, reads=(), writes=(), dma=False):
        self.ops.append((eng, fn, tuple(reads), tuple(writes), dma))

    def dma(self, eng, out, in_, reads=(), writes=(), **kw):
        self.op(eng, lambda e: e.dma_start(out=out, in_=in_, **kw), reads, writes, dma=True)

    def _wait(self, eng, sk, val):
        key = (eng, sk)
        if self.waited.get(key, 0) >= val:
            return
        self.waited[key] = val
        self.eng[eng].wait_ge(self.sem_obj[sk], val)
        self.n_inst += 1

    def flush(self):
        ops = self.ops
        self.ops = []
        n = len(ops)
        last_w, readers = {}, {}
        deps = [None] * n
        last_on_eng = {}
        dma_ops = []
        for i, (eng, fn, reads, writes, dma) in enumerate(ops):
            d = set()
            for k in reads:
                if k in last_w:
                    d.add(last_w[k])
            for k in writes:
                if k in last_w:
                    d.add(last_w[k])
                r = readers.get(k)
                if r:
                    d.update(r[0].values())
                    d.update(r[1])
            d.discard(i)
            deps[i] = d
            for k in reads:
                r = readers.setdefault(k, ({}, []))
                if dma:
                    r[1].append(i)
                else:
                    r[0][eng] = i
            for k in writes:
                last_w[k] = i
                readers[k] = ({}, [])
            if dma:
                dma_ops.append(i)
            else:
                last_on_eng[eng] = i
        signal = [False] * n
        for i, (eng, fn, reads, writes, dma) in enumerate(ops):
            keep = set()
            for j in deps[i]:
                ej, _, rj, wj, dj = ops[j]
                if (not dj) and ej == eng and not dma:
                    if eng == "pe":
                        continue
                    if not (set(wj) & set(reads)):
                        continue
                keep.add(j)
            deps[i] = keep
            for j in keep:
                signal[j] = True
        for e, j in last_on_eng.items():
            signal[j] = True
        event = [None] * n
        for i, (eng, fn, reads, writes, dma) in enumerate(ops):
            need = {}
            if self.pend[eng]:
                need.update(self.pend[eng])
                self.pend[eng] = {}
            for j in deps[i]:
                sk, val = event[j]
                if need.get(sk, 0) < val:
                    need[sk] = val
            for sk, val in need.items():
                self._wait(eng, sk, val)
            if dma:
                st = self.dstate[eng]
                k = st["next"]
                st["next"] = (k + 1) % self.nd
                sk = ("d", eng, k)
                if st["cnt"][k] > 0:
                    self._wait(eng, sk, st["cnt"][k])
                st["cnt"][k] += 16
                ins = fn(self.eng[eng])
                ins.then_inc(self.sem_obj[sk], 16)
                event[i] = (sk, st["cnt"][k])
            else:
                ins = fn(self.eng[eng])
                if signal[i]:
                    self.ccount[eng] += 1
                    ins.then_inc(self.csem[eng], 1)
                    event[i] = (("c", eng), self.ccount[eng])
            self.n_inst += 1
        carry = {}
        for e in COMPUTE:
            if self.ccount[e] > 0:
                carry[("c", e)] = self.ccount[e]
        for e, st in self.dstate.items():
            for k in range(self.nd):
                if st["cnt"][k] > 0:
                    carry[("d", e, k)] = st["cnt"][k]
        for e in self.eng:
            self.pend[e] = dict(carry)

    def finish(self):
        self.flush()
        for sk, val in self.pend["sp"].items():
            self._wait("sp", sk, val)


def V(t, off, *dims):
    F = 1
    for s in t.shape[1:]:
        F *= s
    npart = dims[0]
    return bass.AP(t, off, [[F, npart]] + [list(d) for d in dims[1:]])


CHUNKS = [("sq0", 0, 512), ("sq1", 512, 256), ("sk", 768, 256), ("sv", 1024, 256),
          ("rq", 1280, 384), ("rk", 1664, 384), ("rv0", 2048, 512), ("rv1", 2560, 256),
          ("gf0", 2816, 512), ("gf1", 3328, 256), ("gb0", 3584, 512), ("gb1", 4096, 256),
          ("mq0", 4352, 384), ("mq1", 4736, 384), ("ckv", 5120, 320)]


def build(L, dbg=()):
    nc = bass.Bass("TRN2", target_bir_lowering=False)
    NT = (NCTX + L) // 128
    T = NT * 128
    NLT = L // 128

    in_names = []
    nc._in_names = in_names

    def din(name, shape, dt=F32):
        in_names.append(name)
        return nc.dram_tensor(name, list(shape), dt, kind="ExternalInput").ap()

    def dscr(name, shape, dt=F32):
        kind = "ExternalOutput" if name in dbg else "Internal"
        return nc.dram_tensor(name, list(shape), dt, kind=kind).ap()

    x_in = din("x", [L, D])
    ctx_in = din("ctx", [NCTX, D])
    c2_in = din("c2", [2, D])
    w_ada = din("w_ada", [DEPTH, D, 6 * D])
    b_ada = din("b_ada", [DEPTH, 6 * D])
    w_in = din("w_in", [DEPTH, D, IN_W])
    swa_sink = din("swa_sink", [DEPTH, 6])
    ret_decay = din("ret_decay", [DEPTH, 2, 6])
    kv_norm = din("mla_kv_norm", [DEPTH, 256])
    w_uk = din("mla_w_uk", [DEPTH, 256, 512])
    w_uv = din("mla_w_uv", [DEPTH, 256, 512])
    w_out = din("w_out", [DEPTH, D, D])
    ln_g = [din("ln1_g", [DEPTH, D]), din("ln2_g", [DEPTH, D])]
    ln_b = [din("ln1_b", [DEPTH, D]), din("ln2_b", [DEPTH, D])]
    w_grp = din("moe_w_group", [DEPTH, D, 4])
    b_grp = din("moe_b_group", [DEPTH, 4])
    w_exp = din("moe_w_expert", [DEPTH, D, NE])
    b_exp = din("moe_b_expert", [DEPTH, NE])
    if not (set(dbg) & {"P2", "P5", "P6"}):
        w_gate = din("moe_w_gate", [DEPTH, NE, D, EH])
        w_up = din("moe_w_up", [DEPTH, NE, D, EH])
        w_down = din("moe_w_down", [DEPTH, NE, EH, D])
    ident_in = din("ident", [128, 128])
    rtab_in = din("rtab", [T, 512])
    out_d = nc.dram_tensor("out", [L, D], F32, kind="ExternalOutput").ap()

    xres = dscr("xres", [T, D])
    gsc = dscr("gsc", [DEPTH, 4, 2, D])
    qTs = dscr("qTs", [6, 128, T], BF16)
    kTs = dscr("kTs", [2, 128, T], BF16)
    vs = dscr("vs", [T, 256], BF16)
    qTr = dscr("qTr", [3, 128, T], BF16)
    kTr = dscr("kTr", [3, 128, T], BF16)
    kr = dscr("kr", [T, 384], BF16)
    vr = dscr("vr", [T, 768], BF16)
    gfb = dscr("gfb", [2, T, 768])
    qnT = dscr("qnT", [4, 128, T], BF16)
    qrT = dscr("qrT", [4, 64, T], BF16)
    knT = dscr("knT", [4, 128, T], BF16)
    krT = dscr("krT", [64, T], BF16)
    vm = dscr("vm", [T, 512], BF16)
    catT = dscr("catT", [16, 128, T], BF16)

    with ExitStack() as st:
        p = Prog(nc, st)
        _uid = [0]

        def SB(name, shape, dt=F32, s=st):
            _uid[0] += 1
            return s.enter_context(nc.sbuf_tensor("s%d_%s" % (_uid[0], name), list(shape), dt))
        ps = [st.enter_context(nc.psum_tensor("ps%d" % i, [128, 512], F32)) for i in range(8)]
        psk = ["ps%d" % i for i in range(8)]
        psb = [b[:].bitcast(BF16) for b in ps]

        ident = SB("ident", [128, 128])
        identb = SB("identb", [128, 128], BF16)
        modcol = SB("modcol", [128, 96, 2])
        scT = SB("scT", [128, 16, 2], BF16)
        p.dma("sp", ident[:], ident_in, writes=["ident"])
        p.op("dve", lambda e: e.tensor_copy(identb[:], ident[:]), ["ident"], ["identb"])

        with ExitStack() as ph:
            c2 = SB("c2", [2, D], F32, ph)
            p.dma("sp", c2[:], c2_in, writes=["c2"])
            p.op("act", lambda e: e.activation(c2[:], c2[:], AF.Silu), ["c2"], ["c2"])
            for kc in range(16):
                p.op("pe", lambda e, kc=kc: e.transpose(ps[0][:, kc * 2:kc * 2 + 2], c2[:, kc * 128:(kc + 1) * 128],
                                                        ident[0:2, 0:2]), ["c2", "ident"], [psk[0]])
            p.op("dve", lambda e: e.tensor_copy(scT[:].rearrange("p a b -> p (a b)"), ps[0][:, 0:32]), [psk[0]], ["scT"])
            p.flush()

        cst_in = din("cst", [128, 6, 128])
        scol_in = din("scol", [128, 2])
        cst = SB("cst", [128, 6, 128])
        scol = SB("scol", [128, 2])
        mle = SB("mle", [128, 128], BF16)
        mge = SB("mge", [128, 128], BF16)
        onesb = SB("onesb", [128, 128], BF16)
        onesf = SB("onesf", [128, 128])
        p.dma("sp", cst[:], cst_in, writes=["cst"])
        p.dma("sp", scol[:], scol_in, writes=["scol"])
        p.op("dve", lambda e: e.tensor_copy(mle[:], cst[:, 0, :]), ["cst"], ["mle"])
        p.op("dve", lambda e: e.tensor_copy(mge[:], cst[:, 1, :]), ["cst"], ["mge"])
        p.op("dve", lambda e: e.memset(onesb[:], 1.0), [], ["onesb"])
        p.op("dve", lambda e: e.memset(onesf[:], 1.0), [], ["onesf"])
        p.flush()

        def blk_src(l, t):
            if l == 0:
                return ctx_in[t * 128:(t + 1) * 128, :] if t < 2 else x_in[(t - 2) * 128:(t - 1) * 128, :]
            return xres[t * 128:(t + 1) * 128, :]

        cnt = [0]

        def ln_stats(xt, xk, stt, mv, rstd, nmr, pre):
            for c4 in range(4):
                p.op("dve", lambda e, c4=c4: e.bn_stats(stt[:, c4, :], xt[:, c4 * 512:(c4 + 1) * 512]), [xk], [pre + "st"])
            p.op("dve", lambda e: e.bn_aggr(mv[:], stt[:].rearrange("p a b -> p (a b)")), [pre + "st"], [pre + "mv"])
            p.op("dve", lambda e: e.tensor_scalar(rstd[:], mv[:, 1:2], EPS, None, ALU.add), [pre + "mv"], [pre + "rstd"])
            p.op("act", lambda e: e.activation(rstd[:], rstd[:], AF.Sqrt), [pre + "rstd"], [pre + "rstd"])
            p.op("dve", lambda e: e.reciprocal(rstd[:], rstd[:]), [pre + "rstd"], [pre + "rstd"])
            p.op("dve", lambda e: e.scalar_tensor_tensor(nmr[:], mv[:, 0:1], -1.0, rstd[:], ALU.mult, ALU.mult),
                 [pre + "mv", pre + "rstd"], [pre + "nmr"])

        def ln_mod_T(src_ap, m, jsh, jsc, hT, hk, slot, B):
            i = cnt[0]
            cnt[0] += 1
            xt, xk = B["xt"][i % 2], "xt%d" % (i % 2)
            p.dma("sp", xt[:], src_ap, writes=[xk])
            ln_stats(xt, xk, B["stt"], B["mv"], B["rstd"], B["nmr"], "l")
            xn = B["xn"]
            p.op("act", lambda e: e.activation(xn[:], xt[:], AF.Identity, bias=B["nmr"][:], scale=B["rstd"][:]),
                 [xk, "lnmr", "lrstd"], ["xn"])
            for q in range(4):
                bk = 4 + q
                for k4 in range(4):
                    kc = q * 4 + k4
                    p.op("pe", lambda e, kc=kc, k4=k4, bk=bk: e.transpose(
                        ps[bk][:, k4 * 128:(k4 + 1) * 128], xn[:, kc * 128:(kc + 1) * 128], ident[:]),
                        ["xn", "ident"], [psk[bk]])
                for k4 in range(4):
                    kc = q * 4 + k4
                    p.op("act", lambda e, kc=kc, k4=k4, bk=bk: e.activation(
                        hT[:, kc, slot * 128:(slot + 1) * 128], ps[bk][:, k4 * 128:(k4 + 1) * 128], AF.Identity,
                        bias=modcol[:, jsh * 16 + kc, m:m + 1], scale=modcol[:, jsc * 16 + kc, m:m + 1]),
                        [psk[bk], "modcol"], [hk])

        def rope(rs, W, H, d, hb, hstride, hoff, tab, tk, cb, sb, r1, r2, rb, x=""):
            nb2 = d // (2 * hb)
            full = lambda t, o=0: V(t, hoff + o, 128, [hstride, H], [2 * hb, nb2], [1, hb])
            p.op("dve", lambda e: e.tensor_tensor(
                V(r1, hoff, 128, [hstride, H], [1, d]), V(rs, hoff, 128, [hstride, H], [1, d]),
                V(tab, cb, 128, [0, H], [1, d]), ALU.mult), ["rs" + x, tk], ["r1" + x])
            for b in range(2):
                p.op("dve", lambda e, b=b: e.tensor_tensor(
                    full(r2, b * hb), full(rs, (1 - b) * hb),
                    V(tab, sb + b * hb, 128, [0, H], [2 * hb, nb2], [1, hb]), ALU.mult), ["rs" + x, tk], ["r2" + x])
            p.op("dve", lambda e: e.tensor_tensor(
                V(rb, hoff, 128, [hstride, H], [1, d]), V(r1, hoff, 128, [hstride, H], [1, d]),
                V(r2, hoff, 128, [hstride, H], [1, d]), ALU.add), ["r1" + x, "r2" + x], ["rb" + x])

        for l in range(DEPTH):
            with ExitStack() as ph:
                wa = [SB("wa%d" % i, [128, 16, 1024], BF16, ph) for i in range(2)]
                bar = SB("bar", [96, 128], F32, ph)
                bcol = SB("bcol", [128, 96], F32, ph)
                rows2 = SB("rows2", [2, 4, D], F32, ph)
                p.dma("sp", bar[:], b_ada[l].rearrange("(a b) -> a b", b=128), writes=["bar"])
                p.op("pe", lambda e: e.transpose(ps[1][:, 0:96], bar[:], ident[0:96, 0:96]), ["bar", "ident"], [psk[1]])
                p.op("dve", lambda e: e.tensor_copy(bcol[:], ps[1][:, 0:96]), [psk[1]], ["bcol"])
                wav = w_ada[l].rearrange("(kc p) n -> p kc n", p=128)
                for g in range(12):
                    w = wa[g % 2]
                    wk = "wa%d" % (g % 2)
                    p.dma("pool", w[:], wav[:, :, g * 1024:(g + 1) * 1024], writes=[wk])
                    for nn in range(8):
                        n = g * 8 + nn
                        for kc in range(16):
                            p.op("pe", lambda e, w=w, nn=nn, n=n, kc=kc: e.matmul(
                                ps[0][:, 2 * n:2 * n + 2], w[:, kc, nn * 128:(nn + 1) * 128], scT[:, kc, :],
                                start=(kc == 0), stop=(kc == 15)), [wk, "scT"], [psk[0]])
                p.op("dve", lambda e: e.tensor_tensor(
                    modcol[:], ps[0][:, 0:192].rearrange("p (a b) -> p a b", b=2),
                    V(bcol, 0, 128, [1, 96], [0, 2]), ALU.add), [psk[0], "bcol"], ["modcol"])
                for j in (1, 4):
                    p.op("dve", lambda e, j=j: e.tensor_scalar_add(modcol[:, j * 16:(j + 1) * 16, :],
                                                                   modcol[:, j * 16:(j + 1) * 16, :], 1.0),
                         ["modcol"], ["modcol"])
                for si, j in enumerate((2, 5, 3, 4)):
                    for kc in range(16):
                        bk = 2 + kc // 4
                        p.op("pe", lambda e, j=j, kc=kc, bk=bk: e.transpose(
                            ps[bk][0:2, (kc % 4) * 128:(kc % 4 + 1) * 128], modcol[:, j * 16 + kc, :], ident[:]),
                            ["modcol", "ident"], [psk[bk]])
                    for q in range(4):
                        p.op("dve", lambda e, si=si, q=q: e.tensor_copy(rows2[:, si, q * 512:(q + 1) * 512], ps[2 + q][0:2, :]),
                             [psk[2 + q]], ["rows2"])
                p.dma("sp", gsc[l].rearrange("s m d -> m s d"), rows2[:], reads=["rows2"], writes=[])
                p.flush()

            with ExitStack() as ph:
                B = dict(xt=[SB("xt%d" % i, [128, D], F32, ph) for i in range(2)], xn=SB("xn", [128, D], F32, ph),
                         stt=SB("stt", [128, 4, 6], F32, ph), mv=SB("mv", [128, 2], F32, ph),
                         rstd=SB("rstd", [128, 1], F32, ph), nmr=SB("nmr", [128, 1], F32, ph))
                hT = SB("hT", [128, 16, 1024], BF16, ph)
                wch = [SB("wch%d" % i, [128, 16, 512], BF16, ph) for i in range(2)]
                rt = [SB("rt%d" % i, [128, 512], F32, ph) for i in range(8)]
                rsL = [SB("rs%d" % i, [128, 768], F32, ph) for i in range(3)]
                r1L = [SB("r1%d" % i, [128, 768], F32, ph) for i in range(3)]
                r2L = [SB("r2%d" % i, [128, 768], F32, ph) for i in range(3)]
                rbL = [SB("rb%d" % i, [128, 768], BF16, ph) for i in range(3)]
                tTL = [SB("tT%d" % i, [128, 6, 128], BF16, ph) for i in range(3)]
                gstL = [SB("gst%d" % i, [128, 512], F32, ph) for i in range(3)]
                cnTL = [SB("cnT%d" % i, [128, 2, 128], BF16, ph) for i in range(3)]
                ssqL = [SB("ssq%d" % i, [128, 1], F32, ph) for i in range(3)]
                gkv = SB("gkv", [128, 256], F32, ph)
                wuk = SB("wuk", [128, 2, 512], BF16, ph)
                wuv = SB("wuv", [128, 2, 512], BF16, ph)
                p.dma("sp", gkv[:], kv_norm[l:l + 1, :].to_broadcast([128, 256]), writes=["gkv"])
                p.dma("pool", wuk[:], w_uk[l].rearrange("(rc p) n -> p rc n", p=128), writes=["wuk"])
                p.dma("pool", wuv[:], w_uv[l].rearrange("(rc p) n -> p rc n", p=128), writes=["wuv"])
                wiv = w_in[l].rearrange("(kc p) n -> p kc n", p=128)
                wi = 0
                mmc = [0]
                pcnt = [0]
                pendq = []
                for g0 in range(0, NT, 8):
                    tblks = list(range(g0, min(g0 + 8, NT)))
                    for s_, t in enumerate(tblks):
                        ln_mod_T(blk_src(l, t), 1 if t < 2 else 0, 0, 1, hT, "hT", s_, B)
                        p.dma("sp", rt[s_][:], rtab_in[t * 128:(t + 1) * 128, :], writes=["rt%d" % s_])
                    for (cname, c0, cw) in CHUNKS:
                        w = wch[wi % 2]
                        wk = "wch%d" % (wi % 2)
                        wi += 1
                        p.dma("pool", w[:, :, 0:cw], wiv[:, :, c0:c0 + cw], writes=[wk])
                        for s_, t in enumerate(tblks):
                            tok = slice(t * 128, (t + 1) * 128)
                            bk = mmc[0] % 3
                            mmc[0] += 1
                            pk = psk[bk]
                            pt = ps[bk]
                            tab, tk = rt[s_], "rt%d" % s_
                            for kc in range(16):
                                p.op("pe", lambda e, kc=kc, s_=s_, w=w, pt=pt, cw=cw: e.matmul(
                                    pt[:, 0:cw], hT[:, kc, s_ * 128:(s_ + 1) * 128], w[:, kc, 0:cw],
                                    start=(kc == 0), stop=(kc == 15)), ["hT", wk], [pk])

                            def post(cname=cname, cw=cw, pt=pt, pk=pk, tab=tab, tk=tk, tok=tok, x=str(pcnt[0] % 3)):
                                rs, r1, r2, rb, tT, gst, cnT, ssq = (rsL[int(x)], r1L[int(x)], r2L[int(x)], rbL[int(x)], tTL[int(x)],
                                                                      gstL[int(x)], cnTL[int(x)], ssqL[int(x)])
                                def transp(nblk, width, dst_ap, srcoff=lambda b: b * 128):
                                    for b in range(nblk):
                                        p.op("pe", lambda e, b=b: e.transpose(
                                            psb[7][0:width, b * 128:(b + 1) * 128], rb[:, srcoff(b):srcoff(b) + width], identb[:]),
                                            ["rb" + x, "identb"], [psk[7]])
                                    p.op("act", lambda e: e.copy(tT[0:width, 0:nblk, :],
                                                                 psb[7][0:width, 0:nblk * 128].rearrange("p (a b) -> p a b", b=128)),
                                         [psk[7]], ["tT" + x])
                                    p.dma("sp", dst_ap, tT[0:width, 0:nblk, :], reads=["tT" + x], writes=[])

                                if cname in ("sq0", "sq1", "sk"):
                                    H = cw // 128
                                    sc = 128 ** -0.5 if cname != "sk" else 1.0
                                    p.op("act", lambda e, pt=pt, cw=cw, sc=sc: e.mul(rs[:, 0:cw], pt[:, 0:cw], sc), [pk], ["rs" + x])
                                    rope(rs, cw, H, 128, 32, 128, 0, tab, tk, 0, 128, r1, r2, rb, x)
                                    if cname == "sk":
                                        dst = kTs[:, :, tok]
                                    else:
                                        h0 = 0 if cname == "sq0" else 4
                                        dst = qTs[h0:h0 + H, :, tok]
                                    transp(H, 128, dst.rearrange("h p t -> p h t"))
                                elif cname in ("sv", "rv0", "rv1"):
                                    p.op("act", lambda e, pt=pt, cw=cw: e.copy(rb[:, 0:cw], pt[:, 0:cw]), [pk], ["rb" + x])
                                    if cname == "sv":
                                        dst = vs[tok, :]
                                    else:
                                        o = 0 if cname == "rv0" else 512
                                        dst = vr[tok, o:o + cw]
                                    p.dma("sp", dst, rb[:, 0:cw], reads=["rb" + x], writes=[])
                                elif cname in ("gf0", "gf1", "gb0", "gb1"):
                                    p.op("act", lambda e, pt=pt, cw=cw: e.activation(gst[:, 0:cw], pt[:, 0:cw], AF.Silu), [pk], ["gst" + x])
                                    o = 0 if cname[2] == "0" else 512
                                    p.dma("sp", gfb[0 if cname[1] == "f" else 1, tok, o:o + cw], gst[:, 0:cw], reads=["gst" + x], writes=[])
                                elif cname in ("rq", "rk"):
                                    sc = 64 ** -0.5 if cname == "rq" else 1.0
                                    p.op("act", lambda e, pt=pt, sc=sc: e.mul(rs[:, 0:384], pt[:, 0:384], sc), [pk], ["rs" + x])
                                    rope(rs, 384, 6, 64, 32, 64, 0, tab, tk, 256, 320, r1, r2, rb, x)
                                    if cname == "rk":
                                        p.dma("sp", kr[tok, :], rb[:, 0:384], reads=["rb" + x], writes=[])
                                    dst = (qTr if cname == "rq" else kTr)[:, :, tok]
                                    transp(3, 128, dst.rearrange("h p t -> p h t"))
                                elif cname in ("mq0", "mq1"):
                                    sc = 192 ** -0.5
                                    p.op("act", lambda e, pt=pt, sc=sc: e.mul(rs[:, 0:384], pt[:, 0:384], sc), [pk], ["rs" + x])
                                    p.op("dve", lambda e: e.tensor_copy(rb[:, 0:384], rs[:, 0:384]), ["rs" + x], ["rb" + x])
                                    rope(rs, 384, 2, 64, 16, 192, 128, tab, tk, 384, 448, r1, r2, rb, x)
                                    h0 = 0 if cname == "mq0" else 2
                                    transp(2, 128, qnT[h0:h0 + 2, :, tok].rearrange("h p t -> p h t"), srcoff=lambda b: b * 192)
                                    transp(2, 64, qrT[h0:h0 + 2, :, tok].rearrange("h p t -> p h t"), srcoff=lambda b: b * 192 + 128)
                                else:
                                    p.op("act", lambda e, pt=pt: e.copy(rs[:, 0:320], pt[:, 0:320]), [pk], ["rs" + x])
                                    rope(rs, 64, 1, 64, 16, 64, 256, tab, tk, 384, 448, r1, r2, rb, x)
                                    transp(1, 64, krT[:, tok].rearrange("p (a t) -> p a t", a=1), srcoff=lambda b: 256)
                                    p.op("dve", lambda e: e.tensor_tensor(r1[:, 0:256], rs[:, 0:256], rs[:, 0:256], ALU.mult), ["rs" + x], ["r1" + x])
                                    p.op("dve", lambda e: e.reduce_sum(ssq[:], r1[:, 0:256], axis=AX.X), ["r1" + x], ["ssq" + x])
                                    p.op("dve", lambda e: e.tensor_scalar(ssq[:], ssq[:], 1.0 / 256, EPS, ALU.mult, ALU.add), ["ssq" + x], ["ssq" + x])
                                    p.op("act", lambda e: e.activation(ssq[:], ssq[:], AF.Sqrt), ["ssq" + x], ["ssq" + x])
                                    p.op("dve", lambda e: e.reciprocal(ssq[:], ssq[:]), ["ssq" + x], ["ssq" + x])
                                    p.op("dve", lambda e: e.scalar_tensor_tensor(rb[:, 0:256], rs[:, 0:256], ssq[:, 0:1], gkv[:],
                                                                                 ALU.mult, ALU.mult), ["rs" + x, "ssq" + x, "gkv"], ["rb" + x])
                                    for b in range(2):
                                        p.op("pe", lambda e, b=b: e.transpose(psb[7][:, b * 128:(b + 1) * 128], rb[:, b * 128:(b + 1) * 128], identb[:]),
                                             ["rb" + x, "identb"], [psk[7]])
                                    p.op("act", lambda e: e.copy(cnT[:].rearrange("p a b -> p (a b)"), psb[7][:, 0:256]), [psk[7]], ["cnT" + x])
                                    bk2 = 3
                                    for h in range(4):
                                        for rc in range(2):
                                            p.op("pe", lambda e, h=h, rc=rc, bk2=bk2: e.matmul(
                                                ps[bk2][:, h * 128:(h + 1) * 128], wuk[:, rc, h * 128:(h + 1) * 128], cnT[:, rc, :],
                                                start=(rc == 0), stop=(rc == 1)), ["wuk", "cnT" + x], [psk[bk2]])
                                    p.op("act", lambda e, bk2=bk2: e.copy(tT[:, 0:4, :].rearrange("p a b -> p (a b)"), ps[bk2][:, 0:512]), [psk[bk2]], ["tT" + x])
                                    p.dma("sp", knT[:, :, tok].rearrange("h p t -> p h t"), tT[:, 0:4, :], reads=["tT" + x], writes=[])
                                    bk3 = 6
                                    for rc in range(2):
                                        p.op("pe", lambda e, rc=rc, bk3=bk3: e.matmul(
                                            ps[bk3][:, 0:512], cnT[:, rc, :], wuv[:, rc, :], start=(rc == 0), stop=(rc == 1)),
                                            ["wuv", "cnT" + x], [psk[bk3]])
                                    p.op("act", lambda e, bk3=bk3: e.copy(rb[:, 0:512], ps[bk3][:, 0:512]), [psk[bk3]], ["rb" + x])
                                    p.dma("sp", vm[tok, :], rb[:, 0:512], reads=["rb" + x], writes=[])
                            pcnt[0] += 1
                            pendq.append(post)
                            if len(pendq) > 2:
                                pendq.pop(0)()
                    while pendq:
                        pendq.pop(0)()
                p.flush()
            if "P2" in dbg:
                break
            LN2 = math.log(2.0)
            import os
            SKIP = os.environ.get("KSKIP", "").split(",")
            with ExitStack() as ph:
              if "P3" not in SKIP:
                  kT = SB("kT_s", [128, T], BF16, ph)
                  vv = SB("v_s", [128, NT, 128], BF16, ph)
                  q3 = [SB("q3_%d" % i, [128, 3, 128], BF16, ph) for i in range(2)]
                  PT = [SB("PT%d" % i, [128, 384], BF16, ph) for i in range(2)]
                  sink6 = SB("sink6", [1, 6], F32, ph)
                  esrow = SB("esrow", [1, 768], BF16, ph)
                  rec = SB("rec", [128, 384], F32, ph)
                  oT = SB("oT", [128, 384], BF16, ph)
                  p.dma("sp", sink6[:], swa_sink[l:l + 1, :], writes=["sink6"])
                  p.op("act", lambda e: e.activation(sink6[:], sink6[:], AF.Exp), ["sink6"], ["sink6"])
                  for h in range(6):
                      p.op("dve", lambda e, h=h: e.tensor_scalar(esrow[0:1, h * 128:(h + 1) * 128], onesf[0:1, 0:128],
                                                                 sink6[0:1, h:h + 1], None, ALU.mult), ["sink6", "onesf"], ["esrow"])
                  qi = 0
                  pi = 0
                  for hk in range(2):
                      p.dma("sp", kT[:], kTs[hk], writes=["kT"])
                      p.dma("sp", vv[:], vs[:, hk * 128:(hk + 1) * 128].rearrange("(n p) d -> p n d", p=128), writes=["vv"])
                      for t in range(NT):
                          keys = [(0, None), (1, None)]
                          if t >= 2:
                              n = t - 2
                              if n > 0:
                                  keys.append((t - 1, mge))
                              keys.append((t, None))
                              if n < NLT - 1:
                                  keys.append((t + 1, mle))
                          q = q3[qi % 2]
                          qk = "q3_%d" % (qi % 2)
                          dn, nm = (2, 3) if qi % 2 == 0 else (4, 5)
                          qi += 1
                          tok = slice(t * 128, (t + 1) * 128)
                          p.dma("sp", q[:], qTs[hk * 3:(hk + 1) * 3, :, tok].rearrange("h p t -> p h t"), writes=[qk])
                          pend_ = []
                          for ki, (kt, mk) in enumerate(keys):
                              sb_ = pi % 2
                              P_ = PT[pi % 2]
                              Pk = "PT%d" % (pi % 2)
                              pi += 1
                              p.op("pe", lambda e, sb_=sb_, kt=kt, q=q: e.matmul(
                                  ps[sb_][:, 0:384], kT[:, kt * 128:(kt + 1) * 128], q[:].rearrange("p a b -> p (a b)"),
                                  start=True, stop=True), ["kT", qk], [psk[sb_]])
                              p.op("act", lambda e, sb_=sb_, P_=P_: e.activation(P_[:], ps[sb_][:, 0:384], AF.Exp), [psk[sb_]], [Pk])
                              if mk is not None:
                                  p.op("dve", lambda e, P_=P_, mk=mk: e.tensor_tensor(
                                      V(P_, 0, 128, [128, 3], [1, 128]), V(P_, 0, 128, [128, 3], [1, 128]),
                                      V(mk, 0, 128, [0, 3], [1, 128]), ALU.mult), [Pk, "mle", "mge"], [Pk])

                              def fin(P_=P_, Pk=Pk, ki=ki, kt=kt, dn=dn, nm=nm, last=(ki == len(keys) - 1)):
                                  p.op("pe", lambda e: e.matmul(ps[dn][:, 0:384], onesb[:], P_[:], start=(ki == 0), stop=False),
                                       [Pk, "onesb"], [psk[dn]])
                                  p.op("pe", lambda e: e.matmul(ps[nm][:, 0:384], vv[:, kt, :], P_[:], start=(ki == 0), stop=last),
                                       [Pk, "vv"], [psk[nm]])
                              for f_ in pend_:
                                  f_()
                              pend_ = [fin]
                          for f_ in pend_:
                              f_()
                          p.op("pe", lambda e, dn=dn, hk=hk: e.matmul(
                              ps[dn][:, 0:384], onesb[0:1, :], esrow[0:1, hk * 384:(hk + 1) * 384], start=False, stop=True),
                              ["esrow", "onesb"], [psk[dn]])
                          p.op("dve", lambda e, dn=dn: e.reciprocal(rec[:], ps[dn][:, 0:384]), [psk[dn]], ["rec"])
                          p.op("dve", lambda e, nm=nm: e.tensor_tensor(oT[:], ps[nm][:, 0:384], rec[:], ALU.mult), [psk[nm], "rec"], ["oT"])
                          p.dma("sp", catT[hk * 3:(hk + 1) * 3, :, tok].rearrange("h p t -> p h t"),
                                oT[:].rearrange("p (a b) -> p a b", b=128), reads=["oT"])
                  p.flush()

            with ExitStack() as ph:
              if "P5" not in SKIP:
                  knS = SB("knS", [128, T], BF16, ph)
                  krS = SB("krS", [64, T], BF16, ph)
                  vS = SB("vS", [128, NT, 128], BF16, ph)
                  qn = [SB("qn%d" % i, [128, 512], BF16, ph) for i in range(2)]
                  qr = [SB("qr%d" % i, [64, 512], BF16, ph) for i in range(2)]
                  PT = [SB("PTm%d" % i, [128, 512], BF16, ph) for i in range(2)]
                  rec = SB("recm", [128, 512], F32, ph)
                  oT = SB("oTm", [128, 512], BF16, ph)
                  p.dma("sp", krS[:], krT, writes=["krS"])
                  groups = [(0, 2, [0, 1])] + [(g0, min(4, NT - g0), list(range(NT))) for g0 in range(2, NT, 4)]
                  qi = 0
                  pi = 0
                  for h in range(4):
                      p.dma("sp", knS[:], knT[h], writes=["knS"])
                      p.dma("sp", vS[:], vm[:, h * 128:(h + 1) * 128].rearrange("(n p) d -> p n d", p=128), writes=["vS"])
                      for (g0, ng, kblks) in groups:
                          N = ng * 128
                          cols = slice(g0 * 128, g0 * 128 + N)
                          qn_, qr_ = qn[qi % 2], qr[qi % 2]
                          qk = "qm%d" % (qi % 2)
                          dn, nm = (2, 3) if qi % 2 == 0 else (4, 5)
                          qi += 1
                          p.dma("sp", qn_[:, 0:N], qnT[h][:, cols], writes=[qk])
                          p.dma("sp", qr_[:, 0:N], qrT[h][:, cols], writes=[qk])
                          pend_ = []
                          for ki, kt in enumerate(kblks):
                              sb_ = pi % 2
                              P_ = PT[pi % 2]
                              Pk = "PTm%d" % (pi % 2)
                              pi += 1
                              p.op("pe", lambda e, sb_=sb_, kt=kt, qn_=qn_, N=N: e.matmul(
                                  ps[sb_][:, 0:N], knS[:, kt * 128:(kt + 1) * 128], qn_[:, 0:N], start=True, stop=False),
                                  ["knS", qk], [psk[sb_]])
                              p.op("pe", lambda e, sb_=sb_, kt=kt, qr_=qr_, N=N: e.matmul(
                                  ps[sb_][:, 0:N], krS[:, kt * 128:(kt + 1) * 128], qr_[:, 0:N], start=False, stop=True),
                                  ["krS", qk], [psk[sb_]])
                              p.op("act", lambda e, sb_=sb_, P_=P_, N=N: e.activation(P_[:, 0:N], ps[sb_][:, 0:N], AF.Exp), [psk[sb_]], [Pk])

                              def fin(P_=P_, Pk=Pk, ki=ki, kt=kt, dn=dn, nm=nm, N=N, last=(ki == len(kblks) - 1)):
                                  p.op("pe", lambda e: e.matmul(ps[dn][:, 0:N], onesb[:], P_[:, 0:N], start=(ki == 0), stop=last),
                                       [Pk, "onesb"], [psk[dn]])
                                  p.op("pe", lambda e: e.matmul(ps[nm][:, 0:N], vS[:, kt, :], P_[:, 0:N], start=(ki == 0), stop=last),
                                       [Pk, "vS"], [psk[nm]])
                              for f_ in pend_:
                                  f_()
                              pend_ = [fin]
                          for f_ in pend_:
                              f_()
                          p.op("dve", lambda e, dn=dn, N=N: e.reciprocal(rec[:, 0:N], ps[dn][:, 0:N]), [psk[dn]], ["recm"])
                          p.op("dve", lambda e, nm=nm, N=N: e.tensor_tensor(oT[:, 0:N], ps[nm][:, 0:N], rec[:, 0:N], ALU.mult),
                               [psk[nm], "recm"], ["oTm"])
                          p.dma("sp", catT[12 + h][:, cols], oT[:, 0:N], reads=["oTm"])
                  p.flush()

            with ExitStack() as ph:
              if "P4" not in SKIP:
                  lgb = SB("lgb", [128, 2, 6], F32, ph)
                  nlgb = SB("nlgb", [128, 2, 6], F32, ph)
                  lgcol = SB("lgcol", [128, 2, 3], F32, ph)
                  maskT = SB("maskT", [128, 2, 6, 128], F32, ph)
                  mtmp = SB("mtmp", [128, 128], F32, ph)
                  qdec = SB("qdec", [64, 2, 6, 128], F32, ph)
                  kdf = SB("kdf", [128, 2, 6], F32, ph)
                  gc = SB("gc", [64, 2, 6], F32, ph)
                  gCt = SB("gCt", [64, 2, 6, 128], F32, ph)
                  S = SB("S", [64, 6, 128], F32, ph)
                  Stmp = SB("Stmp", [64, 6, 128], F32, ph)
                  Sb = SB("Sb", [64, 6, 128], BF16, ph)
                  qt = [SB("qt%d" % i, [64, 6, 128], BF16, ph) for i in range(2)]
                  ktT = [SB("ktT%d" % i, [64, 6, 128], BF16, ph) for i in range(2)]
                  ktm = [SB("ktm%d" % i, [128, 384], BF16, ph) for i in range(2)]
                  vt = [SB("vt%d" % i, [128, 768], BF16, ph) for i in range(2)]
                  gt = [SB("gt%d" % i, [128, 768], F32, ph) for i in range(2)]
                  ra = [SB("ra%d" % i, [128, 768], F32, ph) for i in range(2)]
                  PTr = SB("PTr", [128, 6, 128], BF16, ph)
                  qd = SB("qd", [64, 6, 128], BF16, ph)
                  kd = SB("kd", [128, 6, 64], BF16, ph)
                  st6 = SB("st6", [128, 6, 6], F32, ph)
                  mv6 = SB("mv6", [128, 6, 2], F32, ph)
                  rs6 = SB("rs6", [128, 6], F32, ph)
                  y1 = SB("y1", [128, 6, 128], F32, ph)
                  y2 = SB("y2", [128, 6, 128], F32, ph)
                  yb = SB("yb", [128, 768], BF16, ph)
                  tT2 = SB("tT2", [128, 6, 128], BF16, ph)
                  racc = dscr("racc%d" % l, [T, 768])
                  p.dma("sp", lgb[:].rearrange("p a b -> p (a b)"),
                        ret_decay[l:l + 1].rearrange("o a b -> o (a b)").to_broadcast([128, 12]), writes=["lgb"])
                  p.op("act", lambda e: e.activation(lgb[:], lgb[:], AF.Exp, scale=-LN2), ["lgb"], ["lgb"])
                  p.op("act", lambda e: e.activation(lgb[:], lgb[:], AF.Ln, scale=-1.0, bias=1.0), ["lgb"], ["lgb"])
                  p.op("dve", lambda e: e.tensor_scalar(nlgb[:], lgb[:], -1.0, None, ALU.mult), ["lgb"], ["nlgb"])
                  for dr in range(2):
                      for j in range(3):
                          for hh in range(2):
                              p.op("dve", lambda e, dr=dr, j=j, hh=hh: e.tensor_copy(
                                  lgcol[hh * 64:(hh + 1) * 64, dr, j:j + 1], lgb[hh * 64:(hh + 1) * 64, dr, 2 * j + hh:2 * j + hh + 1]),
                                  ["lgb"], ["lgcol"])
                  for dr in range(2):
                      for h in range(6):
                          src = lgb if dr == 0 else nlgb
                          p.op("act", lambda e, dr=dr, h=h, src=src: e.activation(mtmp[:], cst[:, 2, :], AF.Exp, scale=src[:, dr, h:h + 1]),
                               ["cst", "lgb", "nlgb"], ["mtmp"])
                          p.op("dve", lambda e, dr=dr, h=h: e.tensor_tensor(maskT[:, dr, h, :], mtmp[:], cst[:, dr, :], ALU.mult),
                               ["mtmp", "cst"], ["maskT"])
                      for h in range(6):
                          p.op("act", lambda e, dr=dr, h=h: e.activation(qdec[:, dr, h, :], cst[0:64, 3 + dr, :], AF.Exp,
                                                                         scale=lgb[0:64, dr, h:h + 1]), ["cst", "lgb"], ["qdec"])
                      p.op("dve", lambda e, dr=dr: e.tensor_scalar(kdf[:, dr, :], lgb[:, dr, :], scol[:, dr:dr + 1], None, ALU.mult),
                           ["lgb", "scol"], ["kdf"])
                      p.op("act", lambda e, dr=dr: e.activation(kdf[:, dr, :], kdf[:, dr, :], AF.Exp), ["kdf"], ["kdf"])
                      p.op("act", lambda e, dr=dr: e.activation(gc[:, dr, :], lgb[0:64, dr, :], AF.Exp, scale=128.0), ["lgb"], ["gc"])
                      p.op("dve", lambda e, dr=dr: e.tensor_copy(gCt[:, dr], V(gc, dr * 6, 64, [1, 6], [0, 128])), ["gc"], ["gCt"])
                  bi = 0
                  RS = int(os.environ.get("RS", "9"))
                  for dr in range(2 if RS > 0 else 0):
                      order = list(range(NT)) if dr == 0 else [1, 0] + list(range(NT - 1, 1, -1))
                      p.op("dve", lambda e: e.memset(S[:], 0.0), [], ["S"])
                      p.op("dve", lambda e: e.memset(Sb[:], 0.0), [], ["Sb"])
                      for t in order:
                          tok = slice(t * 128, (t + 1) * 128)
                          b_ = bi % 2
                          bi += 1
                          qt_, kt_, km_, vt_, gt_, ra_ = qt[b_], ktT[b_], ktm[b_], vt[b_], gt[b_], ra[b_]
                          bk = "rin%d" % b_
                          p.dma("sp", qt_[:], qTr.rearrange("j (hh d) t -> d (j hh) t", d=64)[:, :, tok], writes=[bk + "q"])
                          p.dma("sp", kt_[:], kTr.rearrange("j (hh d) t -> d (j hh) t", d=64)[:, :, tok], writes=[bk + "k"])
                          p.dma("sp", km_[:], kr[tok, :], writes=[bk + "km"])
                          p.dma("sp", vt_[:], vr[tok, :], writes=[bk + "v"])
                          p.dma("sp", gt_[:], gfb[dr, tok, :], writes=[bk + "g"])
                          if dr == 1:
                              p.dma("sp", ra_[:], racc[tok, :], reads=["racc%d" % t], writes=[bk + "ra"])
                          psS = lambda h: ps[0][:, h * 128:(h + 1) * 128] if h < 4 else ps[1][:, (h - 4) * 128:(h - 3) * 128]
                          psY = lambda h: ps[2][:, h * 128:(h + 1) * 128] if h < 4 else ps[3][:, (h - 4) * 128:(h - 3) * 128]
                          kS = lambda h: psk[0] if h < 4 else psk[1]
                          kY = lambda h: psk[2] if h < 4 else psk[3]
                          for h in range(6):
                              j, hh = h // 2, h % 2
                              p.op("pe", lambda e, h=h, j=j, hh=hh, kt_=kt_, qt_=qt_, psS=psS: e.matmul(
                                  psS(h), kt_[:, h, :], qt_[:, h, :], start=True, stop=True),
                                  [bk + "q", bk + "k"], [kS(h)])
                          p.op("dve", lambda e, dr=dr: e.tensor_tensor(
                              PTr[:, 0:4, :], ps[0][:, 0:512].rearrange("p (a b) -> p a b", b=128), maskT[:, dr, 0:4, :], ALU.mult),
                              [psk[0], "maskT"], ["PTr"])
                          p.op("dve", lambda e, dr=dr: e.tensor_tensor(
                              PTr[:, 4:6, :], ps[1][:, 0:256].rearrange("p (a b) -> p a b", b=128), maskT[:, dr, 4:6, :], ALU.mult),
                              [psk[1], "maskT"], ["PTr"])
                          if RS < 2:
                              continue
                          p.op("pool", lambda e, dr=dr, qt_=qt_: e.tensor_tensor(qd[:], qt_[:], qdec[:, dr], ALU.mult), [bk + "q", "qdec"], ["qd"])
                          p.op("pool", lambda e, dr=dr, km_=km_: e.tensor_tensor(
                              kd[:], km_[:].rearrange("p (a b) -> p a b", b=64), V(kdf, dr * 6, 128, [1, 6], [0, 64]), ALU.mult),
                              [bk + "km", "kdf"], ["kd"])
                          for h in range(6):
                              j, hh = h // 2, h % 2
                              p.op("pe", lambda e, h=h, vt_=vt_, psY=psY: e.matmul(
                                  psY(h), PTr[:, h, :], vt_[:, h * 128:(h + 1) * 128], start=True, stop=False), ["PTr", bk + "v"], [kY(h)])
                              p.op("pe", lambda e, h=h, j=j, hh=hh, psY=psY: e.matmul(
                                  psY(h), qd[:, h, :], Sb[:, h, :],
                                  start=False, stop=True), ["qd", "Sb"], [kY(h)])
                          if RS < 3:
                              continue
                          for h in range(6):
                              ub, uo = (4, h * 128) if h < 4 else (5, (h - 4) * 128)
                              p.op("pe", lambda e, h=h, ub=ub, uo=uo, vt_=vt_: e.matmul(
                                  ps[ub][0:64, uo:uo + 128], kd[:, h, :], vt_[:, h * 128:(h + 1) * 128], start=True, stop=True), ["kd", bk + "v"], [psk[ub]])
                          p.op("dve", lambda e, dr=dr: e.tensor_tensor(Stmp[:], S[:], gCt[:, dr], ALU.mult), ["S", "gCt"], ["Stmp"])
                          p.op("dve", lambda e: e.tensor_tensor(S[:, 0:4, :], Stmp[:, 0:4, :],
                                                                ps[4][0:64, 0:512].rearrange("p (a b) -> p a b", b=128), ALU.add), ["Stmp", psk[4]], ["S"])
                          p.op("dve", lambda e: e.tensor_tensor(S[:, 4:6, :], Stmp[:, 4:6, :],
                                                                ps[5][0:64, 0:256].rearrange("p (a b) -> p a b", b=128), ALU.add), ["Stmp", psk[5]], ["S"])
                          p.op("act", lambda e: e.copy(Sb[:], S[:]), ["S"], ["Sb"])
                          if RS < 4:
                              continue
                          for h in range(6):
                              p.op("dve", lambda e, h=h, psY=psY: e.bn_stats(st6[:, h, :], psY(h)), [kY(h)], ["st6"])
                          for h in range(6):
                              p.op("dve", lambda e, h=h: e.bn_aggr(mv6[:, h, :], st6[:, h, :]), ["st6"], ["mv6"])
                          p.op("dve", lambda e: e.tensor_scalar(rs6[:], V(mv6, 1, 128, [2, 6]), EPS, None, ALU.add), ["mv6"], ["rs6"])
                          p.op("act", lambda e: e.activation(rs6[:], rs6[:], AF.Sqrt), ["rs6"], ["rs6"])
                          p.op("dve", lambda e: e.reciprocal(rs6[:], rs6[:]), ["rs6"], ["rs6"])
                          p.op("dve", lambda e: e.tensor_tensor(y1[:, 0:4, :], ps[2][:, 0:512].rearrange("p (a b) -> p a b", b=128),
                                                                V(mv6, 0, 128, [2, 4], [0, 128]), ALU.subtract), [psk[2], "mv6"], ["y1"])
                          p.op("dve", lambda e: e.tensor_tensor(y1[:, 4:6, :], ps[3][:, 0:256].rearrange("p (a b) -> p a b", b=128),
                                                                V(mv6, 8, 128, [2, 2], [0, 128]), ALU.subtract), [psk[3], "mv6"], ["y1"])
                          p.op("pool", lambda e: e.tensor_tensor(y2[:], y1[:], V(rs6, 0, 128, [1, 6], [0, 128]), ALU.mult), ["y1", "rs6"], ["y2"])
                          p.op("pool", lambda e, gt_=gt_: e.tensor_tensor(y1[:].rearrange("p a b -> p (a b)"),
                                                                          y2[:].rearrange("p a b -> p (a b)"), gt_[:], ALU.mult),
                               ["y2", bk + "g"], ["y1"])
                          if RS < 5:
                              continue
                          if dr == 0:
                              p.dma("sp", racc[tok, :], y1[:].rearrange("p a b -> p (a b)"), reads=["y1"], writes=["racc%d" % t])
                          else:
                              p.op("dve", lambda e, ra_=ra_: e.tensor_tensor(yb[:], y1[:].rearrange("p a b -> p (a b)"), ra_[:], ALU.add),
                                   ["y1", bk + "ra"], ["yb"])
                              for h in range(6):
                                  p.op("pe", lambda e, h=h: e.transpose(psb[6][:, h * 128:(h + 1) * 128], yb[:, h * 128:(h + 1) * 128], identb[:]),
                                       ["yb", "identb"], [psk[6]])
                              p.op("act", lambda e: e.copy(tT2[:].rearrange("p a b -> p (a b)"), psb[6][:, 0:768]), [psk[6]], ["tT2"])
                              p.dma("sp", catT[6:12, :, tok].rearrange("h p t -> p h t"), tT2[:], reads=["tT2"])
                  p.flush()
            if "P5" in dbg:
                break

            with ExitStack() as ph:
                wo = SB("wo", [128, 16, D], BF16, ph)
                g1 = [SB("g1_%d" % m, [128, D], F32, ph) for m in range(2)]
                lnG = SB("lnG", [128, D], F32, ph)
                lnB = SB("lnB", [128, D], F32, ph)
                cT = [SB("cT%d" % i, [128, 16, 128], BF16, ph) for i in range(2)]
                xt2 = [SB("xo%d" % i, [128, D], F32, ph) for i in range(2)]
                ygL = [SB("yg%d" % i, [128, D], F32, ph) for i in range(2)]
                B6 = dict(stt=SB("stt6", [128, 4, 6], F32, ph), mv=SB("mvo", [128, 2], F32, ph),
                          rstd=SB("rstdo", [128, 1], F32, ph), nmr=SB("nmro", [128, 1], F32, ph))
                wov = w_out[l].rearrange("(kc p) n -> p kc n", p=128)
                for q in range(4):
                    p.dma("pool", wo[:, q * 4:(q + 1) * 4, :], wov[:, q * 4:(q + 1) * 4, :], writes=["wo"])
                for m in range(2):
                    p.dma("sp", g1[m][:], gsc[l, 0, m:m + 1, :].to_broadcast([128, D]), writes=["g1"])
                p.dma("sp", lnG[:], ln_g[0][l:l + 1, :].to_broadcast([128, D]), writes=["lnG"])
                p.dma("sp", lnB[:], ln_b[0][l:l + 1, :].to_broadcast([128, D]), writes=["lnB"])
                for t in range(NT):
                    tok = slice(t * 128, (t + 1) * 128)
                    m = 1 if t < 2 else 0
                    c_, ck = cT[t % 2], "cT%d" % (t % 2)
                    x_, xk = xt2[t % 2], "xo%d" % (t % 2)
                    yg, ygk = ygL[t % 2], "yg%d" % (t % 2)
                    p.dma("sp", c_[:], catT[:, :, tok].rearrange("k p t -> p k t"), writes=[ck])
                    p.dma("sp", x_[:], blk_src(l, t), reads=["xres%d" % t], writes=[xk])
                    for oc in range(4):
                        for kc in range(16):
                            p.op("pe", lambda e, oc=oc, kc=kc, c_=c_: e.matmul(
                                ps[oc][:, :], c_[:, kc, :], wo[:, kc, oc * 512:(oc + 1) * 512], start=(kc == 0), stop=(kc == 15)),
                                [ck, "wo"], [psk[oc]])
                    for oc in range(4):
                        p.op("dve", lambda e, oc=oc, m=m, yg=yg: e.tensor_tensor(yg[:, oc * 512:(oc + 1) * 512], ps[oc][:, :],
                                                                          g1[m][:, oc * 512:(oc + 1) * 512], ALU.mult),
                             [psk[oc], "g1"], [ygk])
                    p.op("dve", lambda e, x_=x_, yg=yg: e.scalar_tensor_tensor(yg[:], x_[:], ALPHA, yg[:], ALU.mult, ALU.add), [xk, ygk], [ygk])
                    ln_stats(yg, ygk, B6["stt"], B6["mv"], B6["rstd"], B6["nmr"], "o")
                    p.op("act", lambda e, x_=x_, yg=yg: e.activation(x_[:], yg[:], AF.Identity, bias=B6["nmr"][:], scale=B6["rstd"][:]),
                         [ygk, "onmr", "orstd"], [xk])
                    p.op("dve", lambda e, x_=x_: e.tensor_tensor(x_[:], x_[:], lnG[:], ALU.mult), [xk, "lnG"], [xk])
                    p.op("pool", lambda e, x_=x_: e.tensor_tensor(x_[:], x_[:], lnB[:], ALU.add), [xk, "lnB"], [xk])
                    p.dma("sp", xres[tok, :], x_[:], reads=[xk], writes=["xres%d" % t])
                p.flush()
            if "P6" in dbg:
                break
            NB = 2 * NT + NE
            if l == 0:
                jv_in = din("jv", [128, NB])
                pidx_in = din("pidx", [128, 1])
                h2d = dscr("h2d", [T, D], BF16)
                xslots = dscr("xslots", [NB * 128, D], BF16)
                yslots = dscr("yslots", [NB * 128, D])
                dest_i = SB("dest_i", [128, NT, 2], I32)
                gatew = SB("gatew", [128, NT, 2], F32)
                offs_i = SB("offs_i", [128, NB], I32)
                breg = st.enter_context(nc.gpsimd.register("bndreg"))
                nc.gpsimd.reg_mov(breg, DEPTH * NE * 128 - 1)
                bnd_val = [nc.gpsimd.snap(breg)]
            with ExitStack() as ph:
                B = dict(xt=[SB("xt%d" % i, [128, D], F32, ph) for i in range(2)], xn=SB("xn", [128, D], F32, ph),
                         stt=SB("stt", [128, 4, 6], F32, ph), mv=SB("mv", [128, 2], F32, ph),
                         rstd=SB("rstd", [128, 1], F32, ph), nmr=SB("nmr", [128, 1], F32, ph))
                scb = [SB("scb%d" % m, [128, D], F32, ph) for m in range(2)]
                shb = [SB("shb%d" % m, [128, D], F32, ph) for m in range(2)]
                h2 = SB("h2", [128, D], F32, ph)
                h2b = [SB("h2b%d" % i, [128, D], BF16, ph) for i in range(2)]
                h2T = SB("h2T", [128, 16, 128], F32, ph)
                wr = SB("wr", [128, 16, 36], F32, ph)
                brt = SB("brt", [128, 36], F32, ph)
                lg = SB("lg", [128, 36], F32, ph)
                sm = SB("sm", [128, 16], F32, ph)
                ohg = SB("ohg", [128, 4], F32, ph)
                ge = SB("ge", [128, 4], F32, ph)
                ein = SB("ein", [128, 8], F32, ph)
                ein2 = SB("ein2", [128, 8], F32, ph)
                oh8 = SB("oh8", [128, 2, 8], F32, ph)
                ohall = SB("ohall", [128, NT, 2, 32], F32, ph)
                At = SB("At", [128, 32], BF16, ph)
                Us = SB("Us", [128, 128], BF16, ph)
                base = SB("base", [128, 32], F32, ph)
                rank = SB("rank", [128, NT, 32], F32, ph)
                tmpr = SB("tmpr", [128, NT, 32], F32, ph)
                cs = [SB("cs%d" % i, [128, 32], F32, ph) for i in range(2)]
                padd = SB("padd", [128, 32], F32, ph)
                pst = SB("pst", [128, 32], F32, ph)
                dest_f = SB("dest_f", [128, NT, 2], F32, ph)
                jv = SB("jv", [128, NB], F32, ph)
                pidx = SB("pidx", [128, 1], F32, ph)
                cmp_ = SB("cmp", [128, NB, 32], F32, ph)
                be = SB("be", [128, NB], F32, ph)
                same = SB("same", [128, NB], F32, ph)
                zt = SB("zt", [128, D], BF16, ph)
                p.dma("sp", jv[:], jv_in, writes=["jv"])
                p.dma("sp", pidx[:], pidx_in, writes=["pidx"])
                p.op("dve", lambda e: e.tensor_copy(Us[:], cst[:, 5, :]), ["cst"], ["Us"])
                p.op("dve", lambda e: e.memset(base[:], 0.0), [], ["base"])
                p.op("pool", lambda e: e.memset(zt[:], 0.0), [], ["zt"])
                for j in range(NB):
                    p.dma("sp", xslots[j * 128:(j + 1) * 128, :], zt[:], reads=["zt"], writes=["xslots"])
                for m in range(2):
                    p.dma("sp", shb[m][:], gsc[l, 2, m:m + 1, :].to_broadcast([128, D]), writes=["shb"])
                    p.dma("sp", scb[m][:], gsc[l, 3, m:m + 1, :].to_broadcast([128, D]), writes=["scb"])
                p.dma("sp", wr[:, :, 0:4], w_grp[l].rearrange("(kc p) n -> p kc n", p=128), writes=["wr"])
                p.dma("sp", wr[:, :, 4:36], w_exp[l].rearrange("(kc p) n -> p kc n", p=128), writes=["wr"])
                p.dma("sp", brt[:, 0:4], b_grp[l:l + 1, :].to_broadcast([128, 4]), writes=["brt"])
                p.dma("sp", brt[:, 4:36], b_exp[l:l + 1, :].to_broadcast([128, 32]), writes=["brt"])
                for t in range(NT):
                    tok = slice(t * 128, (t + 1) * 128)
                    m = 1 if t < 2 else 0
                    xt, xk = B["xt"][t % 2], "xt%d" % (t % 2)
                    hb, hbk = h2b[t % 2], "h2b%d" % (t % 2)
                    p.dma("sp", xt[:], xres[tok, :], writes=[xk])
                    ln_stats(xt, xk, B["stt"], B["mv"], B["rstd"], B["nmr"], "l")
                    xn = B["xn"]
                    p.op("act", lambda e, xt=xt: e.activation(xn[:], xt[:], AF.Identity, bias=B["nmr"][:], scale=B["rstd"][:]),
                         [xk, "lnmr", "lrstd"], ["xn"])
                    p.op("dve", lambda e, m=m: e.tensor_tensor(h2[:], xn[:], scb[m][:], ALU.mult), ["xn", "scb"], ["h2"])
                    p.op("pool", lambda e, m=m: e.tensor_tensor(h2[:], h2[:], shb[m][:], ALU.add), ["h2", "shb"], ["h2"])
                    p.op("act", lambda e, hb=hb: e.copy(hb[:], h2[:]), ["h2"], [hbk])
                    p.dma("sp", h2d[tok, :], hb[:], reads=[hbk], writes=["h2d%d" % t])
                    for q in range(4):
                        bk = 4 + q
                        for k4 in range(4):
                            kc = q * 4 + k4
                            p.op("pe", lambda e, kc=kc, k4=k4, bk=bk: e.transpose(
                                ps[bk][:, k4 * 128:(k4 + 1) * 128], h2[:, kc * 128:(kc + 1) * 128], ident[:]), ["h2", "ident"], [psk[bk]])
                        p.op("act" if q % 2 else "dve", lambda e, q=q, bk=bk: e.tensor_copy(
                            h2T[:, q * 4:(q + 1) * 4, :].rearrange("p a b -> p (a b)"), ps[bk][:, :]) if q % 2 == 0 else e.copy(
                            h2T[:, q * 4:(q + 1) * 4, :].rearrange("p a b -> p (a b)"), ps[bk][:, :]), [psk[bk]], ["h2T"])
                    for kc in range(16):
                        p.op("pe", lambda e, kc=kc: e.matmul(ps[0][:, 0:36], h2T[:, kc, :], wr[:, kc, :], start=(kc == 0), stop=(kc == 15)),
                             ["h2T", "wr"], [psk[0]])
                    p.op("dve", lambda e: e.tensor_tensor(lg[:], ps[0][:, 0:36], brt[:], ALU.add), [psk[0], "brt"], ["lg"])
                    D_ = lambda fn, r, w: p.op("dve", fn, r, w)
                    D_(lambda e: e.reduce_max(sm[:, 0:1], lg[:, 0:4], axis=AX.X), ["lg"], ["sm"])
                    D_(lambda e: e.tensor_scalar(ohg[:], lg[:, 0:4], sm[:, 0:1], None, ALU.is_equal), ["lg", "sm"], ["ohg"])
                    D_(lambda e: e.tensor_scalar(sm[:, 1:2], sm[:, 0:1], -1.0, None, ALU.mult), ["sm"], ["sm"])
                    p.op("act", lambda e: e.activation(ge[:], lg[:, 0:4], AF.Exp, bias=sm[:, 1:2], scale=1.0), ["lg", "sm"], ["ge"])
                    D_(lambda e: e.reduce_sum(sm[:, 2:3], ge[:], axis=AX.X), ["ge"], ["sm"])
                    D_(lambda e: e.reciprocal(sm[:, 3:4], sm[:, 2:3]), ["sm"], ["sm"])
                    D_(lambda e: e.tensor_scalar(ein[:], lg[:, 4:12], ohg[:, 0:1], None, ALU.mult), ["lg", "ohg"], ["ein"])
                    for g in range(1, 4):
                        D_(lambda e, g=g: e.scalar_tensor_tensor(ein[:], lg[:, 4 + g * 8:12 + g * 8], ohg[:, g:g + 1], ein[:], ALU.mult, ALU.add),
                           ["lg", "ohg", "ein"], ["ein"])
                    D_(lambda e: e.reduce_max(sm[:, 4:5], ein[:], axis=AX.X), ["ein"], ["sm"])
                    D_(lambda e: e.tensor_scalar(oh8[:, 0, :], ein[:], sm[:, 4:5], None, ALU.is_equal), ["ein", "sm"], ["oh8"])
                    D_(lambda e: e.scalar_tensor_tensor(ein2[:], oh8[:, 0, :], -1e30, ein[:], ALU.mult, ALU.add), ["oh8", "ein"], ["ein2"])
                    D_(lambda e: e.reduce_max(sm[:, 5:6], ein2[:], axis=AX.X), ["ein2"], ["sm"])
                    D_(lambda e: e.tensor_scalar(oh8[:, 1, :], ein2[:], sm[:, 5:6], None, ALU.is_equal), ["ein2", "sm"], ["oh8"])
                    D_(lambda e: e.tensor_tensor(sm[:, 6:7], sm[:, 5:6], sm[:, 4:5], ALU.subtract), ["sm"], ["sm"])
                    p.op("act", lambda e: e.activation(sm[:, 7:8], sm[:, 6:7], AF.Exp), ["sm"], ["sm"])
                    D_(lambda e: e.tensor_scalar(sm[:, 8:9], sm[:, 7:8], 1.0, None, ALU.add), ["sm"], ["sm"])
                    D_(lambda e: e.reciprocal(sm[:, 9:10], sm[:, 8:9]), ["sm"], ["sm"])
                    D_(lambda e: e.tensor_tensor(sm[:, 10:11], sm[:, 7:8], sm[:, 9:10], ALU.mult), ["sm"], ["sm"])
                    D_(lambda e, t=t: e.tensor_tensor(gatew[:, t, 0:1], sm[:, 9:10], sm[:, 3:4], ALU.mult), ["sm"], ["gatew"])
                    D_(lambda e, t=t: e.tensor_tensor(gatew[:, t, 1:2], sm[:, 10:11], sm[:, 3:4], ALU.mult), ["sm"], ["gatew"])
                    for k in range(2):
                        for g in range(4):
                            D_(lambda e, t=t, k=k, g=g: e.tensor_scalar(ohall[:, t, k, g * 8:(g + 1) * 8], oh8[:, k, :], ohg[:, g:g + 1], None, ALU.mult),
                               ["oh8", "ohg"], ["ohall"])
                    D_(lambda e, t=t: e.tensor_tensor(At[:], ohall[:, t, 0, :], ohall[:, t, 1, :], ALU.add), ["ohall"], ["At"])
                    p.op("pe", lambda e: e.matmul(ps[1][:, 0:32], Us[:], At[:], start=True, stop=True), ["Us", "At"], [psk[1]])
                    p.op("pe", lambda e: e.matmul(ps[2][:, 0:32], onesb[:], At[:], start=True, stop=True), ["onesb", "At"], [psk[2]])
                    D_(lambda e, t=t: e.tensor_tensor(rank[:, t, :], ps[1][:, 0:32], base[:], ALU.add), [psk[1], "base"], ["rank"])
                    D_(lambda e: e.tensor_tensor(base[:], ps[2][:, 0:32], base[:], ALU.add), [psk[2], "base"], ["base"])
                D_(lambda e: e.tensor_tensor(V(cmp_, 0, 128, [NB, 32], [1, NB]), V(base, 0, 128, [1, 32], [0, NB]),
                                             V(jv, 0, 128, [0, 32], [1, NB]), ALU.is_gt), ["base", "jv"], ["cmp"])
                D_(lambda e: e.reduce_sum(padd[:], V(cmp_, 0, 128, [NB, 32], [1, NB]), axis=AX.X), ["cmp"], ["padd"])
                D_(lambda e: e.tensor_scalar(padd[:], padd[:], 128.0, None, ALU.mult), ["padd"], ["padd"])
                D_(lambda e: e.tensor_copy(cs[0][:], padd[:]), ["padd"], ["cs0"])
                cur = 0
                for sh in (1, 2, 4, 8, 16):
                    a, b = cs[cur], cs[1 - cur]
                    ak, bkk = "cs%d" % cur, "cs%d" % (1 - cur)
                    D_(lambda e, a=a, b=b: e.tensor_copy(b[:], a[:]), [ak], [bkk])
                    D_(lambda e, a=a, b=b, sh=sh: e.tensor_tensor(b[:, sh:32], a[:, sh:32], a[:, 0:32 - sh], ALU.add), [ak], [bkk])
                    cur = 1 - cur
                ends, ek = cs[cur], "cs%d" % cur
                D_(lambda e: e.tensor_tensor(pst[:], ends[:], padd[:], ALU.subtract), [ek, "padd"], ["pst"])
                D_(lambda e: e.tensor_tensor(rank[:], rank[:], V(pst, 0, 128, [0, NT], [1, 32]), ALU.add), ["rank", "pst"], ["rank"])
                for k in range(2):
                    D_(lambda e, k=k: e.tensor_tensor(tmpr[:], ohall[:, :, k, :], rank[:], ALU.mult), ["ohall", "rank"], ["tmpr"])
                    D_(lambda e, k=k: e.reduce_sum(dest_f[:, :, k], tmpr[:], axis=AX.X), ["tmpr"], ["dest_f"])
                D_(lambda e: e.tensor_copy(dest_i[:], dest_f[:]), ["dest_f"], ["dest_i"])
                D_(lambda e: e.tensor_tensor(cmp_[:], V(ends, 0, 128, [0, NB], [1, 32]), V(jv, 0, 128, [1, NB], [0, 32]), ALU.is_le),
                   [ek, "jv"], ["cmp"])
                D_(lambda e: e.reduce_sum(be[:], cmp_[:], axis=AX.X), ["cmp"], ["be"])
                D_(lambda e: e.tensor_scalar(be[:], be[:], 31.0, None, ALU.min), ["be"], ["be"])
                D_(lambda e: e.memset(same[:], 0.0), [], ["same"])
                D_(lambda e: e.tensor_tensor(same[:, 1:NB], be[:, 1:NB], be[:, 0:NB - 1], ALU.is_equal), ["be"], ["same"])
                D_(lambda e: e.tensor_scalar(be[:], be[:], 128.0, None, ALU.mult), ["be"], ["be"])
                D_(lambda e: e.scalar_tensor_tensor(be[:], same[:], 1.0e6, be[:], ALU.mult, ALU.add), ["be", "same"], ["be"])
                D_(lambda e: e.tensor_scalar(be[:], be[:], pidx[:, 0:1], float(l * NE * 128), ALU.add, ALU.add), ["be", "pidx"], ["be"])
                D_(lambda e: e.tensor_copy(offs_i[:], be[:]), ["be"], ["offs_i"])
                p.flush()
                for t in range(NT):
                    tok = slice(t * 128, (t + 1) * 128)
                    hb, hbk = h2b[t % 2], "h2b%d" % (t % 2)
                    p.dma("sp", hb[:], h2d[tok, :], reads=["h2d%d" % t], writes=[hbk])
                    for k in range(2):
                        p.op("pool", lambda e, t=t, k=k, hb=hb: e.indirect_dma_start(
                            out=xslots, out_offset=bass.IndirectOffsetOnAxis(ap=dest_i[:, t, k:k + 1], axis=0),
                            in_=hb[:], in_offset=None), [hbk, "dest_i", "xslots"], [], dma=True)
                p.flush()

            with ExitStack() as ph:
                Wg = SB("Wg", [128, 16, EH], BF16, ph)
                Wu = SB("Wu", [128, 16, EH], BF16, ph)
                Wd = SB("Wd", [128, 8, D], BF16, ph)
                xb = [SB("xb%d" % i, [128, D], BF16, ph) for i in range(2)]
                xT = SB("xT", [128, 16, 128], BF16, ph)
                sg = SB("sg", [128, EH], F32, ph)
                act_ = SB("act", [128, EH], BF16, ph)
                actT = SB("actT", [128, 8, 128], BF16, ph)
                yb = [SB("yb%d" % i, [128, D], F32, ph) for i in range(2)]
                wg_tab = w_gate.rearrange("l e (p kc) n -> (l e p) (kc n)", kc=16)
                wu_tab = w_up.rearrange("l e (p kc) n -> (l e p) (kc n)", kc=16)
                wd_tab = w_down.rearrange("l e (p hc) n -> (l e p) (hc n)", hc=8)
                xTL = [xT, SB("xT1", [128, 16, 128], BF16, ph)]

                def w_loads(j):
                    for (W_, tab, wk) in ((Wg, wg_tab, "Wg"), (Wu, wu_tab, "Wu"), (Wd, wd_tab, "Wd")):
                        p.op("pool", lambda e, W_=W_, tab=tab, j=j: e.indirect_dma_start(
                            out=W_[:].rearrange("p a b -> p (a b)"), out_offset=None, in_=tab,
                            in_offset=bass.IndirectOffsetOnAxis(ap=offs_i[:, j:j + 1], axis=0),
                            bounds_check=bnd_val[0], oob_is_err=False), ["offs_i"], [wk], dma=True)

                def x_stage(j):
                    x_, xk = xb[j % 2], "xb%d" % (j % 2)
                    xT_, xTk = xTL[j % 2], "xT%d" % (j % 2)
                    p.dma("sp", x_[:], xslots[j * 128:(j + 1) * 128, :], writes=[xk])
                    for kc in range(16):
                        p.op("pe", lambda e, kc=kc, x_=x_: e.transpose(
                            psb[4 + kc // 8][:, (kc % 8) * 128:(kc % 8 + 1) * 128], V(x_, kc, 128, [16, 128]), identb[:]),
                            [xk, "identb"], [psk[4 + kc // 8]])
                    p.op("act", lambda e, xT_=xT_: e.copy(xT_[:, 0:8, :].rearrange("p a b -> p (a b)"), psb[4][:, :]), [psk[4]], [xTk])
                    p.op("dve", lambda e, xT_=xT_: e.tensor_copy(xT_[:, 8:16, :].rearrange("p a b -> p (a b)"), psb[5][:, :]), [psk[5]], [xTk])

                w_loads(0)
                x_stage(0)
                for j in range(NB):
                    y_, yk = yb[j % 2], "yb%d" % (j % 2)
                    xT_, xTk = xTL[j % 2], "xT%d" % (j % 2)
                    for wi_, (W_, wk) in enumerate(((Wg, "Wg"), (Wu, "Wu"))):
                        for nh in range(2):
                            bk = wi_ * 2 + nh
                            for kc in range(16):
                                p.op("pe", lambda e, W_=W_, nh=nh, kc=kc, bk=bk, xT_=xT_: e.matmul(
                                    ps[bk][:, :], xT_[:, kc, :], W_[:, kc, nh * 512:(nh + 1) * 512], start=(kc == 0), stop=(kc == 15)),
                                    [xTk, wk], [psk[bk]])
                    for nh in range(2):
                        p.op("act", lambda e, nh=nh: e.activation(sg[:, nh * 512:(nh + 1) * 512], ps[nh][:, :], AF.Silu), [psk[nh]], ["sg"])
                        p.op("dve", lambda e, nh=nh: e.tensor_tensor(act_[:, nh * 512:(nh + 1) * 512], sg[:, nh * 512:(nh + 1) * 512],
                                                                     ps[2 + nh][:, :], ALU.mult), ["sg", psk[2 + nh]], ["act"])
                    if j + 1 < NB:
                        x_stage(j + 1)
                    for hc in range(8):
                        p.op("pe", lambda e, hc=hc: e.transpose(psb[6][:, hc * 128:(hc + 1) * 128], V(act_, hc, 128, [8, 128]), identb[:]),
                             ["act", "identb"], [psk[6]])
                    p.op("act", lambda e: e.copy(actT[:].rearrange("p a b -> p (a b)"), psb[6][:, :]), [psk[6]], ["actT"])
                    for oc in range(4):
                        bk = oc
                        for hc in range(8):
                            p.op("pe", lambda e, oc=oc, hc=hc, bk=bk: e.matmul(
                                ps[bk][:, :], actT[:, hc, :], Wd[:, hc, oc * 512:(oc + 1) * 512], start=(hc == 0), stop=(hc == 7)),
                                ["actT", "Wd"], [psk[bk]])
                        p.op("act" if oc % 2 else "dve", (lambda e, oc=oc, bk=bk, y_=y_: e.copy(y_[:, oc * 512:(oc + 1) * 512], ps[bk][:, :])) if oc % 2
                             else (lambda e, oc=oc, bk=bk, y_=y_: e.tensor_copy(y_[:, oc * 512:(oc + 1) * 512], ps[bk][:, :])), [psk[bk]], [yk])
                    p.dma("sp", yslots[j * 128:(j + 1) * 128, :], y_[:], reads=[yk])
                    if j + 1 < NB:
                        w_loads(j + 1)
                p.flush()

            with ExitStack() as ph:
                g2 = [SB("g2_%d" % m, [128, D], F32, ph) for m in range(2)]
                lnG = SB("lnG2", [128, D], F32, ph)
                lnB = SB("lnB2", [128, D], F32, ph)
                ya = [SB("ya%d" % i, [128, D], F32, ph) for i in range(2)]
                yc = [SB("yc%d" % i, [128, D], F32, ph) for i in range(2)]
                xo = [SB("xq%d" % i, [128, D], F32, ph) for i in range(2)]
                B6 = dict(stt=SB("stt7", [128, 4, 6], F32, ph), mv=SB("mv7", [128, 2], F32, ph),
                          rstd=SB("rstd7", [128, 1], F32, ph), nmr=SB("nmr7", [128, 1], F32, ph))
                for m in range(2):
                    p.dma("sp", g2[m][:], gsc[l, 1, m:m + 1, :].to_broadcast([128, D]), writes=["g2"])
                p.dma("sp", lnG[:], ln_g[1][l:l + 1, :].to_broadcast([128, D]), writes=["lnG"])
                p.dma("sp", lnB[:], ln_b[1][l:l + 1, :].to_broadcast([128, D]), writes=["lnB"])
                for t in range(NT):
                    if l == DEPTH - 1 and t < 2:
                        continue
                    tok = slice(t * 128, (t + 1) * 128)
                    m = 1 if t < 2 else 0
                    i_ = t % 2
                    for k, (yy, ykk) in enumerate(((ya[i_], "ya%d" % i_), (yc[i_], "yc%d" % i_))):
                        p.op("pool", lambda e, t=t, k=k, yy=yy: e.indirect_dma_start(
                            out=yy[:], out_offset=None, in_=yslots,
                            in_offset=bass.IndirectOffsetOnAxis(ap=dest_i[:, t, k:k + 1], axis=0)), ["dest_i"], [ykk], dma=True)
                    x_, xk = xo[i_], "xq%d" % i_
                    p.dma("sp", x_[:], xres[tok, :], reads=["xres%d" % t], writes=[xk])
                    y1_, y2_ = ya[i_], yc[i_]
                    p.op("dve", lambda e, t=t, y1_=y1_: e.tensor_scalar(y1_[:], y1_[:], gatew[:, t, 0:1], None, ALU.mult), ["ya%d" % i_, "gatew"], ["ya%d" % i_])
                    p.op("dve", lambda e, t=t, y1_=y1_, y2_=y2_: e.scalar_tensor_tensor(y1_[:], y2_[:], gatew[:, t, 1:2], y1_[:], ALU.mult, ALU.add),
                         ["ya%d" % i_, "yc%d" % i_, "gatew"], ["ya%d" % i_])
                    p.op("pool", lambda e, y1_=y1_, m=m: e.tensor_tensor(y1_[:], y1_[:], g2[m][:], ALU.mult), ["ya%d" % i_, "g2"], ["ya%d" % i_])
                    p.op("dve", lambda e, x_=x_, y1_=y1_: e.scalar_tensor_tensor(y1_[:], x_[:], ALPHA, y1_[:], ALU.mult, ALU.add),
                         [xk, "ya%d" % i_], ["ya%d" % i_])
                    ln_stats(y1_, "ya%d" % i_, B6["stt"], B6["mv"], B6["rstd"], B6["nmr"], "q")
                    p.op("act", lambda e, x_=x_, y1_=y1_: e.activation(x_[:], y1_[:], AF.Identity, bias=B6["nmr"][:], scale=B6["rstd"][:]),
                         ["ya%d" % i_, "qnmr", "qrstd"], [xk])
                    p.op("dve", lambda e, x_=x_: e.tensor_tensor(x_[:], x_[:], lnG[:], ALU.mult), [xk, "lnG"], [xk])
                    p.op("pool", lambda e, x_=x_: e.tensor_tensor(x_[:], x_[:], lnB[:], ALU.add), [xk, "lnB"], [xk])
                    if l == DEPTH - 1:
                        p.dma("sp", out_d[(t - 2) * 128:(t - 1) * 128, :], x_[:], reads=[xk])
                    else:
                        p.dma("sp", xres[tok, :], x_[:], reads=[xk], writes=["xres%d" % t])
                p.flush()
            if "L0" in dbg:
                break
        p.finish()
        print("instructions:", p.n_inst)
    return nc


def _consts(L):
    T = NCTX + L
    f32 = np.float32
    t = np.arange(T)
    lat = t >= NCTX
    i = np.where(lat, t - NCTX, 0)
    row = (i // 64).astype(f32)
    col = (i % 64).astype(f32)
    pos = t.astype(f32)
    fa = (10000.0 ** (-np.arange(32, dtype=f32) / 32)).astype(f32)
    fb = (10000.0 ** (-np.arange(16, dtype=f32) / 16)).astype(f32)

    def cs(pp, fr, mask):
        ang = (pp[:, None] * fr[None, :]).astype(f32)
        c = np.cos(ang).astype(f32)
        s = np.sin(ang).astype(f32)
        if mask is not None:
            c = np.where(mask[:, None], c, 1.0).astype(f32)
            s = np.where(mask[:, None], s, 0.0).astype(f32)
        return c, s
    cr, sr = cs(row, fa, lat)
    cc, sc = cs(col, fa, lat)
    swa = [np.concatenate([cr, cr, cc, cc], 1), np.concatenate([-sr, sr, -sc, sc], 1)]
    cp, sp = cs(pos, fa, None)
    ret = [np.concatenate([cp, cp], 1), np.concatenate([-sp, sp], 1)]
    cr, sr = cs(row, fb, lat)
    cc, sc = cs(col, fb, lat)
    mla = [np.concatenate([cr, cr, cc, cc], 1), np.concatenate([-sr, sr, -sc, sc], 1)]
    rtab = np.ascontiguousarray(np.concatenate(swa + ret + mla, 1).astype(f32))
    s = np.arange(128)[:, None]
    c = np.arange(128)[None, :]
    z = 0 * s + 0 * c
    cst = np.ascontiguousarray(np.stack([(s <= c) + z, (s >= c) + z, (c - s) + z, (c + 1) + z, (128 - c) + z, (s < c) + z], 1).astype(f32))
    scol = np.ascontiguousarray(np.stack([127 - np.arange(128), np.arange(128)], 1).astype(f32))
    NB = 2 * (T // 128) + NE
    jv = np.ascontiguousarray(np.tile((np.arange(NB, dtype=f32) * 128)[None, :], (128, 1)))
    pidx = np.arange(128, dtype=f32).reshape(128, 1)
    return dict(ident=np.eye(128, dtype=f32), rtab=rtab, cst=cst, scol=scol, jv=jv, pidx=pidx)


_WKEYS = ("w_ada", "b_ada", "w_in", "swa_sink", "ret_decay", "mla_kv_norm", "mla_w_uk", "mla_w_uv", "w_out",
          "ln1_g", "ln1_b", "ln2_g", "ln2_b", "moe_w_group", "moe_b_group", "moe_w_expert", "moe_b_expert",
          "moe_w_gate", "moe_w_up", "moe_w_down")


def core_inputs(inp, b, L, consts=None, names=None):
    m = dict(consts if consts is not None else _consts(L))
    m["x"] = np.ascontiguousarray(inp["x"][b], dtype=np.float32)
    m["ctx"] = np.ascontiguousarray(inp["ctx"][b], dtype=np.float32)
    m["c2"] = np.ascontiguousarray(np.stack([inp["c"][b], inp["c_ctx"]], 0), dtype=np.float32)
    for k in _WKEYS:
        if names is None or k in names:
            m[k] = np.ascontiguousarray(inp[k], dtype=np.float32)
    return m


_NC_CACHE = {}


def kernel(**inputs):
    L = int(inputs["x"].shape[1])
    Bn = int(inputs["x"].shape[0])
    if L not in _NC_CACHE:
        _NC_CACHE[L] = build(L)
    nc = _NC_CACHE[L]
    consts = _consts(L)
    inp = {k: np.asarray(v) for k, v in inputs.items()}
    in_maps = [core_inputs(inp, b, L, consts, names=nc._in_names) for b in range(Bn)]
    res = run_bass_kernel_spmd(nc, in_maps, core_ids=list(range(Bn)))
    out = np.stack([np.asarray(res.results[b]["out"]) for b in range(Bn)], 0)
    return out.astype(np.float32)
```
